# Optimizing a Trainium2 kernel written in Bass

```python
import math
import jax, jax.numpy as jnp
from jax import lax
import numpy as np

D_MODEL = 1024
BATCH = 8
SEQ = 8192
DEPTH = 2

GRID_W = 64
CTX_LEN = 256

SSD_EXPAND = 2
D_INNER = SSD_EXPAND * D_MODEL
SSD_HEADDIM = 64
SSD_HEADS = D_INNER // SSD_HEADDIM
SSD_GROUPS = 4
SSD_STATE = 128
SSD_CONV = 5
SSD_CHUNK = 128
D_BC = SSD_GROUPS * SSD_STATE
D_XBC = D_INNER + 2 * D_BC

POOL_WINDOWS = (2, 4, 8, 16)
POOL_WIDTH = D_MODEL
POOL_GROUPS = len(POOL_WINDOWS)
POOL_GROUP_DIM = POOL_WIDTH // POOL_GROUPS

IN_SPLITS = (D_INNER, D_INNER + D_XBC, D_INNER + D_XBC + 2 * SSD_HEADS,
             D_INNER + D_XBC + 2 * SSD_HEADS + POOL_WIDTH)
IN_COLS = IN_SPLITS[-1] + 2 * D_MODEL

MOE_GROUPS = 4
MOE_EXPERTS_PER_GROUP = 8
MOE_EXPERTS = MOE_GROUPS * MOE_EXPERTS_PER_GROUP
MOE_TOP_K = 2
D_EXPERT = 512
MOE_BLOCK = 256

DEEPNORM_ALPHA = (2.0 * DEPTH) ** 0.25
DEEPNORM_BETA = (8.0 * DEPTH) ** -0.25
NORM_EPS = 1e-5

kernel_name = 'hybrid_ssd_pool_hmoe_prefix_dit'


def layer_norm(x, g, b):
    xf = x.astype(jnp.float32)
    mu = xf.mean(-1, keepdims=True)
    var = jnp.square(xf - mu).mean(-1, keepdims=True)
    return ((xf - mu) * lax.rsqrt(var + NORM_EPS) * g.astype(jnp.float32) + b.astype(jnp.float32)).astype(x.dtype)


def post_norm(x, y, g, b):
    return layer_norm(DEEPNORM_ALPHA * x + y, g, b)


def modulate(x, shift, scale):
    return x * (1 + scale) + shift


def rms_norm_gated(y, z, g):
    yz = y.astype(jnp.float32) * jax.nn.silu(z.astype(jnp.float32))
    return yz * lax.rsqrt(jnp.mean(yz * yz, -1, keepdims=True) + NORM_EPS) * g.astype(jnp.float32)


def flip_seq(t):
    return t[:, ::-1]


def conv_centred(u, w, b):
    ch = u.shape[-1]
    y = lax.conv_general_dilated(u, w[:, None, :].astype(u.dtype), window_strides=(1,),
                                 padding=((SSD_CONV // 2, SSD_CONV // 2),),
                                 dimension_numbers=('NWC', 'WIO', 'NWC'), feature_group_count=ch)
    return y + b


def ssd_decay(dt_raw, lp):
    b, l, _ = dt_raw.shape
    dt = jax.nn.softplus(dt_raw.reshape(b, l, 2, SSD_HEADS).astype(jnp.float32) + lp['dt_bias'].astype(jnp.float32))
    a = dt * -jnp.exp(lp['a_log'].astype(jnp.float32))
    return dt, a


def ssd_scan(xdt, a, bm, cm, h0):
    b, l = a.shape[:2]
    r = SSD_HEADS // SSD_GROUPS
    q = SSD_CHUNK
    nc = l // q

    def chunks(t):
        return jnp.moveaxis(t.reshape((b, nc, q) + t.shape[2:]), 1, 0)

    xs = (chunks(xdt.reshape(b, l, SSD_GROUPS, r, SSD_HEADDIM)), chunks(a.reshape(b, l, SSD_GROUPS, r)),
          chunks(bm), chunks(cm))
    lower = jnp.tril(jnp.ones((q, q), dtype=bool))[None, :, :, None, None]

    def step(state, inp):
        xc, ac, bc, cc = inp
        acs = jnp.cumsum(ac, axis=1)
        seg = acs[:, :, None] - acs[:, None, :]
        decay = jnp.exp(jnp.where(lower, seg, -jnp.inf))
        cb = jnp.einsum('bign,bjgn->bijg', cc, bc)
        y_diag = jnp.einsum('bijg,bijgr,bjgrp->bigrp', cb, decay, xc)
        y_off = jnp.einsum('bign,bgrpn->bigrp', cc, state) * jnp.exp(acs)[..., None]
        tail = jnp.exp(acs[:, -1:] - acs)
        state = state * jnp.exp(acs[:, -1])[..., None, None] + jnp.einsum('bjgn,bjgr,bjgrp->bgrpn', bc, tail, xc)
        return state, y_diag + y_off

    h_final, ys = lax.scan(step, h0.reshape(b, SSD_GROUPS, r, SSD_HEADDIM, SSD_STATE), xs)
    y = jnp.moveaxis(ys, 0, 1).reshape(b, l, SSD_HEADS, SSD_HEADDIM)
    return y, h_final.reshape(b, SSD_HEADS, SSD_HEADDIM, SSD_STATE)


def ssd_final_state(xdt, a, bm):
    b, l = a.shape[:2]
    r = SSD_HEADS // SSD_GROUPS
    acs = jnp.cumsum(a, axis=1)
    tail = jnp.exp(acs[:, -1:] - acs).reshape(b, l, SSD_GROUPS, r)
    st = jnp.einsum('blgn,blgr,blgrp->bgrpn', bm, tail, xdt.reshape(b, l, SSD_GROUPS, r, SSD_HEADDIM))
    return st.reshape(b, SSD_HEADS, SSD_HEADDIM, SSD_STATE)


def ssd_branch(z, xbc, dt_raw, lp, h0_f, h0_b):
    b, l, _ = z.shape
    xbc = jax.nn.silu(conv_centred(xbc, lp['conv_w'], lp['conv_b'])).astype(jnp.float32)
    xs, bm, cm = jnp.split(xbc, (D_INNER, D_INNER + D_BC), axis=-1)
    xs = xs.reshape(b, l, SSD_HEADS, SSD_HEADDIM)
    bm = bm.reshape(b, l, SSD_GROUPS, SSD_STATE)
    cm = cm.reshape(b, l, SSD_GROUPS, SSD_STATE)
    dt, a = ssd_decay(dt_raw, lp)
    y_f, s_f = ssd_scan(xs * dt[:, :, 0, :, None], a[:, :, 0], bm, cm, h0_f)
    y_b, s_b = ssd_scan(flip_seq(xs * dt[:, :, 1, :, None]), flip_seq(a[:, :, 1]), flip_seq(bm), flip_seq(cm), h0_b)
    y = y_f + flip_seq(y_b) + xs * lp['d_skip'].astype(jnp.float32)[:, None]
    y = rms_norm_gated(y.reshape(b, l, D_INNER), z, lp['ssd_norm_g'])
    return y.astype(z.dtype), s_f, s_b


def ssd_context_states(hc, lp):
    b, l, _ = hc.shape
    w = lp['w_in']
    xb = hc @ w[:, IN_SPLITS[0]:IN_SPLITS[0] + D_INNER + D_BC]
    dt_raw = hc @ w[:, IN_SPLITS[1]:IN_SPLITS[2]]
    xb = jax.nn.silu(conv_centred(xb, lp['conv_w'][:, :D_INNER + D_BC], lp['conv_b'][:D_INNER + D_BC])).astype(jnp.float32)
    xs, bm = jnp.split(xb, (D_INNER,), axis=-1)
    xs = xs.reshape(b, l, SSD_HEADS, SSD_HEADDIM)
    bm = bm.reshape(b, l, SSD_GROUPS, SSD_STATE)
    dt, a = ssd_decay(dt_raw, lp)
    s_f = ssd_final_state(xs * dt[:, :, 0, :, None], a[:, :, 0], bm)
    s_b = ssd_final_state(flip_seq(xs * dt[:, :, 1, :, None]), flip_seq(a[:, :, 1]), flip_seq(bm))
    return s_f, s_b


def window_sum(u, axis, k):
    n = u.shape[axis]
    pad = [(0, 0)] * u.ndim
    pad[axis] = (1, 0)
    cs = jnp.cumsum(jnp.pad(u, pad), axis=axis)
    pos = jnp.arange(n)
    lo = jnp.clip(pos - k // 2, 0, n)
    hi = jnp.clip(pos - k // 2 + k, 0, n)
    s = jnp.take(cs, hi, axis=axis) - jnp.take(cs, lo, axis=axis)
    return s, (hi - lo).astype(jnp.float32)


def pool_grid(uf):
    b, l = uf.shape[:2]
    rows = l // GRID_W
    ug_all = uf.reshape(b, rows, GRID_W, POOL_GROUPS, POOL_GROUP_DIM)
    outs = []
    for gi, k in enumerate(POOL_WINDOWS):
        ug = ug_all[:, :, :, gi]
        s, cnt_r = window_sum(ug, 1, k)
        s, cnt_c = window_sum(s, 2, k)
        outs.append(s / (cnt_r[:, None] * cnt_c[None, :])[None, :, :, None])
    return jnp.stack(outs, axis=3).reshape(b, l, POOL_GROUPS, POOL_GROUP_DIM)


def pool_seq(uf):
    outs = []
    for gi, k in enumerate(POOL_WINDOWS):
        s, cnt = window_sum(uf[:, :, gi], 1, k)
        outs.append(s / cnt[None, :, None])
    return jnp.stack(outs, axis=2)


def pool_branch(u, lp, on_grid):
    b, l, _ = u.shape
    uf = u.astype(jnp.float32).reshape(b, l, POOL_GROUPS, POOL_GROUP_DIM)
    mean = pool_grid(uf) if on_grid else pool_seq(uf)
    y = jnp.einsum('blgc,gcd->blgd', mean - uf, lp['pool_w'].astype(jnp.float32)).reshape(b, l, POOL_WIDTH)
    return (y * lp['pool_scale'].astype(jnp.float32)).astype(u.dtype)


def mix_stream(h, lp, on_grid, h0_f, h0_b):
    z, xbc, dt_raw, u, gates = jnp.split(h @ lp['w_in'], IN_SPLITS, axis=-1)
    y_a, s_f, s_b = ssd_branch(z, xbc, dt_raw, lp, h0_f, h0_b)
    y_p = pool_branch(u, lp, on_grid)
    g = jax.nn.sigmoid((gates + lp['b_gate']).astype(jnp.float32)).astype(h.dtype)
    g_a, g_p = jnp.split(g, 2, axis=-1)
    merged = g_a * (y_a @ lp['w_branch_a']) + g_p * (y_p @ lp['w_branch_b'])
    return merged @ lp['w_out'], s_f, s_b


def swiglu(xb, wg, wu, wd):
    return (jax.nn.silu(xb @ wg) * (xb @ wu)) @ wd


def hier_moe(h, lp):
    t, d = h.shape
    hf = h.astype(jnp.float32)
    lg = hf @ lp['w_rg'].astype(jnp.float32) + lp['b_rg'].astype(jnp.float32)
    grp = jnp.argmax(lg, axis=-1)
    p_grp = jnp.take_along_axis(jax.nn.softmax(lg, axis=-1), grp[:, None], axis=-1)
    le = jnp.einsum('td,gde->tge', hf, lp['w_re'].astype(jnp.float32)) + lp['b_re'].astype(jnp.float32)
    le = jnp.take_along_axis(le, grp[:, None, None], axis=1)[:, 0]
    top_v, top_i = lax.top_k(le, MOE_TOP_K)
    w = p_grp * jax.nn.softmax(top_v, axis=-1)
    eid = grp[:, None].astype(jnp.int32) * MOE_EXPERTS_PER_GROUP + top_i.astype(jnp.int32)
    n_assign = t * MOE_TOP_K
    flat_e = eid.reshape(-1)
    flat_t = jnp.repeat(jnp.arange(t, dtype=jnp.int32), MOE_TOP_K)
    order = jnp.argsort(flat_e)
    se, st, sw = flat_e[order], flat_t[order], w.reshape(-1)[order]
    counts = jnp.bincount(flat_e, length=MOE_EXPERTS)
    padded = (counts + MOE_BLOCK - 1) // MOE_BLOCK * MOE_BLOCK
    pad_end = jnp.cumsum(padded)
    pad_start = pad_end - padded
    start = jnp.cumsum(counts) - counts
    dest = pad_start[se] + jnp.arange(n_assign, dtype=jnp.int32) - start[se]
    n_blocks = -(-n_assign // MOE_BLOCK) + MOE_EXPERTS
    n_rows = n_blocks * MOE_BLOCK
    row_tok = jnp.full((n_rows,), t, jnp.int32).at[dest].set(st)
    block_e = jnp.minimum(jnp.searchsorted(pad_end, jnp.arange(n_blocks, dtype=jnp.int32) * MOE_BLOCK, side='right'),
                          MOE_EXPERTS - 1)
    h_pad = jnp.concatenate([h, jnp.zeros((1, d), h.dtype)], axis=0)
    xin = h_pad[row_tok].reshape(n_blocks, MOE_BLOCK, d)

    def expert_block(args):
        xb, e = args
        return swiglu(xb, lp['w_eg'][e], lp['w_eu'][e], lp['w_ed'][e])

    yb = lax.map(expert_block, (xin, block_e)).reshape(n_rows, d)
    return jax.ops.segment_sum(yb[dest] * sw[:, None].astype(h.dtype), st, num_segments=t)


def setup_inputs(seed: int = 0) -> dict:
    key = jax.random.key(seed)
    ks = jax.random.split(key, 32)
    f32 = jnp.float32
    L, D = DEPTH, D_MODEL

    def nrm(k, shape, scale):
        return jax.random.normal(k, shape, f32) * scale

    dt0 = jnp.exp(jax.random.uniform(ks[8], (L, 2, SSD_HEADS), f32, math.log(1e-3), math.log(1e-1)))
    return {
        'x': nrm(ks[0], (BATCH, SEQ, D), 1.0),
        'c': nrm(ks[1], (BATCH, D), 1.0),
        'ctx': nrm(ks[2], (BATCH, CTX_LEN, D), 1.0),
        'c_ctx': nrm(ks[3], (D,), 1.0),
        'w_ada': nrm(ks[4], (L, D, 6 * D), D ** -0.5),
        'b_ada': nrm(ks[5], (L, 6 * D), 0.02),
        'w_in': nrm(ks[6], (L, D, IN_COLS), D ** -0.5),
        'b_gate': nrm(ks[7], (L, 2 * D), 0.1),
        'conv_w': nrm(ks[9], (L, SSD_CONV, D_XBC), SSD_CONV ** -0.5),
        'conv_b': nrm(ks[10], (L, D_XBC), 0.02),
        'dt_bias': dt0 + jnp.log(-jnp.expm1(-dt0)),
        'a_log': jnp.log(jax.random.uniform(ks[11], (L, 2, SSD_HEADS), f32, 1.0, 16.0)),
        'd_skip': 1.0 + nrm(ks[12], (L, SSD_HEADS), 0.1),
        'ssd_norm_g': 1.0 + nrm(ks[13], (L, D_INNER), 0.1),
        'pool_w': nrm(ks[14], (L, POOL_GROUPS, POOL_GROUP_DIM, POOL_GROUP_DIM), POOL_GROUP_DIM ** -0.5),
        'pool_scale': 1.0 + nrm(ks[15], (L, POOL_WIDTH), 0.1),
        'w_branch_a': nrm(ks[16], (L, D_INNER, D), D_INNER ** -0.5),
        'w_branch_b': nrm(ks[17], (L, POOL_WIDTH, D), POOL_WIDTH ** -0.5),
        'w_out': nrm(ks[18], (L, D, D), D ** -0.5 * DEEPNORM_BETA),
        'ln1_g': 1.0 + nrm(ks[19], (L, D), 0.1),
        'ln1_b': nrm(ks[20], (L, D), 0.02),
        'ln2_g': 1.0 + nrm(ks[21], (L, D), 0.1),
        'ln2_b': nrm(ks[22], (L, D), 0.02),
        'w_router_group': nrm(ks[23], (L, D, MOE_GROUPS), D ** -0.5),
        'b_router_group': nrm(ks[24], (L, MOE_GROUPS), 0.01),
        'w_router_expert': nrm(ks[25], (L, MOE_GROUPS, D, MOE_EXPERTS_PER_GROUP), D ** -0.5),
        'b_router_expert': nrm(ks[26], (L, MOE_GROUPS, MOE_EXPERTS_PER_GROUP), 0.01),
        'w_expert_gate': nrm(ks[27], (L, MOE_EXPERTS, D, D_EXPERT), D ** -0.5),
        'w_expert_up': nrm(ks[28], (L, MOE_EXPERTS, D, D_EXPERT), D ** -0.5),
        'w_expert_down': nrm(ks[29], (L, MOE_EXPERTS, D_EXPERT, D), D_EXPERT ** -0.5 * DEEPNORM_BETA),
    }


def reference(x, c, ctx, c_ctx, w_ada, b_ada, w_in, b_gate, conv_w, conv_b, dt_bias, a_log, d_skip,
              ssd_norm_g, pool_w, pool_scale, w_branch_a, w_branch_b, w_out, ln1_g, ln1_b, ln2_g, ln2_b,
              w_router_group, b_router_group, w_router_expert, b_router_expert,
              w_expert_gate, w_expert_up, w_expert_down):
    b, l, d = x.shape
    lc = ctx.shape[1]
    xl, xc = x, ctx
    zero_state = jnp.zeros((b, SSD_HEADS, SSD_HEADDIM, SSD_STATE), jnp.float32)
    for i in range(DEPTH):
        last = i == DEPTH - 1
        lp = {'w_in': w_in[i], 'b_gate': b_gate[i], 'conv_w': conv_w[i], 'conv_b': conv_b[i],
              'dt_bias': dt_bias[i], 'a_log': a_log[i], 'd_skip': d_skip[i], 'ssd_norm_g': ssd_norm_g[i],
              'pool_w': pool_w[i], 'pool_scale': pool_scale[i], 'w_branch_a': w_branch_a[i],
              'w_branch_b': w_branch_b[i], 'w_out': w_out[i],
              'w_rg': w_router_group[i], 'b_rg': b_router_group[i],
              'w_re': w_router_expert[i], 'b_re': b_router_expert[i],
              'w_eg': w_expert_gate[i], 'w_eu': w_expert_up[i], 'w_ed': w_expert_down[i]}
        mod_l = jnp.split((jax.nn.silu(c) @ w_ada[i] + b_ada[i])[:, None, :], 6, axis=-1)
        mod_c = jnp.split((jax.nn.silu(c_ctx) @ w_ada[i] + b_ada[i])[None, None, :], 6, axis=-1)

        hc = modulate(xc, mod_c[0], mod_c[1])
        hl = modulate(xl, mod_l[0], mod_l[1])
        if last:
            s_f, s_b = ssd_context_states(hc, lp)
        else:
            out_c, s_f, s_b = mix_stream(hc, lp, False, zero_state, zero_state)
            xc = post_norm(xc, mod_c[2] * out_c, ln1_g[i], ln1_b[i])
        out_l, _, _ = mix_stream(hl, lp, True, s_f, s_b)
        xl = post_norm(xl, mod_l[2] * out_l, ln1_g[i], ln1_b[i])

        hl = modulate(xl, mod_l[3], mod_l[4]).reshape(b * l, d)
        if last:
            yl = hier_moe(hl, lp)
        else:
            hc = modulate(xc, mod_c[3], mod_c[4]).reshape(b * lc, d)
            y = hier_moe(jnp.concatenate([hl, hc], axis=0), lp)
            yl, yc = y[:b * l], y[b * l:]
            xc = post_norm(xc, mod_c[5] * yc.reshape(b, lc, d), ln2_g[i], ln2_b[i])
        xl = post_norm(xl, mod_l[5] * yl.reshape(b, l, d), ln2_g[i], ln2_b[i])
    return xl
```

```python
import numpy as np
import ml_dtypes
import concourse.bass as bass
import concourse.mybir as mybir
from concourse.bass_utils import run_bass_kernel_spmd

F32 = mybir.dt.float32
BF16 = mybir.dt.bfloat16
I32 = mybir.dt.int32
AF = mybir.ActivationFunctionType
ALU = mybir.AluOpType
AX = mybir.AxisListType

D = 1024
LCTX = 256
LLAT = 8192
T = LCTX + LLAT
NCH = T // 128
DI = 2048
NH = 32
HP = 64
NG = 4
NS = 128
DXBC = 3072
INC = 8256
C_Z, C_XBC, C_DT, C_U, C_G = 0, 2048, 5120, 5184, 6208
NE = 32
DE = 512
NBLK = 98
NROWS = NBLK * 256
ALPHA = (2.0 * 2) ** 0.25
EPS = 1e-5
GEN = 30000
SAME_SYNC = True
USE_ALIAS = True


import re
_CANON_PAT = r"^(st_|bc_)?(?:[A-H]\d_\d_|[A-H]\d_)"


_ONESHOT = ("ident", "sel", "cmat", "cvec", "cc", "bada", "bg", "modrows_d", "c1", "c2", "c3", "c4", "c5", "c6",
            "pc1", "pc2", "pc3", "pc4", "pc5")


_ALIAS = {"bc_gate0": "g0", "bc_scb0": "g0", "bc_shb0": "g2", "bc_gate1": "g1", "bc_scb1": "g1", "bc_shb1": "g3",
          "pP2": "pP0", "pP3": "pP1", "zf2": "zf0", "zf3": "zf1",
          "sc0_0": "r1_0", "sc1_0": "r1_1", "sc2_0": "r1_2", "sc0_1": "r2_0", "sc1_1": "r2_1", "sc2_1": "r2_2",
          "st_yb0": "st_x1_0", "st_yb1": "st_x1_1", "st_x2_0": "st_x1_0", "st_x2_1": "st_x1_1",
          "wd0": "xs0", "wd1": "xs1", "wgu0": "bt0", "wgu1": "bt1", "xr0": "bct0", "xr1": "bct1",
          "rb0_0": "ut0", "rb0_1": "ut1", "rb1_0": "ut2", "rb1_1": "ut3", "rb2_0": "pP0", "rb2_1": "pP1",
          "rb3_0": "zf0", "rb3_1": "zf1", "wada0": "wld0", "wada1": "wld1", "st_cumT0": "st_dt0", "st_cumT1": "st_dt1"}


def _canon(key):
    key = re.sub(_CANON_PAT, lambda m: (m.group(1) or ""), key)
    if key in _ONESHOT:
        return "once%d" % (_ONESHOT.index(key) % 3)
    return _ALIAS.get(key, key) if USE_ALIAS else key


class _Op:
    __slots__ = ("fn", "deps", "key", "val", "sig", "cnt", "eng", "idx", "pseudo")


class Prog:
    ENGS = ("pe", "act", "dve", "pool", "sp")

    def __init__(self, nc):
        self.nc = nc
        self.ops = {e: [] for e in self.ENGS}
        self.lw = {}
        self.rd = {}
        self.keys = {}
        self.lastkey = {}

    def op(self, eng, fn, reads=(), writes=(), key=None, multi=False, extra=()):
        deps = set(extra)
        for r in reads:
            deps.update(w for w, _ in self.lw.get(r, ()))
        for wr in writes:
            if multi:
                deps.update(w for w, m in self.lw.get(wr, ()) if not m)
            else:
                deps.update(w for w, _ in self.lw.get(wr, ()))
            deps.update(self.rd.get(wr, ()))
        if key is not None:
            key = _canon(key)
        if key is not None and key in self.lastkey:
            deps.add(self.lastkey[key])
        o = _Op()
        o.fn = fn
        o.eng = eng
        o.idx = len(self.ops[eng])
        o.deps = deps
        o.key = key
        o.sig = False
        o.pseudo = False
        o.cnt = 0
        o.val = 0
        if key is not None:
            v = self.keys.get(key, 0) + 16
            self.keys[key] = v
            o.val = v
        self.ops[eng].append(o)
        for wr in writes:
            if multi:
                if self.rd.get(wr):
                    self.lw[wr] = [(o, True)]
                    self.rd[wr] = []
                else:
                    self.lw.setdefault(wr, []).append((o, True))
            else:
                self.lw[wr] = [(o, False)]
                self.rd[wr] = []
        for r in reads:
            self.rd.setdefault(r, []).append(o)
        if key is not None:
            self.lastkey[key] = o
        return o

    def barrier(self, final=False):
        tails = []
        for e in self.ENGS:
            for o in reversed(self.ops[e]):
                if not o.pseudo and o.key is None:
                    tails.append(o)
                    break
        tails += [o for k, o in self.lastkey.items() if final or not k.startswith("bg")]
        for e in self.ENGS:
            self.op(e, lambda eng: None, extra=tails).pseudo = True
        self.lw = {k: v for k, v in self.lw.items() if isinstance(k, str) and k.startswith("bg:")}
        self.rd = {k: v for k, v in self.rd.items() if isinstance(k, str) and k.startswith("bg:")}

    def emit(self):
        nc = self.nc
        for e in self.ENGS:
            for o in self.ops[e]:
                for d in o.deps:
                    if d.key is None and (d.eng != e or (SAME_SYNC and e != "pe")):
                        d.sig = True
        ngen = {}
        for e in self.ENGS:
            c = 0
            for o in self.ops[e]:
                if o.key is None and o.sig:
                    c += 1
                o.cnt = c
            ngen[e] = max(1, (c + GEN - 1) // GEN)
        self.counts = {e: (len(self.ops[e]), self.ops[e][-1].cnt if self.ops[e] else 0) for e in self.ENGS}
        import contextlib
        with contextlib.ExitStack() as st:
            esem = {e: [st.enter_context(nc.semaphore("s_%s_%d" % (e, g))) for g in range(ngen[e])]
                    for e in self.ENGS}
            dsem = {k: st.enter_context(nc.semaphore("d_%d" % i)) for i, k in enumerate(self.keys)}
            block = st.enter_context(nc.Block())

            def run(e, eng):
                seen = {}
                for o in self.ops[e]:
                    waits = {}
                    for d in o.deps:
                        if d.key is not None:
                            sem, v = dsem[d.key], d.val
                        elif d.eng != e or (SAME_SYNC and e != "pe"):
                            g = (d.cnt - 1) // GEN
                            sem, v = esem[d.eng][g], d.cnt - g * GEN
                        else:
                            continue
                        if v > waits.get(sem, (0, None))[0]:
                            waits[sem] = (v, sem)
                    for v, sem in waits.values():
                        if seen.get(sem, 0) >= v:
                            continue
                        seen[sem] = v
                        eng.wait_ge(sem, v)
                    ins = o.fn(eng)
                    if ins is None:
                        continue
                    if o.key is not None:
                        ins.then_inc(dsem[o.key], 16)
                    elif o.sig:
                        g = (o.cnt - 1) // GEN
                        ins.then_inc(esem[e][g], 1)

            @block.tensor
            def _(eng):
                run("pe", eng)

            @block.scalar
            def _(eng):
                run("act", eng)

            @block.vector
            def _(eng):
                run("dve", eng)

            @block.gpsimd
            def _(eng):
                run("pool", eng)

            @block.sync
            def _(eng):
                run("sp", eng)


class Ctx:
    pass


def chunk_stream(c):
    return 0 if c < 2 else 1


def build(nlayers=2, stop=None, dbg=()):
    nc = bass.Bass("TRN2", target_bir_lowering=False)
    P = Prog(nc)
    K = Ctx()
    K.nc, K.P = nc, P
    dt = nc.dram_tensor

    def ext(name, shape, dtype=F32):
        return dt(name, list(shape), dtype, kind="ExternalInput").ap()

    I = {}
    I["xcat"] = ext("xcat", [T, D])
    I["cc"] = ext("cc", [128, 8, 2])
    I["w_ada"] = ext("w_ada", [2, D, 6 * D])
    I["b_ada"] = ext("b_ada", [2, 6 * D])
    I["w_in"] = ext("w_in", [2, D, INC])
    I["b_gate"] = ext("b_gate", [2, 128, 16])
    I["ident"] = ext("ident", [128, 128])
    I["cmat"] = ext("cmat", [128, 7, 128])
    I["cw"] = ext("cw", [2, 128, 5, 24])
    I["cbfm"] = ext("cbfm", [2, 128, 24])
    I["conv_b"] = ext("conv_b", [2, DXBC])
    I["dt_bias"] = ext("dt_bias", [2, 64])
    I["a_log"] = ext("a_log", [2, 64])
    I["d_skip"] = ext("d_skip", [2, 32])
    I["poolP"] = ext("poolP", [3, 128, 31, 512], BF16)
    I["poolC"] = ext("poolC", [128, 8, 256], BF16)
    I["pool_w"] = ext("pool_w", [2, 4, 256, 256])
    I["pscfm"] = ext("pscfm", [2, 128, 8])
    I["gnfm"] = ext("gnfm", [2, 128, 16])
    I["w_branch_a"] = ext("w_branch_a", [2, DI, D])
    I["w_branch_b"] = ext("w_branch_b", [2, D, D])
    I["w_out"] = ext("w_out", [2, D, D])
    I["ln1_g"] = ext("ln1_g", [2, D])
    I["ln1_b"] = ext("ln1_b", [2, D])
    I["ln2_g"] = ext("ln2_g", [2, D])
    I["ln2_b"] = ext("ln2_b", [2, D])
    I["wr"] = ext("wr", [2, 128, 8, 36])
    I["br"] = ext("br", [2, 36])
    I["wgu"] = ext("wgu", [2, NE * 128, 8 * 1024])
    I["wdr"] = ext("wdr", [2, NE * 128, 4 * 1024])
    I["cvec"] = ext("cvec", [128, 197])
    I["sel"] = ext("sel", [2, 2, 128])
    K.I = I
    out = dt("out", [LLAT, D], F32, kind="ExternalOutput").ap()
    K.out = out
    S = {}

    def scr(name, shape, dtype):
        S[name] = dt(name, list(shape), dtype, kind=("ExternalOutput" if name in dbg else "Internal")).ap()
    scr("xres", [T, D], F32)
    scr("modrows", [2, 2, 6 * D], F32)
    scr("sz", [T, DI], BF16)
    scr("xbcT", [DXBC, T], BF16)
    scr("dtraw", [T, 64], F32)
    scr("u", [T, D], BF16)
    scr("gT", [DI, T], BF16)
    scr("xs", [T, DI], BF16)
    scr("Bt", [T, 512], BF16)
    scr("bct", [NCH, 128, 1024], BF16)
    scr("sbin", [NCH, 128, 2048], BF16)
    scr("yaT", [DI, T], BF16)
    scr("ypT", [D, T], BF16)
    scr("x1", [T, D], F32)
    scr("h2T", [D, T], BF16)
    scr("wgub", [2 * NE * 128, 8 * 1024], BF16)
    scr("wdb", [2 * NE * 128, 4 * 1024], BF16)
    scr("cumT", [NCH, 64, 128], F32)
    scr("xin", [NROWS, D], BF16)
    scr("yrows", [NROWS, D], F32)
    if "dest" in dbg:
        S["dest"] = dt("dest", [128, NCH * 2], I32, kind="ExternalOutput").ap()
        S["idxw"] = dt("idxw", [128, NBLK], I32, kind="ExternalOutput").ap()
        S["e12"] = dt("e12", [128, NCH * 2], F32, kind="ExternalOutput").ap()
        S["w12"] = dt("w12", [128, NCH * 2], F32, kind="ExternalOutput").ap()
    K.S = S
    K.dbg = {}

    import contextlib
    with contextlib.ExitStack() as st:
        def sb(name, shape, dtype=F32):
            return st.enter_context(nc.sbuf_tensor("sb_" + name, list(shape), dtype))
        K.sb = sb
        K.ps = [st.enter_context(nc.psum_tensor("ps%d" % i, [128, 512], F32)) for i in range(8)]
        K.ident = sb("ident", [128, 128])
        K.identb = sb("identb", [128, 128], BF16)
        K.sel = sb("sel", [2, 2, 128])
        P.op("sp", lambda e: e.dma_start(out=K.ident[:], in_=I["ident"]), writes=["ident"], key="ident")
        P.op("sp", lambda e: e.dma_start(out=K.sel[:], in_=I["sel"]), writes=["sel"], key="sel")
        P.op("dve", lambda e: e.tensor_copy(out=K.identb[:], in_=K.ident[:]), reads=["ident"], writes=["identb"])
        K.cm = sb("cmat", [128, 7, 128])
        K.cb = sb("cmatb", [128, 7, 128], BF16)
        K.cvec = sb("cvec", [128, 197])
        P.op("sp", lambda e: e.dma_start(out=K.cvec[:], in_=I["cvec"]), writes=["cvec"], key="cvec")
        P.op("sp", lambda e: e.dma_start(out=K.cm[:], in_=I["cmat"]), writes=["cmat"], key="cmat")
        P.op("dve", lambda e: e.tensor_copy(out=K.cb[:], in_=K.cm[:]), reads=["cmat"], writes=["cmatb"])
        for l in range(nlayers):
            layer(K, l, stop)
        P.barrier(final=True)
        P.emit()
    return nc


def phase_mod(K, l, st):
    nc, P, I = K.nc, K.P, K.I
    sb = lambda n, s, d=F32: st.enter_context(nc.sbuf_tensor("sb_" + n, list(s), d))
    cc = sb("cc%d" % l, [128, 8, 2])
    cs = sb("cs%d" % l, [128, 8, 2], BF16)
    bada = sb("bada%d" % l, [2, 6 * D])
    P.op("sp", lambda e: e.dma_start(out=cc[:], in_=I["cc"]), writes=["cc"], key="cc")
    P.op("sp", lambda e: e.dma_start(out=bada[:], in_=I["b_ada"][l].partition_broadcast(2)), writes=["bada"], key="bada")
    P.op("act", lambda e: e.activation(out=cs[:], in_=cc[:], func=AF.Silu), reads=["cc"], writes=["cs"])
    wbuf = [sb("wada%d_%d" % (l, i), [128, 8, 1024], BF16) for i in range(2)]
    rows = sb("modrows%d" % l, [2, 6 * D])
    for blk in range(6):
        wb = wbuf[blk % 2]
        wn = "wada%d" % (blk % 2)
        P.op("pool", lambda e, wb=wb, blk=blk: e.dma_start(
            out=wb[:], in_=I["w_ada"][l, :, blk * 1024:(blk + 1) * 1024].rearrange("(k p) n -> p k n", p=128)),
            writes=[wn], key=wn)
        for hf in range(2):
            ps = K.ps[hf]
            pn = "ps%d" % hf

            def mm(e, wb=wb, hf=hf, ps=ps):
                for k in range(8):
                    ins = e.matmul(ps[0:2, :], cs[:, k, :], wb[:, k, hf * 512:(hf + 1) * 512],
                                   start=(k == 0), stop=(k == 7))
                return ins
            P.op("pe", mm, reads=["cs", wn], writes=[pn])
            c0 = blk * 1024 + hf * 512
            P.op("dve", lambda e, ps=ps, c0=c0: e.tensor_tensor(
                out=rows[0:2, c0:c0 + 512], in0=ps[0:2, :], in1=bada[0:2, c0:c0 + 512], op=ALU.add),
                reads=[pn, "bada"], writes=["modrows"])
    P.op("sp", lambda e: e.dma_start(out=K.S["modrows"][l], in_=rows[:]), reads=["modrows"], writes=["modrows_d"], key="modrows_d")
    modfm = K.modfm
    ps = K.ps[2]

    def tr(e):
        for j in range(48):
            ins = e.matmul(ps[:, 2 * j:2 * j + 2], rows[0:2, j * 128:(j + 1) * 128], K.ident[0:2, 0:2],
                           start=True, stop=True)
        return ins
    P.op("pe", tr, reads=["modrows", "ident"], writes=["ps2"])
    P.op("dve", lambda e: e.tensor_copy(out=modfm[:].rearrange("p j s -> p (j s)"), in_=ps[:, 0:96]),
         reads=["ps2"], writes=["modfm"])
    for a in (8, 32):
        P.op("dve", lambda e, a=a: e.tensor_scalar_add(out=modfm[:, a:a + 8, :], in0=modfm[:, a:a + 8, :], scalar1=1.0),
             reads=["modfm"], writes=["modfm"])


def bcast_rows(K, l, dst, dname, col0, s):
    K.P.op("sp", lambda e: e.dma_start(out=dst[:], in_=K.S["modrows"][l, s, col0:col0 + 1024].partition_broadcast(128)),
           writes=[dname], key="bc_" + dname)


def load_xT(K, st_name, src, c, xt, hT, q, shcol, sccol, want_f32=None):
    P = K.P
    s = chunk_stream(c)
    xn = st_name
    P.op("sp", lambda e: e.dma_start(out=xt[:], in_=src[c * 128:(c + 1) * 128, :]),
         reads=[("x", c)], writes=[xn], key=xn)
    for hf in range(2):
        ps = K.ps[hf]
        pn = "ps%d" % hf

        def tr(e, ps=ps, hf=hf):
            for jj in range(4):
                j = hf * 4 + jj
                ins = e.matmul(ps[:, jj * 128:(jj + 1) * 128], xt[:, j * 128:(j + 1) * 128], K.ident[:],
                               start=True, stop=True)
            return ins
        P.op("pe", tr, reads=[xn, "ident"], writes=[pn])
        for jj in range(4):
            j = hf * 4 + jj
            o = hT[0][:, j, q * 128:(q + 1) * 128]
            sc = K.modfm[:, sccol + j, s:s + 1]
            sh = K.modfm[:, shcol + j, s:s + 1]
            if hf == 0:
                P.op("act", lambda e, ps=ps, jj=jj, o=o, sc=sc, sh=sh: e.activation(
                    out=o, in_=ps[:, jj * 128:(jj + 1) * 128], func=AF.Identity, bias=sh, scale=sc),
                    reads=[pn, "modfm"], writes=[hT[1]])
            else:
                P.op("dve", lambda e, ps=ps, jj=jj, o=o, sc=sc, sh=sh: e.tensor_scalar(
                    out=o, in0=ps[:, jj * 128:(jj + 1) * 128], scalar1=sc, scalar2=sh, op0=ALU.mult, op1=ALU.add),
                    reads=[pn, "modfm"], writes=[hT[1]])
            if want_f32 is not None:
                o2 = want_f32[0][:, j, q * 128:(q + 1) * 128]
                P.op("dve" if jj % 2 == 0 else "act",
                     (lambda e, ps=ps, jj=jj, o2=o2, sc=sc, sh=sh: e.tensor_scalar(
                         out=o2, in0=ps[:, jj * 128:(jj + 1) * 128], scalar1=sc, scalar2=sh, op0=ALU.mult, op1=ALU.add))
                     if jj % 2 == 0 else
                     (lambda e, ps=ps, jj=jj, o2=o2, sc=sc, sh=sh: e.activation(
                         out=o2, in_=ps[:, jj * 128:(jj + 1) * 128], func=AF.Identity, bias=sh, scale=sc)),
                     reads=[pn, "modfm"], writes=[want_f32[1]])


def groups():
    g = [(0, 2)]
    for k in range(16):
        g.append((2 + 4 * k, 4))
    return g


def pass_inproj(K, l, src, sub):
    nc, P, I, S = K.nc, K.P, K.I, K.S
    import contextlib
    with contextlib.ExitStack() as st:
        sb = lambda n, s, d=F32: st.enter_context(nc.sbuf_tensor("sb_" + n, list(s), d))
        if sub == 0:
            col0, ncol = 0, 5120
        else:
            col0, ncol = 5120, 3136
        tg = "A%d_%d_" % (l, sub)
        w = sb(tg + "w", [128, 8, ncol], BF16)
        piece = 640 if sub == 0 else 784
        for pi in range(ncol // piece):
            P.op("pool", lambda e, pi=pi: e.dma_start(
                out=w[:, :, pi * piece:(pi + 1) * piece],
                in_=I["w_in"][l, :, col0 + pi * piece:col0 + (pi + 1) * piece].rearrange("(k p) n -> p k n", p=128)),
                writes=[tg + "w"], key="wld%d" % (pi % 4), multi=True)
        xts = [sb(tg + "x%d" % i, [128, D]) for i in range(2)]
        hTs = [sb(tg + "h%d" % i, [128, 8, 512], BF16) for i in range(2)]
        if sub == 1:
            bg = sb(tg + "bg", [128, 16])
            P.op("sp", lambda e: e.dma_start(out=bg[:], in_=I["b_gate"][l]), writes=[tg + "bg"], key=tg + "bg")
        stg_tm = [sb(tg + "tm%d" % i, [128, 2048], BF16) for i in range(2)]
        stg_fm = [sb(tg + "fm%d" % i, [128, 8, 512], BF16) for i in range(2)]
        stg_dt = [sb(tg + "dt%d" % i, [128, 64]) for i in range(2)]
        psn = [2, 3, 4, 5, 6, 7]
        pscnt = [0]

        def nextps():
            i = psn[pscnt[0] % len(psn)]
            pscnt[0] += 1
            return K.ps[i], "ps%d" % i
        xi = 0
        tmi = 0
        fmi = 0
        for gi, (c0, ncg) in enumerate(groups()):
            hT = hTs[gi % 2]
            hn = tg + "h%d" % (gi % 2)
            ntok = ncg * 128
            t0 = c0 * 128
            for q in range(ncg):
                load_xT(K, tg + "x%d" % (xi % 2), src, c0 + q, xts[xi % 2], (hT, hn), q, 0, 8)
                xi += 1
            for q in range(ncg):
                c = c0 + q
                if sub == 0:
                    blocks = [(C_Z + b * 512, 512) for b in range(4)]
                else:
                    blocks = [(C_DT, 64), (C_U, 512), (C_U + 512, 512)]
                stm = stg_tm[tmi % 2]
                stn = tg + "tm%d" % (tmi % 2)
                sdt = stg_dt[tmi % 2]
                sdn = tg + "dt%d" % (tmi % 2)
                tmi += 1
                for bi, (cb, nb) in enumerate(blocks):
                    ps, pn = nextps()

                    def mm(e, ps=ps, q=q, cb=cb, nb=nb, hT=hT):
                        for k in range(8):
                            ins = e.matmul(ps[:, 0:nb], hT[:, k, q * 128:(q + 1) * 128], w[:, k, cb - col0:cb - col0 + nb],
                                           start=(k == 0), stop=(k == 7))
                        return ins
                    P.op("pe", mm, reads=[hn, tg + "w"], writes=[pn])
                    if sub == 0:
                        P.op("act", lambda e, ps=ps, bi=bi, stm=stm: e.activation(
                            out=stm[:, bi * 512:(bi + 1) * 512], in_=ps[:], func=AF.Silu),
                            reads=[pn], writes=[stn])
                    elif nb == 64:
                        P.op("dve", lambda e, ps=ps, sdt=sdt: e.tensor_copy(out=sdt[:], in_=ps[:, 0:64]),
                             reads=[pn], writes=[sdn])
                    else:
                        P.op("dve", lambda e, ps=ps, bi=bi, stm=stm: e.tensor_copy(
                            out=stm[:, (bi - 1) * 512:bi * 512], in_=ps[:]),
                            reads=[pn], writes=[stn])
                if sub == 0:
                    P.op("sp", lambda e, stm=stm, c=c: e.dma_start(out=S["sz"][c * 128:(c + 1) * 128, :], in_=stm[:]),
                         reads=[stn], writes=[("sz", c)], key="st_" + stn)
                else:
                    P.op("sp", lambda e, stm=stm, c=c: e.dma_start(out=S["u"][c * 128:(c + 1) * 128, :], in_=stm[:, 0:1024]),
                         reads=[stn], writes=[("u", c)], key="st_" + stn)
                    P.op("sp", lambda e, sdt=sdt, c=c: e.dma_start(out=S["dtraw"][c * 128:(c + 1) * 128, :], in_=sdt[:]),
                         reads=[sdn], writes=[("dtraw", c)], key="st_" + sdn)
            if sub == 0:
                fcol, nfc, dst, dname = C_XBC, 24, S["xbcT"], "xbcT"
            else:
                fcol, nfc, dst, dname = C_G, 16, S["gT"], "gT"
            for m0 in range(0, nfc, 8):
                sfm = stg_fm[fmi % 2]
                sfn = tg + "fm%d" % (fmi % 2)
                fmi += 1
                for mm_ in range(8):
                    m = m0 + mm_
                    ps, pn = nextps()
                    cb = fcol + m * 128 - col0

                    def mm(e, ps=ps, cb=cb, hT=hT, ntok=ntok):
                        for k in range(8):
                            ins = e.matmul(ps[:, 0:ntok], w[:, k, cb:cb + 128], hT[:, k, 0:ntok],
                                           start=(k == 0), stop=(k == 7))
                        return ins
                    P.op("pe", mm, reads=[hn, tg + "w"], writes=[pn])
                    if sub == 0:
                        eng = "dve" if mm_ % 2 == 0 else "act"
                        if eng == "dve":
                            P.op("dve", lambda e, ps=ps, mm_=mm_, sfm=sfm, ntok=ntok: e.tensor_copy(
                                out=sfm[:, mm_, 0:ntok], in_=ps[:, 0:ntok]), reads=[pn], writes=[sfn])
                        else:
                            P.op("act", lambda e, ps=ps, mm_=mm_, sfm=sfm, ntok=ntok: e.copy(
                                out=sfm[:, mm_, 0:ntok], in_=ps[:, 0:ntok]), reads=[pn], writes=[sfn])
                    else:
                        P.op("act", lambda e, ps=ps, mm_=mm_, sfm=sfm, ntok=ntok, m=m: e.activation(
                            out=sfm[:, mm_, 0:ntok], in_=ps[:, 0:ntok], func=AF.Sigmoid, bias=bg[:, m:m + 1], scale=1.0),
                            reads=[pn, tg + "bg"], writes=[sfn])
                P.op("sp", lambda e, sfm=sfm, m0=m0, t0=t0, ntok=ntok, dst=dst: e.dma_start(
                    out=dst[m0 * 128:(m0 + 8) * 128, t0:t0 + ntok].rearrange("(m p) t -> p m t", p=128),
                    in_=sfm[:, :, 0:ntok]),
                    reads=[sfn], writes=[(dname, gi, m0)], key="st_" + sfn)


def layer_consts(K, l, st):
    nc, P, I = K.nc, K.P, K.I
    sb = lambda n, s, d=F32: st.enter_context(nc.sbuf_tensor("sb_" + n, list(s), d))
    L = Ctx()
    L.cw = sb("cw%d" % l, [128, 5, 24])
    L.cbfm = sb("cbfm%d" % l, [128, 24])
    L.cbrow32 = sb("cbrow32_%d" % l, [1, DXBC])
    L.cbrow = sb("cbrow%d" % l, [1, DXBC], BF16)
    L.onesrow = sb("onesrow%d" % l, [1, 128], BF16)
    L.dtb = sb("dtb%d" % l, [128, 64])
    L.negA = sb("negA%d" % l, [128, 64])
    L.dskd = sb("dskd%d" % l, [128, 32, 128], BF16)
    L.dsk = sb("dsk%d" % l, [128, 32])
    P.op("sp", lambda e: e.dma_start(out=L.cw[:], in_=I["cw"][l]), writes=["cw"], key="c1")
    P.op("sp", lambda e: e.dma_start(out=L.cbfm[:], in_=I["cbfm"][l]), writes=["cbfm"], key="c2")
    P.op("sp", lambda e: e.dma_start(out=L.cbrow32[:], in_=I["conv_b"][l:l + 1, :]), writes=["cbrow32"], key="c3")
    P.op("sp", lambda e: e.dma_start(out=L.dtb[:], in_=I["dt_bias"][l].partition_broadcast(128)), writes=["dtb"], key="c4")
    P.op("sp", lambda e: e.dma_start(out=L.negA[:], in_=I["a_log"][l].partition_broadcast(128)), writes=["negA"], key="c5")
    P.op("sp", lambda e: e.dma_start(out=L.dsk[:], in_=I["d_skip"][l].partition_broadcast(128)), writes=["dsk"], key="c6")
    P.op("dve", lambda e: e.tensor_copy(out=L.cbrow[:], in_=L.cbrow32[:]), reads=["cbrow32"], writes=["cbrow"])
    P.op("dve", lambda e: e.memset(L.onesrow[:], 1.0), writes=["onesrow"])
    P.op("act", lambda e: e.activation(out=L.negA[:], in_=L.negA[:], func=AF.Exp), reads=["negA"], writes=["negA"])
    P.op("dve", lambda e: e.tensor_scalar_mul(out=L.negA[:], in0=L.negA[:], scalar1=-1.0), reads=["negA"], writes=["negA"])
    P.op("dve", lambda e: e.tensor_tensor(out=L.dskd[:], in0=K.cm[:, 0, :].unsqueeze(1).to_broadcast([128, 32, 128]),
                                          in1=L.dsk[:].unsqueeze(2).to_broadcast([128, 32, 128]), op=ALU.mult),
         reads=["cmat", "dsk"], writes=["dskd"])
    return L


def dt_prep(K, L, c, R, tag, cumT=None):
    P, S = K.P, K.S
    n = lambda x: tag + x
    P.op("sp", lambda e: e.dma_start(out=R["raw"][:], in_=S["dtraw"][c * 128:(c + 1) * 128, :]),
         writes=[n("raw")], key=n("raw"))
    P.op("dve", lambda e: e.tensor_tensor(out=R["v"][:], in0=R["raw"][:], in1=L.dtb[:], op=ALU.add),
         reads=[n("raw"), "dtb"], writes=[n("v")])
    P.op("dve", lambda e: e.scalar_tensor_tensor(out=R["t"][:], in0=R["v"][:], scalar=-1.0, in1=R["v"][:],
                                                 op0=ALU.mult, op1=ALU.max),
         reads=[n("v")], writes=[n("t")])
    P.op("act", lambda e: e.activation(out=R["t"][:], in_=R["t"][:], func=AF.Exp, scale=-1.0),
         reads=[n("t")], writes=[n("t")])
    P.op("act", lambda e: e.activation(out=R["t"][:], in_=R["t"][:], func=AF.Ln, bias=1.0, scale=1.0),
         reads=[n("t")], writes=[n("t")])
    P.op("dve", lambda e: e.scalar_tensor_tensor(out=R["dt"][:], in0=R["v"][:], scalar=0.0, in1=R["t"][:],
                                                 op0=ALU.max, op1=ALU.add),
         reads=[n("v"), n("t")], writes=[n("dt")])
    P.op("dve", lambda e: e.tensor_tensor(out=R["a"][:], in0=R["dt"][:], in1=L.negA[:], op=ALU.mult),
         reads=[n("dt"), "negA"], writes=[n("a")])
    P.op("dve", lambda e: e.tensor_copy(out=R["ahl"][:, 0, :], in_=R["a"][:]), reads=[n("a")], writes=[n("ahi")])
    P.op("dve", lambda e: e.tensor_tensor(out=R["ahl"][:, 1, :], in0=R["a"][:], in1=R["ahl"][:, 0, :], op=ALU.subtract),
         reads=[n("a"), n("ahi")], writes=[n("alo")])
    ps = K.ps[7]

    def mm(e):
        e.matmul(ps[:, 0:32], K.cm[:, 1, :], R["a"][:, 0:32], start=True, stop=True)
        e.matmul(ps[:, 32:64], K.cm[:, 2, :], R["a"][:, 32:64], start=True, stop=True)
        return e.matmul(ps[:, 64:128], K.cm[:, 3, :], R["a"][:], start=True, stop=True)
    P.op("pe", mm, reads=[n("a"), "cmat"], writes=["ps7"])
    P.op("act", lambda e: e.copy(out=R["cum"][:], in_=ps[:, 0:64]), reads=["ps7"], writes=[n("cum")])
    P.op("act", lambda e: e.copy(out=R["tot"][:], in_=ps[:, 64:128]), reads=["ps7"], writes=[n("tot")])
    P.op("act", lambda e: e.activation(out=R["dec"][:], in_=R["tot"][:], func=AF.Exp), reads=[n("tot")], writes=[n("dec")])
    P.op("dve", lambda e: e.tensor_tensor(out=R["tail"][:], in0=R["tot"][:], in1=R["cum"][:], op=ALU.subtract),
         reads=[n("tot"), n("cum")], writes=[n("tail")])
    P.op("act", lambda e: e.activation(out=R["tail"][:], in_=R["tail"][:], func=AF.Exp), reads=[n("tail")], writes=[n("tail")])
    P.op("dve", lambda e: e.tensor_tensor(out=R["tail"][:], in0=R["tail"][:], in1=R["dt"][:], op=ALU.mult),
         reads=[n("tail"), n("dt")], writes=[n("tail")])
    P.op("act", lambda e: e.activation(out=R["e"][:], in_=R["cum"][:], func=AF.Exp), reads=[n("cum")], writes=[n("e")])
    if cumT is not None:
        ct, ctn = cumT
        P.op("pe", lambda e: e.matmul(ps[0:64, 128:256], R["cum"][:], K.cm[:, 0, :], start=True, stop=True),
             reads=[n("cum"), "cmat"], writes=["ps7"])
        P.op("act", lambda e: e.copy(out=ct[:], in_=ps[0:64, 128:256]), reads=["ps7"], writes=[ctn])
        P.op("act", lambda e: e.dma_start(out=K.S["cumT"][c], in_=ct[:]), reads=[ctn], writes=[("cumT", c)], key="st_" + ctn)
    P.op("act", lambda e: e.activation(out=R["lnb"][:], in_=R["dt"][:], func=AF.Ln), reads=[n("dt")], writes=[n("lnb")])
    P.op("dve", lambda e: e.tensor_tensor(out=R["lnb"][:], in0=R["lnb"][:], in1=R["cum"][:], op=ALU.subtract),
         reads=[n("lnb"), n("cum")], writes=[n("lnb")])


def alloc_prep(K, st, tag):
    nc = K.nc
    R = {}
    for nm in ("raw", "v", "t", "dt", "a", "cum", "dec", "tail", "e", "lnb", "tot"):
        R[nm] = st.enter_context(nc.sbuf_tensor("sb_" + tag + nm, [128, 64], F32))
    R["ahl"] = st.enter_context(nc.sbuf_tensor("sb_" + tag + "ahl", [128, 2, 64], BF16))
    return R


def pass_conv_bwd(K, l, L):
    nc, P, I, S = K.nc, K.P, K.I, K.S
    import contextlib
    with contextlib.ExitStack() as st:
        sb = lambda n, s, d=F32: st.enter_context(nc.sbuf_tensor("sb_" + n, list(s), d))
        tg = "B%d_" % l
        L.diagw = sb(tg + "diagw", [128, 24, 5, 128], BF16)
        for hf_, eng_ in ((0, "dve"), (1, "dve")):
            P.op(eng_, lambda e, hf_=hf_: e.tensor_tensor(
                out=L.diagw[:, hf_ * 12:(hf_ + 1) * 12],
                in0=K.cm[:, 0, :].unsqueeze(1).unsqueeze(1).to_broadcast([128, 12, 5, 128]),
                in1=L.cw[:].rearrange("p k m -> p m k")[:, hf_ * 12:(hf_ + 1) * 12, :].unsqueeze(3).to_broadcast([128, 12, 5, 128]),
                op=ALU.mult), reads=["cmat", "cw"], writes=["diagw"], multi=True)
        raws = [sb(tg + "raw%d" % i, [128, 24, 516], BF16) for i in range(2)]
        xs = [sb(tg + "xs%d" % i, [128, 2048], BF16) for i in range(2)]
        bt = [sb(tg + "bt%d" % i, [128, 512], BF16) for i in range(2)]
        bct = [sb(tg + "bct%d" % i, [128, 1024], BF16) for i in range(2)]
        xdt = [sb(tg + "xdt%d" % i, [128, 2048], BF16) for i in range(2)]
        state = sb(tg + "state", [128, 2048])
        tmp = sb(tg + "tmp", [128, 2048])
        sbo = [sb(tg + "sbo%d" % i, [128, 2048], BF16) for i in range(2)]
        Rs = [alloc_prep(K, st, tg + "p%d" % i) for i in range(2)]
        P.op("pool", lambda e: e.memset(state[:], 0.0), writes=[tg + "state"])
        gl = groups()
        order = [0] + list(range(16, 0, -1))
        it = 0
        for gi_i, gi in enumerate(order):
            c0, ncg = gl[gi]
            ntok = ncg * 128
            t0 = c0 * 128
            raw = raws[gi_i % 2]
            rn = tg + "raw%d" % (gi_i % 2)
            left_ok = gi not in (0, 1)
            right_ok = gi not in (0, 16)
            lo = t0 - 2 if left_ok else t0
            hi = t0 + ntok + 2 if right_ok else t0 + ntok
            if not left_ok:
                P.op("pool", lambda e, raw=raw: e.memset(raw[:, :, 0:2], 0.0), writes=[rn])
            if not right_ok:
                P.op("pool", lambda e, raw=raw, ntok=ntok: e.memset(raw[:, :, ntok + 2:ntok + 4], 0.0), writes=[rn],
                     multi=left_ok is False)
            for m0 in range(0, 24, 8):
                P.op("sp", lambda e, raw=raw, lo=lo, hi=hi, t0=t0, m0=m0: e.dma_start(
                    out=raw[:, m0:m0 + 8, lo - (t0 - 2):hi - (t0 - 2)],
                    in_=S["xbcT"][m0 * 128:(m0 + 8) * 128, lo:hi].rearrange("(m p) t -> p m t", p=128)),
                    writes=[rn], key=tg + "raw%d" % (m0 // 8), multi=True)
            for q in range(ncg - 1, -1, -1):
                c = c0 + q
                o = q * 128
                sl = it % 2
                if it < NE:
                    i_ = l * NE + it
                    P.op("pool", lambda e, i_=i_: e.dma_start(out=S["wgub"][i_ * 128:(i_ + 1) * 128, :],
                                                             in_=I["wgu"].rearrange("l r n -> (l r) n")[i_ * 128:(i_ + 1) * 128, :]),
                         writes=["bg:w%d" % l], key="bg%d" % (it % 4), multi=True)
                    P.op("pool", lambda e, i_=i_: e.dma_start(out=S["wdb"][i_ * 128:(i_ + 1) * 128, :],
                                                             in_=I["wdr"].rearrange("l r n -> (l r) n")[i_ * 128:(i_ + 1) * 128, :]),
                         writes=["bg:w%d" % l], key="bg%d" % (it % 4), multi=True)
                it += 1
                xs_t, bt_t, bct_t, xdt_t, R = xs[sl], bt[sl], bct[sl], xdt[sl], Rs[sl]
                xn, bn, cn, dn = tg + "xs%d" % sl, tg + "bt%d" % sl, tg + "bct%d" % sl, tg + "xdt%d" % sl
                pt = tg + "p%d" % sl
                dt_prep(K, L, c, R, pt)
                for b4 in range(5):
                    ps = K.ps[b4 % 4]
                    pn = "ps%d" % (b4 % 4)

                    def mm(e, ps=ps, b4=b4, raw=raw, o=o):
                        for bb in range(4):
                            m = b4 * 4 + bb
                            for k in range(5):
                                e.matmul(ps[:, bb * 128:(bb + 1) * 128], raw[:, m, o + k:o + k + 128], L.diagw[:, m, k, :],
                                         start=(k == 0), stop=False)
                            ins = e.matmul(ps[:, bb * 128:(bb + 1) * 128], L.onesrow[0:1, :], L.cbrow[0:1, m * 128:(m + 1) * 128],
                                           start=False, stop=True)
                        return ins
                    P.op("pe", mm, reads=[rn, "diagw", "cbrow", "onesrow"], writes=[pn])
                    if b4 < 4:
                        P.op("act", lambda e, ps=ps, b4=b4, xs_t=xs_t: e.activation(
                            out=xs_t[:, b4 * 512:(b4 + 1) * 512], in_=ps[:], func=AF.Silu), reads=[pn], writes=[xn])
                    else:
                        P.op("act", lambda e, ps=ps, bt_t=bt_t: e.activation(out=bt_t[:], in_=ps[:], func=AF.Silu),
                             reads=[pn], writes=[bn])
                for b4 in range(2):
                    ps = K.ps[4 + b4]
                    pn = "ps%d" % (4 + b4)

                    def mm2(e, ps=ps, b4=b4, raw=raw, o=o):
                        for bb in range(4):
                            m = 16 + b4 * 4 + bb
                            for k in range(5):
                                ins = e.matmul(ps[:, bb * 128:(bb + 1) * 128], L.diagw[:, m, k, :], raw[:, m, o + k:o + k + 128],
                                               start=(k == 0), stop=(k == 4))
                        return ins
                    P.op("pe", mm2, reads=[rn, "diagw"], writes=[pn])
                    for bb in range(4):
                        m = 16 + b4 * 4 + bb
                        P.op("act", lambda e, ps=ps, bb=bb, m=m, b4=b4, bct_t=bct_t: e.activation(
                            out=bct_t[:, (b4 * 4 + bb) * 128:(b4 * 4 + bb + 1) * 128], in_=ps[:, bb * 128:(bb + 1) * 128],
                            func=AF.Silu, bias=L.cbfm[:, m:m + 1], scale=1.0), reads=[pn, "cbfm"], writes=[cn])
                P.op("sp", lambda e, xs_t=xs_t, c=c: e.dma_start(out=S["xs"][c * 128:(c + 1) * 128, :], in_=xs_t[:]),
                     reads=[xn], key="st_" + xn)
                P.op("sp", lambda e, bt_t=bt_t, c=c: e.dma_start(out=S["Bt"][c * 128:(c + 1) * 128, :], in_=bt_t[:]),
                     reads=[bn], key="st_" + bn)
                P.op("sp", lambda e, bct_t=bct_t, c=c: e.dma_start(out=S["bct"][c], in_=bct_t[:]),
                     reads=[cn], key="st_" + cn)
                so = sbo[sl]
                son = tg + "sbo%d" % sl
                P.op("act", lambda e, so=so: e.copy(out=so[:], in_=state[:]), reads=[tg + "state"], writes=[son])
                P.op("sp", lambda e, so=so, c=c: e.dma_start(out=S["sbin"][c], in_=so[:]), reads=[son], key="st_" + son)
                P.op("dve", lambda e, xs_t=xs_t, xdt_t=xdt_t, R=R: e.tensor_tensor(
                    out=xdt_t[:].rearrange("p (h d) -> p h d", h=32), in0=xs_t[:].rearrange("p (h d) -> p h d", h=32),
                    in1=R["tail"][:, 32:64].unsqueeze(2).to_broadcast([128, 32, 64]), op=ALU.mult),
                    reads=[xn, pt + "tail"], writes=[dn])
                for g in range(4):
                    ps = K.ps[6]

                    def mm3(e, ps=ps, g=g, bt_t=bt_t, xdt_t=xdt_t):
                        return e.matmul(ps[:], bt_t[:, g * 128:(g + 1) * 128], xdt_t[:, g * 512:(g + 1) * 512],
                                        start=True, stop=True)
                    P.op("pe", mm3, reads=[bn, dn], writes=["ps6"])
                    P.op("pool", lambda e, g=g, R=R: e.tensor_tensor(
                        out=tmp[:, g * 512:(g + 1) * 512].rearrange("p (h d) -> p h d", h=8),
                        in0=state[:, g * 512:(g + 1) * 512].rearrange("p (h d) -> p h d", h=8),
                        in1=R["dec"][:, 32 + g * 8:32 + (g + 1) * 8].unsqueeze(2).to_broadcast([128, 8, 64]), op=ALU.mult),
                        reads=[tg + "state", pt + "dec"], writes=[tg + "tmp"])
                    P.op("dve", lambda e, g=g, ps=ps: e.tensor_tensor(
                        out=state[:, g * 512:(g + 1) * 512], in0=ps[:], in1=tmp[:, g * 512:(g + 1) * 512], op=ALU.add),
                        reads=["ps6", tg + "tmp"], writes=[tg + "state"])


def pass_ssd_fwd(K, l, L):
    nc, P, I, S = K.nc, K.P, K.I, K.S
    import contextlib
    with contextlib.ExitStack() as st:
        sb = lambda n, s, d=F32: st.enter_context(nc.sbuf_tensor("sb_" + n, list(s), d))
        tg = "C%d_" % l
        xs = [sb(tg + "xs%d" % i, [128, 2048], BF16) for i in range(2)]
        bt = [sb(tg + "bt%d" % i, [128, 512], BF16) for i in range(2)]
        bct = [sb(tg + "bct%d" % i, [128, 1024], BF16) for i in range(2)]
        sbi = [sb(tg + "sbi%d" % i, [128, 2048], BF16) for i in range(2)]
        szt = [sb(tg + "sz%d" % i, [128, 2048], BF16) for i in range(2)]
        Rs = [alloc_prep(K, st, tg + "p%d" % i) for i in range(2)]
        Wd = [[sb(tg + "W%d_%d" % (d, i), [128, 8, 128], BF16) for i in range(2)] for d in range(2)]
        Wsum = [sb(tg + "Ws%d" % i, [128, 8, 128], BF16) for i in range(2)]
        Mt = [sb(tg + "M%d" % i, [128, 8, 128], BF16) for i in range(2)]
        t1 = [sb(tg + "t1_%d" % i, [128, 512]) for i in range(2)]
        t2 = [sb(tg + "t2_%d" % i, [128, 512]) for i in range(2)]
        yz = [sb(tg + "yz%d" % i, [128, 2048]) for i in range(2)]
        junk = sb(tg + "junk", [128, 512], BF16)
        yn = sb(tg + "yn", [128, 2048], BF16)
        ss = [sb(tg + "ss%d" % i, [128, 4]) for i in range(2)]
        xdtf = [sb(tg + "xdtf%d" % i, [128, 2048], BF16) for i in range(2)]
        state = sb(tg + "state", [128, 2048])
        sfb = sb(tg + "sfb", [128, 2048], BF16)
        tmp = [sb(tg + "tmp%d" % i, [128, 512]) for i in range(2)]
        yaT = [sb(tg + "yaT%d" % i, [128, 16, 128], BF16) for i in range(2)]
        cumTs = [sb(tg + "cumT%d" % i, [64, 128]) for i in range(2)]
        rowbc = [sb(tg + "rb%d" % i, [128, 2, 8, 128]) for i in range(4)]
        P.op("pool", lambda e: e.memset(state[:], 0.0), writes=[tg + "state"])
        P.op("pool", lambda e: e.memset(sfb[:], 0.0), writes=[tg + "sfb"])
        ps_cb = [K.ps[0], K.ps[0]]
        ps_y = [K.ps[3], K.ps[4]]
        ps_of, ps_ob, ps_s = K.ps[5], K.ps[6], K.ps[7]

        def names(c):
            sl = c % 2
            return dict(sl=sl, xs=xs[sl], bt=bt[sl], bct=bct[sl], sbi=sbi[sl], sz=szt[sl], R=Rs[sl],
                        xn=tg + "xs%d" % sl, bn=tg + "bt%d" % sl, cn=tg + "bct%d" % sl, sn=tg + "sbi%d" % sl,
                        zn=tg + "sz%d" % sl, pt=tg + "p%d" % sl)

        def load(c):
            N = names(c)
            P.op("sp", lambda e: e.dma_start(out=N["xs"][:], in_=S["xs"][c * 128:(c + 1) * 128, :]), writes=[N["xn"]], key=N["xn"])
            P.op("sp", lambda e: e.dma_start(out=N["bt"][:], in_=S["Bt"][c * 128:(c + 1) * 128, :]), writes=[N["bn"]], key=N["bn"])
            P.op("sp", lambda e: e.dma_start(out=N["bct"][:], in_=S["bct"][c]), writes=[N["cn"]], key=N["cn"])
            P.op("sp", lambda e: e.dma_start(out=N["sbi"][:], in_=S["sbin"][c]), writes=[N["sn"]], key=N["sn"])
            P.op("sp", lambda e: e.dma_start(out=N["sz"][:], in_=S["sz"][c * 128:(c + 1) * 128, :]), writes=[N["zn"]], key=N["zn"])
            dt_prep(K, L, c, N["R"], N["pt"], cumT=(cumTs[c % 2], tg + "cumT%d" % (c % 2)))

        def rb_load(c, g):
            ri = (c * 4 + g) % 4
            rb, rbn = rowbc[ri], tg + "rb%d" % ri
            flat = S["cumT"][c].rearrange("h t -> (h t)")
            for d in range(2):
                o_ = (d * 32 + g * 8) * 128
                P.op("sp", lambda e, d=d, o_=o_: e.dma_start(out=rb[:, d].rearrange("p h t -> p (h t)"),
                                                            in_=flat[o_:o_ + 1024].partition_broadcast(128)),
                     reads=[("cumT", c)], writes=[rbn], key=rbn + "_%d" % d, multi=True)

        def stage_a(c, g):
            N = names(c)
            R, pt = N["R"], N["pt"]
            wi = (c * 4 + g) % 2
            ri = (c * 4 + g) % 4
            rb, rbn = rowbc[ri], tg + "rb%d" % ri
            for d in range(2):
                P.op("dve", lambda e, d=d: e.tensor_tensor(out=rb[:, d], in0=rb[:, d],
                                                          in1=K.cm[:, 4 + d, :].unsqueeze(1).to_broadcast([128, 8, 128]), op=ALU.add),
                     reads=[rbn, "cmat"], writes=[rbn])

        def stage_a2(c, g):
            N = names(c)
            R, pt = N["R"], N["pt"]
            wi = (c * 4 + g) % 2
            ri = (c * 4 + g) % 4
            rb, rbn = rowbc[ri], tg + "rb%d" % ri
            for d in range(2):
                W = Wd[d][wi]
                wn = tg + "W%d_%d" % (d, wi)
                for hh in range(8):
                    col = d * 32 + g * 8 + hh
                    P.op("act", lambda e, d=d, hh=hh, col=col, W=W: e.activation(
                        out=W[:, hh, :], in_=rb[:, d, hh, :], func=AF.Exp, bias=R["lnb"][:, col:col + 1], scale=1.0),
                        reads=[rbn, pt + "lnb"], writes=[wn], multi=True)

        def chunk_head(c):
            N = names(c)
            bct_t = N["bct"]

            pcb = K.ps[c % 2]

            def mmcb(e):
                for g in range(4):
                    ins = e.matmul(pcb[:, g * 128:(g + 1) * 128], bct_t[:, g * 128:(g + 1) * 128],
                                   bct_t[:, (4 + g) * 128:(5 + g) * 128], start=True, stop=True)
                return ins
            P.op("pe", mmcb, reads=[N["cn"]], writes=["ps%d" % (c % 2)])
            xd = xdtf[c % 2]
            P.op("pool", lambda e: e.tensor_tensor(
                out=xd[:].rearrange("p (h d) -> p h d", h=32), in0=N["xs"][:].rearrange("p (h d) -> p h d", h=32),
                in1=N["R"]["tail"][:, 0:32].unsqueeze(2).to_broadcast([128, 32, 64]), op=ALU.mult),
                reads=[N["xn"], N["pt"] + "tail"], writes=[tg + "xdtf%d" % (c % 2)])

        def stage_b(c, g, part):
            N = names(c)
            R, pt, xs_t, bt_t, bct_t, sbi_t, sz_t = N["R"], N["pt"], N["xs"], N["bt"], N["bct"], N["sbi"], N["sz"]
            xn, bn, cn, sn, zn = N["xn"], N["bn"], N["cn"], N["sn"], N["zn"]
            wi = (c * 4 + g) % 2
            M, mn = Mt[wi], tg + "M%d" % wi
            Ws, wsn = Wsum[wi], tg + "Ws%d" % wi
            py, pyn = ps_y[wi], "ps%d" % (3 + wi)
            a1, n1 = t1[wi], tg + "t1_%d" % wi
            a2, n2 = t2[wi], tg + "t2_%d" % wi
            yz_t, yzn = yz[c % 2], tg + "yz%d" % (c % 2)
            ss_t, ssn = ss[c % 2], tg + "ss%d" % (c % 2)
            xd, xdn = xdtf[c % 2], tg + "xdtf%d" % (c % 2)
            tm, tmn = tmp[wi], tg + "tmp%d" % wi
            if part == 2:
              P.op("pool", lambda e: e.tensor_tensor(out=Ws[:], in0=Wd[0][wi][:], in1=Wd[1][wi][:], op=ALU.add),
                 reads=[tg + "W0_%d" % wi, tg + "W1_%d" % wi], writes=[wsn])
              P.op("dve", lambda e: e.tensor_tensor(
                out=M[:], in0=Ws[:], in1=K.ps[c % 2][:, g * 128:(g + 1) * 128].unsqueeze(1).to_broadcast([128, 8, 128]),
                op=ALU.mult), reads=[wsn, "ps%d" % (c % 2)], writes=[mn])

            def mmy(e):
                for hh in range(8):
                    h = g * 8 + hh
                    e.matmul(py[:, hh * 64:(hh + 1) * 64], M[:, hh, :], xs_t[:, h * 64:(h + 1) * 64], start=True, stop=False)
                    ins = e.matmul(py[:, hh * 64:(hh + 1) * 64], L.dskd[:, h, :], xs_t[:, h * 64:(h + 1) * 64], start=False, stop=True)
                return ins
            if part == 3:
                P.op("pe", mmy, reads=[mn, xn, "dskd"], writes=[pyn])
            if part != 1:
                pass
            if part == 1:
              if True:
                  P.op("pe", lambda e: e.matmul(ps_of[:], bct_t[:, (4 + g) * 128:(5 + g) * 128], sfb[:, g * 512:(g + 1) * 512], start=True, stop=True),
                       reads=[cn, tg + "sfb"], writes=["ps5"])
                  P.op("pe", lambda e: e.matmul(ps_ob[:], bct_t[:, (4 + g) * 128:(5 + g) * 128], sbi_t[:, g * 512:(g + 1) * 512], start=True, stop=True),
                       reads=[cn, sn], writes=["ps6"])
                  P.op("pe", lambda e: e.matmul(ps_s[:], bt_t[:, g * 128:(g + 1) * 128], xd[:, g * 512:(g + 1) * 512], start=True, stop=True),
                       reads=[bn, xdn], writes=["ps7"])
            if part == 4:
                P.op("dve", lambda e: e.tensor_tensor(
                    out=a1[:].rearrange("p (h d) -> p h d", h=8), in0=ps_of[:].rearrange("p (h d) -> p h d", h=8),
                    in1=R["e"][:, g * 8:(g + 1) * 8].unsqueeze(2).to_broadcast([128, 8, 64]), op=ALU.mult),
                    reads=["ps5", pt + "e"], writes=[n1])
                P.op("dve", lambda e: e.tensor_tensor(
                    out=a2[:].rearrange("p (h d) -> p h d", h=8), in0=ps_ob[:].rearrange("p (h d) -> p h d", h=8),
                    in1=R["e"][:, 32 + g * 8:32 + (g + 1) * 8].unsqueeze(2).to_broadcast([128, 8, 64]), op=ALU.mult),
                    reads=["ps6", pt + "e"], writes=[n2])
                P.op("pool", lambda e: e.tensor_tensor(out=a1[:], in0=a1[:], in1=a2[:], op=ALU.add), reads=[n1, n2], writes=[n1])
                P.op("pool", lambda e: e.tensor_tensor(
                    out=tm[:].rearrange("p (h d) -> p h d", h=8), in0=state[:, g * 512:(g + 1) * 512].rearrange("p (h d) -> p h d", h=8),
                    in1=R["dec"][:, g * 8:(g + 1) * 8].unsqueeze(2).to_broadcast([128, 8, 64]), op=ALU.mult),
                    reads=[tg + "state", pt + "dec"], writes=[tmn])
                P.op("dve", lambda e: e.tensor_tensor(out=state[:, g * 512:(g + 1) * 512], in0=ps_s[:], in1=tm[:], op=ALU.add),
                     reads=["ps7", tmn], writes=[tg + "state"])
                P.op("act", lambda e: e.copy(out=sfb[:, g * 512:(g + 1) * 512], in_=state[:, g * 512:(g + 1) * 512]),
                     reads=[tg + "state"], writes=[tg + "sfb"])
            if part == 5:
                P.op("dve", lambda e: e.tensor_tensor(out=a1[:], in0=py[:], in1=a1[:], op=ALU.add), reads=[pyn, n1], writes=[n1])
                P.op("pool", lambda e: e.tensor_tensor(out=yz_t[:, g * 512:(g + 1) * 512], in0=a1[:], in1=sz_t[:, g * 512:(g + 1) * 512], op=ALU.mult),
                     reads=[n1, zn], writes=[yzn], multi=True)
                P.op("act", lambda e: e.activation(out=junk[:], in_=yz_t[:, g * 512:(g + 1) * 512],
                                                   func=AF.Square, accum_out=ss_t[:, g:g + 1]), reads=[yzn], writes=[ssn], multi=True)

        def chunk_tail(c):
            sl = c % 2
            yz_t, yzn = yz[sl], tg + "yz%d" % sl
            ss_t, ssn = ss[sl], tg + "ss%d" % sl
            s1 = tg + "ss1_%d" % sl
            P.op("dve", lambda e: e.tensor_reduce(out=ss_t[:, 0:1], in_=ss_t[:, 0:4], axis=AX.X, op=ALU.add), reads=[ssn], writes=[s1])
            P.op("dve", lambda e: e.tensor_scalar(out=ss_t[:, 0:1], in0=ss_t[:, 0:1], scalar1=1.0 / DI, scalar2=EPS, op0=ALU.mult, op1=ALU.add),
                 reads=[s1], writes=[s1])
            P.op("act", lambda e: e.activation(out=ss_t[:, 0:1], in_=ss_t[:, 0:1], func=AF.Sqrt), reads=[s1], writes=[s1])
            P.op("dve", lambda e: e.reciprocal(out=ss_t[:, 0:1], in_=ss_t[:, 0:1]), reads=[s1], writes=[s1])
            P.op("act", lambda e: e.activation(out=yn[:], in_=yz_t[:], func=AF.Copy, scale=ss_t[:, 0:1]),
                 reads=[yzn, s1], writes=[tg + "yn", ssn, yzn])
            ya = yaT[sl]
            yan = tg + "yaT%d" % sl
            for f4 in range(4):
                pt_, ptn = ps_y[f4 % 2], "ps%d" % (3 + f4 % 2)

                def mmt(e, f4=f4, pt_=pt_):
                    for ff in range(4):
                        f = f4 * 4 + ff
                        ins = e.matmul(pt_[:, ff * 128:(ff + 1) * 128], yn[:, f * 128:(f + 1) * 128], K.cb[:, 0, :], start=True, stop=True)
                    return ins
                P.op("pe", mmt, reads=[tg + "yn", "cmatb"], writes=[ptn])
                if f4 % 2 == 0:
                    P.op("dve", lambda e, f4=f4, pt_=pt_: e.tensor_copy(out=ya[:, f4 * 4:(f4 + 1) * 4, :], in_=pt_[:].rearrange("p (f t) -> p f t", f=4)),
                         reads=[ptn], writes=[yan], multi=True)
                else:
                    P.op("act", lambda e, f4=f4, pt_=pt_: e.copy(out=ya[:, f4 * 4:(f4 + 1) * 4, :], in_=pt_[:].rearrange("p (f t) -> p f t", f=4)),
                         reads=[ptn], writes=[yan], multi=True)
            P.op("sp", lambda e: e.dma_start(out=S["yaT"][:, c * 128:(c + 1) * 128].rearrange("(f p) t -> p f t", p=128), in_=ya[:]),
                 reads=[yan], key="st_" + yan)

        seq = [(c, g) for c in range(NCH) for g in range(4)]
        load(0)
        chunk_head(0)
        for n in range(3):
            rb_load(*seq[n])
        for n in range(2):
            stage_a(*seq[n])
            stage_a2(*seq[n])
        for n, (c, g) in enumerate(seq):
            if g == 0 and c + 1 < NCH:
                load(c + 1)
            if n + 3 < len(seq):
                rb_load(*seq[n + 3])
            stage_b(c, g, 1)
            stage_b(c, g, 2)
            if n + 2 < len(seq):
                c2, g2 = seq[n + 2]
                if g2 == 0:
                    chunk_head(c2)
                stage_a(c2, g2)
                stage_a2(c2, g2)
            stage_b(c, g, 3)
            stage_b(c, g, 4)
            stage_b(c, g, 5)
            if g == 3:
                chunk_tail(c)


POOLK = (2, 4, 8, 16)
POOL_DMIN = {2: -1, 4: -1, 8: -2, 16: -4}
POOL_DMAX = {2: 3, 4: 4, 8: 5, 16: 7}


def pool_idx(k, d):
    base = 0
    for kk in POOLK:
        if kk == k:
            return base + d - POOL_DMIN[k]
        base += POOL_DMAX[kk] - POOL_DMIN[kk] + 1
    raise ValueError


def pass_pool(K, l):
    nc, P, I, S = K.nc, K.P, K.I, K.S
    import contextlib
    with contextlib.ExitStack() as st:
        sb = lambda n, s, d=F32: st.enter_context(nc.sbuf_tensor("sb_" + n, list(s), d))
        tg = "D1_%d_" % l
        pP = sb(tg + "pP", [128, 31, 512], BF16)
        pC = sb(tg + "pC", [128, 8, 256], BF16)
        pw = sb(tg + "pw", [128, 8, 256], BF16)
        psc = sb(tg + "psc", [128, 8])
        ut = [sb(tg + "ut%d" % i, [128, 12, 1024], BF16) for i in range(2)]
        pmT = sb(tg + "pmT", [128, 8, 512], BF16)
        ypT = [sb(tg + "ypT%d" % i, [128, 8, 512], BF16) for i in range(2)]
        P.op("sp", lambda e: e.dma_start(out=pC[:], in_=I["poolC"]), writes=[tg + "pC"], key="pc1")
        P.op("pool", lambda e: e.dma_start(out=pw[:], in_=I["pool_w"][l].rearrange("g (kc p) d -> p (g kc) d", p=128)),
             writes=[tg + "pw"], key="pc2")
        P.op("sp", lambda e: e.dma_start(out=psc[:], in_=I["pscfm"][l]), writes=[tg + "psc"], key="pc3")
        cur_type = [None]
        for gi, (c0, ncg) in enumerate(groups()):
            ntok = ncg * 128
            t0 = c0 * 128
            u_t = ut[gi % 2]
            un = tg + "ut%d" % (gi % 2)
            if gi == 0:
                tiles = {0: 0, 1: 1}
                for sl_, c in tiles.items():
                    P.op("sp", lambda e, u_t=u_t, sl_=sl_, c=c: e.dma_start(out=u_t[:, sl_, :], in_=S["u"][c * 128:(c + 1) * 128, :]),
                         writes=[un], key="ut%d" % (sl_ % 4), multi=True)
            else:
                ptype = 0 if gi == 1 else (2 if gi == 16 else 1)
                if cur_type[0] != ptype:
                    cur_type[0] = ptype
                    for pi in range(4):
                        a, b = pi * 8, min(31, pi * 8 + 8)
                        P.op("sp", lambda e, a=a, b=b, ptype=ptype: e.dma_start(out=pP[:, a:b, :], in_=I["poolP"][ptype, :, a:b, :]),
                             writes=[tg + "pP"], key="pP%d" % pi, multi=True)
                lt0 = (gi - 1) * 4
                for d in range(-4, 8):
                    lt = lt0 + d
                    if 0 <= lt < 64:
                        c = 2 + lt
                        P.op("sp", lambda e, u_t=u_t, d=d, c=c: e.dma_start(out=u_t[:, d + 4, :], in_=S["u"][c * 128:(c + 1) * 128, :]),
                             writes=[un], key="ut%d" % ((d + 4) % 4), multi=True)
            for kg, k in enumerate(POOLK):
                for cc in range(2):
                    ps = K.ps[(kg * 2 + cc) % 4]
                    pn = "ps%d" % ((kg * 2 + cc) % 4)
                    ch0 = kg * 256 + cc * 128
                    if gi == 0:
                        mats = [(sl_, pC[:, kg * 2 + sl_, :]) for sl_ in range(2)]
                    else:
                        mats = []
                        for d in range(POOL_DMIN[k], POOL_DMAX[k] + 1):
                            if 0 <= lt0 + d < 64:
                                mats.append((d + 4, pP[:, pool_idx(k, d), :]))

                    def mm(e, ps=ps, mats=mats, u_t=u_t, ch0=ch0, ntok=ntok):
                        for i, (sl_, pm) in enumerate(mats):
                            ins = e.matmul(ps[:, 0:ntok], u_t[:, sl_, ch0:ch0 + 128], pm[:, 0:ntok],
                                           start=(i == 0), stop=(i == len(mats) - 1))
                        return ins
                    P.op("pe", mm, reads=[un, tg + "pP", tg + "pC"], writes=[pn])
                    if cc == 0:
                        P.op("dve", lambda e, ps=ps, kg=kg, cc=cc, ntok=ntok: e.tensor_copy(
                            out=pmT[:, kg * 2 + cc, 0:ntok], in_=ps[:, 0:ntok]), reads=[pn], writes=[tg + "pmT"], multi=True)
                    else:
                        P.op("act", lambda e, ps=ps, kg=kg, cc=cc, ntok=ntok: e.copy(
                            out=pmT[:, kg * 2 + cc, 0:ntok], in_=ps[:, 0:ntok]), reads=[pn], writes=[tg + "pmT"], multi=True)
            yp = ypT[gi % 2]
            ypn = tg + "ypT%d" % (gi % 2)
            for kg in range(4):
                for dc in range(2):
                    ps = K.ps[4 + (kg * 2 + dc) % 4]
                    pn = "ps%d" % (4 + (kg * 2 + dc) % 4)

                    def mm2(e, ps=ps, kg=kg, dc=dc, ntok=ntok):
                        for kc in range(2):
                            ins = e.matmul(ps[:, 0:ntok], pw[:, kg * 2 + kc, dc * 128:(dc + 1) * 128], pmT[:, kg * 2 + kc, 0:ntok],
                                           start=(kc == 0), stop=(kc == 1))
                        return ins
                    P.op("pe", mm2, reads=[tg + "pw", tg + "pmT"], writes=[pn])
                    j = kg * 2 + dc
                    if dc == 0:
                        P.op("act", lambda e, ps=ps, j=j, yp=yp, ntok=ntok: e.activation(
                            out=yp[:, j, 0:ntok], in_=ps[:, 0:ntok], func=AF.Copy, scale=psc[:, j:j + 1]),
                            reads=[pn, tg + "psc"], writes=[ypn], multi=True)
                    else:
                        P.op("dve", lambda e, ps=ps, j=j, yp=yp, ntok=ntok: e.tensor_scalar_mul(
                            out=yp[:, j, 0:ntok], in0=ps[:, 0:ntok], scalar1=psc[:, j:j + 1]),
                            reads=[pn, tg + "psc"], writes=[ypn], multi=True)
            P.op("sp", lambda e, yp=yp, t0=t0, ntok=ntok: e.dma_start(
                out=S["ypT"][:, t0:t0 + ntok].rearrange("(k p) t -> p k t", p=128), in_=yp[:, :, 0:ntok]),
                reads=[ypn, tg + "pmT"], key="st_" + ypn)


def layer_norm_tile(K, tg, r, x1, st6, mv, gbc, bbc, deps_r):
    P = K.P
    rn, xn = deps_r
    for hf in range(2):
        P.op("dve", lambda e, hf=hf: e.bn_stats(out=st6[:, hf * 6:(hf + 1) * 6], in_=r[:, hf * 512:(hf + 1) * 512]),
             reads=[rn], writes=[tg + "st6"], multi=True)
    P.op("dve", lambda e: e.bn_aggr(out=mv[:, 0:2], in_=st6[:]), reads=[tg + "st6"], writes=[tg + "mv"])
    P.op("dve", lambda e: e.tensor_scalar_add(out=mv[:, 1:2], in0=mv[:, 1:2], scalar1=EPS), reads=[tg + "mv"], writes=[tg + "mv"])
    P.op("act", lambda e: e.activation(out=mv[:, 1:2], in_=mv[:, 1:2], func=AF.Sqrt), reads=[tg + "mv"], writes=[tg + "mv"])
    P.op("dve", lambda e: e.reciprocal(out=mv[:, 1:2], in_=mv[:, 1:2]), reads=[tg + "mv"], writes=[tg + "mv"])
    P.op("dve", lambda e: e.tensor_scalar(out=r[:], in0=r[:], scalar1=mv[:, 0:1], scalar2=mv[:, 1:2],
                                          op0=ALU.subtract, op1=ALU.mult), reads=[rn, tg + "mv"], writes=[rn, tg + "st6"])
    P.op("pool", lambda e: e.tensor_tensor(out=r[:], in0=r[:], in1=gbc[:], op=ALU.mult), reads=[rn, tg + "gbc"], writes=[rn])
    P.op("pool", lambda e: e.tensor_tensor(out=x1[:], in0=r[:], in1=bbc[:], op=ALU.add), reads=[rn, tg + "bbc"], writes=[xn])


def pass_merge(K, l, src):
    nc, P, I, S = K.nc, K.P, K.I, K.S
    import contextlib
    with contextlib.ExitStack() as st:
        sb = lambda n, s, d=F32: st.enter_context(nc.sbuf_tensor("sb_" + n, list(s), d))
        tg = "D2_%d_" % l
        Wa = sb(tg + "Wa", [128, 16, 1024], BF16)
        Wb = sb(tg + "Wb", [128, 8, 1024], BF16)
        Wo = sb(tg + "Wo", [128, 8, 1024], BF16)
        gfm = sb(tg + "gfm", [128, 16])
        wr = sb(tg + "wr", [128, 8, 36])
        brow = sb(tg + "brow", [128, 36])
        for i in range(4):
            P.op("pool", lambda e, i=i: e.dma_start(out=Wa[:, i * 4:(i + 1) * 4, :],
                                                    in_=I["w_branch_a"][l, i * 512:(i + 1) * 512, :].rearrange("(k p) n -> p k n", p=128)),
                 writes=[tg + "Wa"], key="wld%d" % i, multi=True)
        for i in range(2):
            P.op("pool", lambda e, i=i: e.dma_start(out=Wb[:, i * 4:(i + 1) * 4, :],
                                                    in_=I["w_branch_b"][l, i * 512:(i + 1) * 512, :].rearrange("(k p) n -> p k n", p=128)),
                 writes=[tg + "Wb"], key="wld%d" % i, multi=True)
            P.op("pool", lambda e, i=i: e.dma_start(out=Wo[:, i * 4:(i + 1) * 4, :],
                                                    in_=I["w_out"][l, i * 512:(i + 1) * 512, :].rearrange("(k p) n -> p k n", p=128)),
                 writes=[tg + "Wo"], key="wld%d" % (2 + i), multi=True)
        P.op("sp", lambda e: e.dma_start(out=gfm[:], in_=I["gnfm"][l]), writes=[tg + "gfm"], key="pc1")
        P.op("sp", lambda e: e.dma_start(out=wr[:], in_=I["wr"][l]), writes=[tg + "wr"], key="pc2")
        P.op("sp", lambda e: e.dma_start(out=brow[:], in_=I["br"][l].partition_broadcast(128)), writes=[tg + "brow"], key="pc3")
        for k in range(16):
            P.op("dve" if k % 2 == 0 else "pool", lambda e, k=k: e.tensor_scalar_mul(out=Wa[:, k, :], in0=Wa[:, k, :], scalar1=gfm[:, k:k + 1]),
                 reads=[tg + "Wa", tg + "gfm"], writes=[tg + "Wa2"], multi=True)
        gate_bc = [sb(tg + "gate%d" % s_, [128, 1024]) for s_ in range(2)]
        gbc = sb(tg + "gbc", [128, 1024])
        bbc = sb(tg + "bbc", [128, 1024])
        for s_ in range(2):
            bcast_rows(K, l, gate_bc[s_], tg + "gate%d" % s_, 2 * 1024, s_)
        P.op("sp", lambda e: e.dma_start(out=gbc[:], in_=I["ln1_g"][l].partition_broadcast(128)), writes=[tg + "gbc"], key="pc4")
        P.op("sp", lambda e: e.dma_start(out=bbc[:], in_=I["ln1_b"][l].partition_broadcast(128)), writes=[tg + "bbc"], key="pc5")
        yaT = sb(tg + "yaT", [128, 16, 512], BF16)
        gT = sb(tg + "gT", [128, 16, 512], BF16)
        ypT = sb(tg + "ypT", [128, 8, 512], BF16)
        mT = sb(tg + "mT", [128, 8, 512], BF16)
        t1 = [sb(tg + "t1_%d" % i, [128, 512]) for i in range(2)]
        t2 = [sb(tg + "t2_%d" % i, [128, 512]) for i in range(2)]
        xt = [sb(tg + "x%d" % i, [128, 1024]) for i in range(2)]
        rt = [sb(tg + "r%d" % i, [128, 1024]) for i in range(2)]
        x1t = [sb(tg + "x1_%d" % i, [128, 1024]) for i in range(2)]
        st6 = sb(tg + "st6", [128, 12])
        mv = sb(tg + "mv", [128, 2])
        h2b = [sb(tg + "h2b%d" % i, [128, 8, 128], BF16) for i in range(2)]
        h2f = [sb(tg + "h2f%d" % i, [128, 8, 128]) for i in range(2)]
        rsm = {nm: sb(tg + "rs_" + nm, [128, w_]) for nm, w_ in
               (("lg", 36), ("m4", 1), ("oh", 4), ("ex", 4), ("se", 1), ("tmp32", 32), ("le8", 8), ("m1", 1), ("mk1", 8),
                ("le2", 8), ("m2", 1), ("mk2", 8), ("w1", 1), ("w2", 1), ("g8", 8), ("s32", 32), ("ssel", 32), ("jk", 32), ("mk12", 8))}
        rsm["s32b"] = sb(tg + "rs_s32b", [128, 32], BF16)
        P.op("pool", lambda e: e.memset(K.pref[:], 0.0), writes=["pref"])
        it = 0
        for gi, (c0, ncg) in enumerate(groups()):
            ntok = ncg * 128
            t0 = c0 * 128
            P.op("sp", lambda e, t0=t0, ntok=ntok: e.dma_start(
                out=yaT[:, :, 0:ntok], in_=S["yaT"][:, t0:t0 + ntok].rearrange("(k p) t -> p k t", p=128)),
                writes=[tg + "yaT"], key=tg + "yaT")
            P.op("sp", lambda e, t0=t0, ntok=ntok: e.dma_start(
                out=gT[:, :, 0:ntok], in_=S["gT"][:, t0:t0 + ntok].rearrange("(k p) t -> p k t", p=128)),
                writes=[tg + "gT"], key=tg + "gT")
            P.op("sp", lambda e, t0=t0, ntok=ntok: e.dma_start(
                out=ypT[:, :, 0:ntok], in_=S["ypT"][:, t0:t0 + ntok].rearrange("(k p) t -> p k t", p=128)),
                writes=[tg + "ypT"], key=tg + "ypT")
            for oc in range(8):
                psA, pnA = K.ps[(oc % 2) * 2], "ps%d" % ((oc % 2) * 2)
                psB, pnB = K.ps[(oc % 2) * 2 + 1], "ps%d" % ((oc % 2) * 2 + 1)

                def mma(e, psA=psA, oc=oc, ntok=ntok):
                    for k in range(16):
                        ins = e.matmul(psA[:, 0:ntok], Wa[:, k, oc * 128:(oc + 1) * 128], yaT[:, k, 0:ntok], start=(k == 0), stop=(k == 15))
                    return ins
                P.op("pe", mma, reads=[tg + "Wa2", tg + "yaT"], writes=[pnA])

                def mmb(e, psB=psB, oc=oc, ntok=ntok):
                    for k in range(8):
                        ins = e.matmul(psB[:, 0:ntok], Wb[:, k, oc * 128:(oc + 1) * 128], ypT[:, k, 0:ntok], start=(k == 0), stop=(k == 7))
                    return ins
                P.op("pe", mmb, reads=[tg + "Wb", tg + "ypT"], writes=[pnB])
                a1, a2 = t1[oc % 2], t2[oc % 2]
                n1, n2 = tg + "t1_%d" % (oc % 2), tg + "t2_%d" % (oc % 2)
                P.op("dve", lambda e, psA=psA, oc=oc, a1=a1, ntok=ntok: e.tensor_tensor(
                    out=a1[:, 0:ntok], in0=psA[:, 0:ntok], in1=gT[:, oc, 0:ntok], op=ALU.mult), reads=[pnA, tg + "gT"], writes=[n1])
                P.op("dve", lambda e, psB=psB, oc=oc, a2=a2, ntok=ntok: e.tensor_tensor(
                    out=a2[:, 0:ntok], in0=psB[:, 0:ntok], in1=gT[:, 8 + oc, 0:ntok], op=ALU.mult), reads=[pnB, tg + "gT"], writes=[n2])
                P.op("pool", lambda e, oc=oc, a1=a1, a2=a2, ntok=ntok: e.tensor_tensor(
                    out=mT[:, oc, 0:ntok], in0=a1[:, 0:ntok], in1=a2[:, 0:ntok], op=ALU.add), reads=[n1, n2], writes=[tg + "mT"], multi=True)
            for q in range(ncg):
                c = c0 + q
                s_ = chunk_stream(c)
                sl = it % 2
                it += 1
                x_t, r_t, x1_t = xt[sl], rt[sl], x1t[sl]
                xn, rn, x1n = tg + "x%d" % sl, tg + "r%d" % sl, tg + "x1_%d" % sl
                P.op("sp", lambda e, x_t=x_t, c=c: e.dma_start(out=x_t[:], in_=src[c * 128:(c + 1) * 128, :]), writes=[xn], key=xn)
                for hf in range(2):
                    ps, pn = K.ps[4 + hf], "ps%d" % (4 + hf)

                    def mmo(e, ps=ps, hf=hf, q=q):
                        for k in range(8):
                            ins = e.matmul(ps[:], mT[:, k, q * 128:(q + 1) * 128], Wo[:, k, hf * 512:(hf + 1) * 512], start=(k == 0), stop=(k == 7))
                        return ins
                    P.op("pe", mmo, reads=[tg + "mT", tg + "Wo"], writes=[pn])
                    P.op("dve", lambda e, ps=ps, hf=hf, r_t=r_t, s_=s_: e.tensor_tensor(
                        out=r_t[:, hf * 512:(hf + 1) * 512], in0=ps[:], in1=gate_bc[s_][:, hf * 512:(hf + 1) * 512], op=ALU.mult),
                        reads=[pn, tg + "gate%d" % s_], writes=[rn], multi=True)
                P.op("dve", lambda e, x_t=x_t, r_t=r_t: e.scalar_tensor_tensor(
                    out=r_t[:], in0=x_t[:], scalar=ALPHA, in1=r_t[:], op0=ALU.mult, op1=ALU.add), reads=[xn, rn], writes=[rn])
                layer_norm_tile(K, tg, r_t, x1_t, st6, mv, gbc, bbc, (rn, x1n))
                P.op("sp", lambda e, x1_t=x1_t, c=c: e.dma_start(out=S["x1"][c * 128:(c + 1) * 128, :], in_=x1_t[:]),
                     reads=[x1n], key="st_" + x1n)
                hb, hf32 = h2b[sl], h2f[sl]
                hbn, hfn = tg + "h2b%d" % sl, tg + "h2f%d" % sl
                for hf in range(2):
                    ps, pn = K.ps[6 + hf], "ps%d" % (6 + hf)

                    def tr(e, ps=ps, hf=hf, x1_t=x1_t):
                        for jj in range(4):
                            j = hf * 4 + jj
                            ins = e.matmul(ps[:, jj * 128:(jj + 1) * 128], x1_t[:, j * 128:(j + 1) * 128], K.ident[:], start=True, stop=True)
                        return ins
                    P.op("pe", tr, reads=[x1n, "ident"], writes=[pn])
                    for jj in range(4):
                        j = hf * 4 + jj
                        sc = K.modfm[:, 32 + j, s_:s_ + 1]
                        sh = K.modfm[:, 24 + j, s_:s_ + 1]
                        P.op("dve", lambda e, ps=ps, jj=jj, j=j, hf32=hf32, sc=sc, sh=sh: e.tensor_scalar(
                            out=hf32[:, j, :], in0=ps[:, jj * 128:(jj + 1) * 128], scalar1=sc, scalar2=sh, op0=ALU.mult, op1=ALU.add),
                            reads=[pn, "modfm"], writes=[hfn], multi=True)
                    P.op("act", lambda e, hf=hf, hb=hb, hf32=hf32: e.copy(out=hb[:, hf * 4:(hf + 1) * 4, :], in_=hf32[:, hf * 4:(hf + 1) * 4, :]),
                         reads=[hfn], writes=[hbn], multi=True)
                P.op("sp", lambda e, hb=hb, c=c: e.dma_start(
                    out=S["h2T"][:, c * 128:(c + 1) * 128].rearrange("(k p) t -> p k t", p=128), in_=hb[:]),
                    reads=[hbn], key="st_" + hbn)
                ps, pn = K.ps[4], "ps4"

                def mmr(e, ps=ps, hf32=hf32):
                    for k in range(8):
                        ins = e.matmul(ps[:, 0:36], hf32[:, k, :], wr[:, k, :], start=(k == 0), stop=(k == 7))
                    return ins
                P.op("pe", mmr, reads=[hfn, tg + "wr"], writes=[pn])
                route(K, tg, rsm, ps, pn, brow, c)


def route(K, tg, r, ps, pn, brow, c):
    P = K.P
    V = lambda fn, rd, wr_: P.op("dve", fn, reads=[tg + x if not x.startswith("ps") else x for x in rd], writes=[tg + x for x in wr_])
    A = lambda fn, rd, wr_: P.op("act", fn, reads=[tg + x for x in rd], writes=[tg + x for x in wr_])
    lg, m4, oh, ex, se, tmp32, le8, m1, mk1, le2, m2, mk2, w1, w2, g8 = [r[k] for k in
        ("lg", "m4", "oh", "ex", "se", "tmp32", "le8", "m1", "mk1", "le2", "m2", "mk2", "w1", "w2", "g8")]
    V(lambda e: e.tensor_tensor(out=lg[:], in0=ps[:, 0:36], in1=brow[:], op=ALU.add), [pn, "brow"], ["rs_lg"])
    V(lambda e: e.tensor_reduce(out=m4[:], in_=lg[:, 0:4], axis=AX.X, op=ALU.max), ["rs_lg"], ["rs_m4"])
    V(lambda e: e.tensor_scalar(out=oh[:], in0=lg[:, 0:4], scalar1=m4[:, 0:1], scalar2=None, op0=ALU.is_ge), ["rs_lg", "rs_m4"], ["rs_oh"])
    V(lambda e: e.tensor_scalar(out=ex[:], in0=lg[:, 0:4], scalar1=m4[:, 0:1], scalar2=None, op0=ALU.subtract), ["rs_lg", "rs_m4"], ["rs_ex"])
    A(lambda e: e.activation(out=ex[:], in_=ex[:], func=AF.Exp, accum_out=se[:, 0:1]), ["rs_ex"], ["rs_ex", "rs_se"])
    V(lambda e: e.reciprocal(out=se[:], in_=se[:]), ["rs_se"], ["rs_se"])
    V(lambda e: e.tensor_tensor(out=tmp32[:].rearrange("p (g e) -> p g e", g=4), in0=lg[:, 4:36].rearrange("p (g e) -> p g e", g=4),
                                in1=oh[:].unsqueeze(2).to_broadcast([128, 4, 8]), op=ALU.mult), ["rs_lg", "rs_oh"], ["rs_tmp32"])
    V(lambda e: e.tensor_reduce(out=le8[:], in_=tmp32[:].rearrange("p (g e) -> p e g", g=4), axis=AX.X, op=ALU.add), ["rs_tmp32"], ["rs_le8"])
    V(lambda e: e.tensor_reduce(out=m1[:], in_=le8[:], axis=AX.X, op=ALU.max), ["rs_le8"], ["rs_m1"])
    V(lambda e: e.tensor_scalar(out=mk1[:], in0=le8[:], scalar1=m1[:, 0:1], scalar2=None, op0=ALU.is_ge), ["rs_le8", "rs_m1"], ["rs_mk1"])
    V(lambda e: e.scalar_tensor_tensor(out=le2[:], in0=mk1[:], scalar=-1e30, in1=le8[:], op0=ALU.mult, op1=ALU.add), ["rs_mk1", "rs_le8"], ["rs_le2"])
    V(lambda e: e.tensor_reduce(out=m2[:], in_=le2[:], axis=AX.X, op=ALU.max), ["rs_le2"], ["rs_m2"])
    V(lambda e: e.tensor_scalar(out=mk2[:], in0=le2[:], scalar1=m2[:, 0:1], scalar2=None, op0=ALU.is_ge), ["rs_le2", "rs_m2"], ["rs_mk2"])
    V(lambda e: e.tensor_tensor(out=w1[:], in0=m2[:], in1=m1[:], op=ALU.subtract), ["rs_m1", "rs_m2"], ["rs_w1"])
    A(lambda e: e.activation(out=w1[:], in_=w1[:], func=AF.Exp), ["rs_w1"], ["rs_w1"])
    V(lambda e: e.tensor_scalar_add(out=w1[:], in0=w1[:], scalar1=1.0), ["rs_w1"], ["rs_w1"])
    V(lambda e: e.reciprocal(out=w1[:], in_=w1[:]), ["rs_w1"], ["rs_w1"])
    V(lambda e: e.tensor_scalar(out=w2[:], in0=w1[:], scalar1=-1.0, scalar2=1.0, op0=ALU.mult, op1=ALU.add), ["rs_w1"], ["rs_w2"])
    V(lambda e: e.tensor_tensor(out=w1[:], in0=w1[:], in1=se[:], op=ALU.mult), ["rs_w1", "rs_se"], ["rs_w1"])
    V(lambda e: e.tensor_tensor(out=w2[:], in0=w2[:], in1=se[:], op=ALU.mult), ["rs_w2", "rs_se"], ["rs_w2"])
    V(lambda e: e.tensor_scalar(out=g8[:], in0=mk1[:], scalar1=w1[:, 0:1], scalar2=None, op0=ALU.mult), ["rs_mk1", "rs_w1"], ["rs_g8"])
    V(lambda e: e.scalar_tensor_tensor(out=g8[:], in0=mk2[:], scalar=w2[:, 0:1], in1=g8[:], op0=ALU.mult, op1=ALU.add), ["rs_mk2", "rs_w2", "rs_g8"], ["rs_g8"])
    ssel, mk12, s32, s32b, jk = r["ssel"], r["mk12"], r["s32"], r["s32b"], r["jk"]
    V(lambda e: e.tensor_copy(out=K.W12[:, c, 0:1], in_=w1[:]), ["rs_w1"], ["rs_wc"])
    V(lambda e: e.tensor_copy(out=K.W12[:, c, 1:2], in_=w2[:]), ["rs_w2"], ["rs_wc2"])
    for ki, mk in enumerate((mk1, mk2)):
        mkn = "rs_mk1" if ki == 0 else "rs_mk2"
        V(lambda e, mk=mk: e.tensor_tensor(out=s32[:].rearrange("p (g e) -> p g e", g=4),
                                           in0=oh[:].unsqueeze(2).to_broadcast([128, 4, 8]),
                                           in1=mk[:].unsqueeze(1).to_broadcast([128, 4, 8]), op=ALU.mult),
          ["rs_oh", mkn], ["rs_s32"])
        V(lambda e: e.tensor_tensor(out=jk[:], in0=s32[:], in1=K.cvec[:, 66:98], op=ALU.mult), ["rs_s32"], ["rs_jk"])
        V(lambda e, ki=ki: e.tensor_reduce(out=K.E12[:, c, ki:ki + 1], in_=jk[:], axis=AX.X, op=ALU.add), ["rs_jk"], ["rs_e%d" % ki])
        if ki == 0:
            V(lambda e: e.tensor_copy(out=ssel[:], in_=s32[:]), ["rs_s32"], ["rs_ssel"])
        else:
            V(lambda e: e.tensor_tensor(out=s32b[:], in0=s32[:], in1=ssel[:], op=ALU.add), ["rs_s32", "rs_ssel"], ["rs_s32b"])
    psr, psrn = K.ps[5], "ps5"

    def mmc(e):
        e.matmul(psr[:, 0:32], K.cb[:, 6, :], s32b[:], start=True, stop=True)
        return e.matmul(psr[:, 32:64], K.cb[:, 3, :], s32b[:], start=True, stop=True)
    P.op("pe", mmc, reads=[tg + "rs_s32b", "cmatb"], writes=[psrn])
    P.op("dve", lambda e: e.tensor_tensor(out=K.RK[:, c, :], in0=psr[:, 0:32], in1=K.pref[:], op=ALU.add),
         reads=[psrn, "pref"], writes=[("RK", c)])
    P.op("dve", lambda e: e.tensor_tensor(out=K.pref[:], in0=psr[:, 32:64], in1=K.pref[:], op=ALU.add),
         reads=[psrn, "pref"], writes=["pref"])


def pass_moe(K, l, last):
    nc, P, I, S = K.nc, K.P, K.I, K.S
    import contextlib
    with contextlib.ExitStack() as st:
        sb = lambda n, s, d=F32: st.enter_context(nc.sbuf_tensor("sb_" + n, list(s), d))
        tg = "E%d_" % l
        NSG = 11
        h2T = sb(tg + "h2T", [128, 8, NSG * 128], BF16)
        yacc = sb(tg + "yacc", [128, NSG, 1024])
        wg = [sb(tg + "wg%d" % i, [128, 8, 512], BF16) for i in range(2)]
        wu = [sb(tg + "wu%d" % i, [128, 8, 512], BF16) for i in range(2)]
        wd = [sb(tg + "wd%d" % i, [128, 4, 1024], BF16) for i in range(2)]
        sg_ = [sb(tg + "sg%d" % i, [128, 512]) for i in range(2)]
        h1T = [sb(tg + "h1T%d" % i, [128, 4, 512], BF16) for i in range(2)]
        gate_bc = [sb(tg + "gate%d" % s_, [128, 1024]) for s_ in range(2)]
        gbc = sb(tg + "gbc", [128, 1024])
        bbc = sb(tg + "bbc", [128, 1024])
        xt = [sb(tg + "x%d" % i, [128, 1024]) for i in range(2)]
        x2t = [sb(tg + "x2_%d" % i, [128, 1024]) for i in range(2)]
        st6 = sb(tg + "st6", [128, 12])
        mv = sb(tg + "mv", [128, 2])
        for s_ in range(2):
            bcast_rows(K, l, gate_bc[s_], tg + "gate%d" % s_, 5 * 1024, s_)
        P.op("sp", lambda e: e.dma_start(out=gbc[:], in_=I["ln2_g"][l].partition_broadcast(128)), writes=[tg + "gbc"], key="pc4")
        P.op("sp", lambda e: e.dma_start(out=bbc[:], in_=I["ln2_b"][l].partition_broadcast(128)), writes=[tg + "bbc"], key="pc5")
        wi = 0
        hi_ = 0
        xi = 0
        for sg0 in range(0, NCH, NSG):
            cs = list(range(sg0, min(NCH, sg0 + NSG)))
            if last and cs[-1] < 2:
                continue
            ntok = len(cs) * 128
            t0 = sg0 * 128
            P.op("sp", lambda e, t0=t0, ntok=ntok: e.dma_start(
                out=h2T[:, :, 0:ntok], in_=S["h2T"][:, t0:t0 + ntok].rearrange("(k p) t -> p k t", p=128)),
                writes=[tg + "h2T"], key=tg + "h2T")
            P.op("pool", lambda e: e.memset(yacc[:], 0.0), writes=[tg + "yacc"] + [(tg + "yaccn", ci) for ci in range(NSG)])
            subs = [(o, min(512, ntok - o)) for o in range(0, ntok, 512)]
            for ex in range(NE):
                sl = wi % 2
                wi += 1
                wg_t, wu_t, wd_t = wg[sl], wu[sl], wd[sl]
                wgn, wun, wdn = tg + "wg%d" % sl, tg + "wu%d" % sl, tg + "wd%d" % sl
                P.op("pool", lambda e, wg_t=wg_t, ex=ex: e.dma_start(
                    out=wg_t[:], in_=I["w_eg"][l, ex].rearrange("(k p) n -> p k n", p=128)), writes=[wgn], key=wgn)
                P.op("pool", lambda e, wu_t=wu_t, ex=ex: e.dma_start(
                    out=wu_t[:], in_=I["w_eu"][l, ex].rearrange("(k p) n -> p k n", p=128)), writes=[wun], key=wun)
                P.op("pool", lambda e, wd_t=wd_t, ex=ex: e.dma_start(
                    out=wd_t[:], in_=I["w_ed"][l, ex].rearrange("(k p) n -> p k n", p=128)), writes=[wdn], key=wdn)
                for (o, n) in subs:
                    hs = hi_ % 2
                    hi_ += 1
                    h1 = h1T[hs]
                    h1n = tg + "h1T%d" % hs
                    for cc in range(4):
                        psG, pnG = K.ps[(cc % 2) * 2], "ps%d" % ((cc % 2) * 2)
                        psU, pnU = K.ps[(cc % 2) * 2 + 1], "ps%d" % ((cc % 2) * 2 + 1)

                        def mmg(e, psG=psG, cc=cc, o=o, n=n, wg_t=wg_t):
                            for k in range(8):
                                ins = e.matmul(psG[:, 0:n], wg_t[:, k, cc * 128:(cc + 1) * 128], h2T[:, k, o:o + n], start=(k == 0), stop=(k == 7))
                            return ins
                        P.op("pe", mmg, reads=[wgn, tg + "h2T"], writes=[pnG])

                        def mmu(e, psU=psU, cc=cc, o=o, n=n, wu_t=wu_t):
                            for k in range(8):
                                ins = e.matmul(psU[:, 0:n], wu_t[:, k, cc * 128:(cc + 1) * 128], h2T[:, k, o:o + n], start=(k == 0), stop=(k == 7))
                            return ins
                        P.op("pe", mmu, reads=[wun, tg + "h2T"], writes=[pnU])
                        sgt = sg_[cc % 2]
                        sgn = tg + "sg%d" % (cc % 2)
                        P.op("act", lambda e, psG=psG, sgt=sgt, n=n: e.activation(out=sgt[:, 0:n], in_=psG[:, 0:n], func=AF.Silu),
                             reads=[pnG], writes=[sgn])
                        P.op("dve", lambda e, psU=psU, sgt=sgt, n=n, cc=cc, h1=h1: e.tensor_tensor(
                            out=h1[:, cc, 0:n], in0=psU[:, 0:n], in1=sgt[:, 0:n], op=ALU.mult),
                            reads=[pnU, sgn], writes=[h1n], multi=True)
                    for q in range(n // 128):
                        ci = (o // 128) + q
                        c = cs[ci]
                        for hf in range(2):
                            ps, pn = K.ps[4 + (q * 2 + hf) % 4], "ps%d" % (4 + (q * 2 + hf) % 4)

                            def mmd(e, ps=ps, q=q, hf=hf, h1=h1, wd_t=wd_t):
                                for k in range(4):
                                    ins = e.matmul(ps[:], h1[:, k, q * 128:(q + 1) * 128], wd_t[:, k, hf * 512:(hf + 1) * 512],
                                                   start=(k == 0), stop=(k == 3))
                                return ins
                            P.op("pe", mmd, reads=[h1n, wdn], writes=[pn])
                            P.op("dve", lambda e, ps=ps, ci=ci, hf=hf, c=c, ex=ex: e.scalar_tensor_tensor(
                                out=yacc[:, ci, hf * 512:(hf + 1) * 512], in0=ps[:], scalar=K.Gall[:, c, ex:ex + 1],
                                in1=yacc[:, ci, hf * 512:(hf + 1) * 512], op0=ALU.mult, op1=ALU.add),
                                reads=[pn, ("G", c), tg + "yacc"], writes=[(tg + "yacc", ci, hf)])
            for ci, c in enumerate(cs):
                if last and c < 2:
                    continue
                s_ = chunk_stream(c)
                sl = xi % 2
                xi += 1
                x_t, x2_t = xt[sl], x2t[sl]
                xn, x2n = tg + "x%d" % sl, tg + "x2_%d" % sl
                P.op("sp", lambda e, x_t=x_t, c=c: e.dma_start(out=x_t[:], in_=S["x1"][c * 128:(c + 1) * 128, :]), writes=[xn], key=xn)
                yv = yacc[:, ci, :]
                yn_ = (tg + "yaccn", ci)
                P.op("dve", lambda e, yv=yv, s_=s_: e.tensor_tensor(out=yv, in0=yv, in1=gate_bc[s_][:], op=ALU.mult),
                     reads=[(tg + "yacc", ci, 0), (tg + "yacc", ci, 1), tg + "gate%d" % s_], writes=[yn_])
                P.op("dve", lambda e, x_t=x_t, yv=yv: e.scalar_tensor_tensor(out=yv, in0=x_t[:], scalar=ALPHA, in1=yv, op0=ALU.mult, op1=ALU.add),
                     reads=[xn, yn_], writes=[yn_])
                layer_norm_tile(K, tg, yv, x2_t, st6, mv, gbc, bbc, (yn_, x2n))
                if last:
                    P.op("sp", lambda e, x2_t=x2_t, c=c: e.dma_start(out=K.out[(c - 2) * 128:(c - 1) * 128, :], in_=x2_t[:]),
                         reads=[x2n], key="st_" + x2n)
                else:
                    P.op("sp", lambda e, x2_t=x2_t, c=c: e.dma_start(out=S["xres"][c * 128:(c + 1) * 128, :], in_=x2_t[:]),
                         reads=[x2n], key="st_" + x2n)


def pass_dispatch(K, l):
    nc, P, I, S = K.nc, K.P, K.I, K.S
    import contextlib
    with contextlib.ExitStack() as st:
        sb = lambda n, s, d=F32: st.enter_context(nc.sbuf_tensor("sb_" + n, list(s), d))
        tg = "F%d_" % l
        big = sb(tg + "big", [128, NBLK * 32])
        nb = sb(tg + "nb", [128, 32])
        nbT = sb(tg + "nbT", [32, 128])
        bs = sb(tg + "bs", [128, 32])
        be = sb(tg + "be", [128, 32])
        pst = sb(tg + "pst", [128, 32])
        blke = sb(tg + "blke", [128, NBLK])
        rkp = sb(tg + "rkp", [128, NCH, 32])
        oh = sb(tg + "oh", [128, NCH, 32])
        dst = sb(tg + "dst", [128, NCH, 2])
        cnt = K.pref
        P.op("dve", lambda e: e.tensor_tensor(out=big[:, 0:32 * 66].rearrange("p (e j) -> p e j", e=32),
                                              in0=cnt[:].unsqueeze(2).to_broadcast([128, 32, 66]),
                                              in1=K.cvec[:, 0:66].unsqueeze(1).to_broadcast([128, 32, 66]), op=ALU.is_gt),
             reads=["pref", "cvec"], writes=[tg + "big"])
        P.op("dve", lambda e: e.tensor_reduce(out=nb[:], in_=big[:, 0:32 * 66].rearrange("p (e j) -> p e j", e=32), axis=AX.X, op=ALU.add),
             reads=[tg + "big"], writes=[tg + "nb"])
        ps = K.ps[0]
        P.op("pe", lambda e: e.matmul(ps[0:32, 0:128], nb[:], K.cm[:, 0, :], start=True, stop=True), reads=[tg + "nb", "cmat"], writes=["ps0"])
        P.op("dve", lambda e: e.tensor_copy(out=nbT[:], in_=ps[0:32, 0:128]), reads=["ps0"], writes=[tg + "nbT"])
        ps1 = K.ps[1]
        P.op("pe", lambda e: e.matmul(ps1[:, 0:32], nbT[:], K.cm[0:32, 6, 0:32], start=True, stop=True), reads=[tg + "nbT", "cmat"], writes=["ps1"])
        P.op("dve", lambda e: e.tensor_copy(out=bs[:], in_=ps1[:, 0:32]), reads=["ps1"], writes=[tg + "bs"])
        P.op("dve", lambda e: e.tensor_tensor(out=be[:], in0=bs[:], in1=nb[:], op=ALU.add), reads=[tg + "bs", tg + "nb"], writes=[tg + "be"])
        P.op("dve", lambda e: e.tensor_scalar_mul(out=pst[:], in0=bs[:], scalar1=256.0), reads=[tg + "bs"], writes=[tg + "pst"])
        P.op("dve", lambda e: e.tensor_tensor(out=big[:].rearrange("p (b e) -> p b e", b=NBLK),
                                              in0=be[:].unsqueeze(1).to_broadcast([128, NBLK, 32]),
                                              in1=K.cvec[:, 98:98 + NBLK].unsqueeze(2).to_broadcast([128, NBLK, 32]), op=ALU.is_le),
             reads=[tg + "be", "cvec", tg + "nb"], writes=[tg + "big"])
        P.op("dve", lambda e: e.tensor_reduce(out=blke[:], in_=big[:].rearrange("p (b e) -> p b e", b=NBLK), axis=AX.X, op=ALU.add),
             reads=[tg + "big"], writes=[tg + "blke"])
        P.op("dve", lambda e: e.tensor_scalar_min(out=blke[:], in0=blke[:], scalar1=31.0), reads=[tg + "blke"], writes=[tg + "blke"])
        P.op("dve", lambda e: e.tensor_scalar(out=blke[:], in0=blke[:], scalar1=128.0, scalar2=K.cvec[:, 196:197], op0=ALU.mult, op1=ALU.add),
             reads=[tg + "blke", "cvec"], writes=[tg + "blke"])
        if l > 0:
            P.op("dve", lambda e: e.tensor_scalar_add(out=blke[:], in0=blke[:], scalar1=float(l * NE * 128)), reads=[tg + "blke"], writes=[tg + "blke"])
        P.op("dve", lambda e: e.tensor_copy(out=K.IDXW[:], in_=blke[:]), reads=[tg + "blke"], writes=["IDXW"])
        P.op("dve", lambda e: e.tensor_tensor(out=rkp[:], in0=K.RK[:], in1=pst[:].unsqueeze(1).to_broadcast([128, NCH, 32]), op=ALU.add),
             reads=[("RK", c) for c in range(NCH)] + [tg + "pst"], writes=[tg + "rkp"])
        for ki in range(2):
            P.op("dve", lambda e, ki=ki: e.tensor_tensor(out=oh[:], in0=K.cvec[:, 66:98].unsqueeze(1).to_broadcast([128, NCH, 32]),
                                                        in1=K.E12[:, :, ki:ki + 1].to_broadcast([128, NCH, 32]), op=ALU.is_equal),
                 reads=["cvec"] + [tg + "rs_e%d" % ki], writes=[tg + "oh"])
            P.op("dve", lambda e: e.tensor_tensor(out=oh[:], in0=oh[:], in1=rkp[:], op=ALU.mult), reads=[tg + "oh", tg + "rkp"], writes=[tg + "oh"])
            P.op("dve", lambda e, ki=ki: e.tensor_reduce(out=dst[:, :, ki:ki + 1], in_=oh[:], axis=AX.X, op=ALU.add),
                 reads=[tg + "oh"], writes=[tg + "dst%d" % ki])
        P.op("dve", lambda e: e.tensor_copy(out=K.DEST[:], in_=dst[:]), reads=[tg + "dst0", tg + "dst1"], writes=["DEST"])
        scb = [sb(tg + "scb%d" % s_, [128, 1024]) for s_ in range(2)]
        shb = [sb(tg + "shb%d" % s_, [128, 1024]) for s_ in range(2)]
        for s_ in range(2):
            bcast_rows(K, l, scb[s_], tg + "scb%d" % s_, 4 * 1024, s_)
            bcast_rows(K, l, shb[s_], tg + "shb%d" % s_, 3 * 1024, s_)
            P.op("pool", lambda e, s_=s_: e.tensor_scalar_add(out=scb[s_][:], in0=scb[s_][:], scalar1=1.0),
                 reads=[tg + "scb%d" % s_], writes=[tg + "scb%d" % s_])
        xt = [sb(tg + "x%d" % i, [128, 1024]) for i in range(3)]
        ht = [sb(tg + "h%d" % i, [128, 1024], BF16) for i in range(3)]
        zt = sb(tg + "zt", [128, 2048], BF16)
        P.op("pool", lambda e: e.memset(zt[:], 0.0), writes=[tg + "zt"])
        for b in range(NBLK):
            P.op("sp", lambda e, b=b: e.dma_start(out=S["xin"][b * 256:(b + 1) * 256, :].rearrange("(p two) d -> p (two d)", two=2), in_=zt[:]),
                 reads=[tg + "zt"], writes=["xinz"], key="zf%d" % (b % 4), multi=True)
        for c in range(NCH):
            s_ = chunk_stream(c)
            sl = c % 3
            x_t, h_t = xt[sl], ht[sl]
            xn, hn = tg + "x%d" % sl, tg + "h%d" % sl
            P.op("sp", lambda e, x_t=x_t, c=c: e.dma_start(out=x_t[:], in_=S["x1"][c * 128:(c + 1) * 128, :]), writes=[xn], key=xn)
            P.op("dve", lambda e, x_t=x_t, s_=s_: e.tensor_tensor(out=x_t[:], in0=x_t[:], in1=scb[s_][:], op=ALU.mult),
                 reads=[xn, tg + "scb%d" % s_], writes=[xn])
            P.op("pool", lambda e, x_t=x_t, h_t=h_t, s_=s_: e.tensor_tensor(out=h_t[:], in0=x_t[:], in1=shb[s_][:], op=ALU.add),
                 reads=[xn, tg + "shb%d" % s_], writes=[hn])
            for ki in range(2):
                P.op("pool", lambda e, h_t=h_t, c=c, ki=ki: e.indirect_dma_start(
                    out=S["xin"][:, :], out_offset=bass.IndirectOffsetOnAxis(ap=K.DEST[:, c, ki:ki + 1], axis=0),
                    in_=h_t[:, :], in_offset=None), reads=[hn, "DEST", "xinz"], writes=[("xin", c, ki)], key=tg + "sc%d_%d" % (sl, ki))


def pass_experts(K, l):
    nc, P, I, S = K.nc, K.P, K.I, K.S
    import contextlib
    with contextlib.ExitStack() as st:
        sb = lambda n, s, d=F32: st.enter_context(nc.sbuf_tensor("sb_" + n, list(s), d))
        tg = "G%d_" % l
        wgu = [sb(tg + "wgu%d" % i, [128, 8 * 1024], BF16) for i in range(2)]
        wd = [sb(tg + "wd%d" % i, [128, 4 * 1024], BF16) for i in range(2)]
        xr = [sb(tg + "xr%d" % i, [128, 2, 1024], BF16) for i in range(2)]
        xT = [sb(tg + "xT%d" % i, [128, 8, 256], BF16) for i in range(2)]
        sg_ = [sb(tg + "sg%d" % i, [128, 256]) for i in range(2)]
        h1T = [sb(tg + "h1T%d" % i, [128, 4, 256], BF16) for i in range(2)]
        yb = [sb(tg + "yb%d" % i, [128, 1024]) for i in range(2)]
        yi = 0
        for b in range(NBLK):
            sl = b % 2
            wgu_t, wd_t, xr_t, xT_t, h1 = wgu[sl], wd[sl], xr[sl], xT[sl], h1T[sl]
            wgn, wdn, xrn, xTn, h1n = [tg + x + "%d" % sl for x in ("wgu", "wd", "xr", "xT", "h1T")]
            P.op("pool", lambda e, wgu_t=wgu_t, b=b: e.indirect_dma_start(
                out=wgu_t[:, :], out_offset=None, in_=S["wgub"][:, :],
                in_offset=bass.IndirectOffsetOnAxis(ap=K.IDXW[:, b:b + 1], axis=0)),
                reads=["IDXW", "bg:w%d" % l], writes=[wgn], key=wgn)
            P.op("pool", lambda e, wd_t=wd_t, b=b: e.indirect_dma_start(
                out=wd_t[:, :], out_offset=None, in_=S["wdb"][:, :],
                in_offset=bass.IndirectOffsetOnAxis(ap=K.IDXW[:, b:b + 1], axis=0)),
                reads=["IDXW", "bg:w%d" % l], writes=[wdn], key=wdn)
            P.op("sp", lambda e, xr_t=xr_t, b=b: e.dma_start(
                out=xr_t[:], in_=S["xin"][b * 256:(b + 1) * 256, :].rearrange("(r p) d -> p r d", p=128)),
                writes=[xrn], key=xrn)
            for r_ in range(2):
                for f4 in range(2):
                    ps, pn = K.ps[(r_ * 2 + f4) % 2], "ps%d" % ((r_ * 2 + f4) % 2)

                    def tr(e, ps=ps, r_=r_, f4=f4, xr_t=xr_t):
                        for ff in range(4):
                            f = f4 * 4 + ff
                            ins = e.matmul(ps[:, ff * 128:(ff + 1) * 128], xr_t[:, r_, f * 128:(f + 1) * 128], K.cb[:, 0, :], start=True, stop=True)
                        return ins
                    P.op("pe", tr, reads=[xrn, "cmatb"], writes=[pn])
                    if f4 == 0:
                        P.op("act", lambda e, ps=ps, r_=r_, f4=f4, xT_t=xT_t: e.copy(
                            out=xT_t[:, f4 * 4:(f4 + 1) * 4, r_ * 128:(r_ + 1) * 128], in_=ps[:].rearrange("p (f t) -> p f t", f=4)),
                            reads=[pn], writes=[xTn], multi=True)
                    else:
                        P.op("dve", lambda e, ps=ps, r_=r_, f4=f4, xT_t=xT_t: e.tensor_copy(
                            out=xT_t[:, f4 * 4:(f4 + 1) * 4, r_ * 128:(r_ + 1) * 128], in_=ps[:].rearrange("p (f t) -> p f t", f=4)),
                            reads=[pn], writes=[xTn], multi=True)
            for cc in range(4):
                ps, pn = K.ps[2 + cc % 2], "ps%d" % (2 + cc % 2)

                def mmg(e, ps=ps, cc=cc, wgu_t=wgu_t, xT_t=xT_t):
                    for k in range(8):
                        e.matmul(ps[:, 0:256], wgu_t[:, k * 1024 + cc * 128:k * 1024 + (cc + 1) * 128], xT_t[:, k, :], start=(k == 0), stop=(k == 7))
                    for k in range(8):
                        ins = e.matmul(ps[:, 256:512], wgu_t[:, k * 1024 + 512 + cc * 128:k * 1024 + 512 + (cc + 1) * 128], xT_t[:, k, :],
                                       start=(k == 0), stop=(k == 7))
                    return ins
                P.op("pe", mmg, reads=[wgn, xTn], writes=[pn])
                sgt, sgn = sg_[cc % 2], tg + "sg%d" % (cc % 2)
                P.op("act", lambda e, ps=ps, sgt=sgt: e.activation(out=sgt[:], in_=ps[:, 0:256], func=AF.Silu), reads=[pn], writes=[sgn])
                P.op("dve", lambda e, ps=ps, sgt=sgt, cc=cc, h1=h1: e.tensor_tensor(out=h1[:, cc, :], in0=ps[:, 256:512], in1=sgt[:], op=ALU.mult),
                     reads=[pn, sgn], writes=[h1n], multi=True)
            for r_ in range(2):
                y_t, yn_ = yb[yi % 2], tg + "yb%d" % (yi % 2)
                yi += 1
                for hf in range(2):
                    ps, pn = K.ps[4 + (r_ * 2 + hf)], "ps%d" % (4 + r_ * 2 + hf)

                    def mmd(e, ps=ps, r_=r_, hf=hf, h1=h1, wd_t=wd_t):
                        for k in range(4):
                            ins = e.matmul(ps[:], h1[:, k, r_ * 128:(r_ + 1) * 128], wd_t[:, k * 1024 + hf * 512:k * 1024 + (hf + 1) * 512],
                                           start=(k == 0), stop=(k == 3))
                        return ins
                    P.op("pe", mmd, reads=[h1n, wdn], writes=[pn])
                    if hf == 0:
                        P.op("act", lambda e, ps=ps, y_t=y_t, hf=hf: e.copy(out=y_t[:, hf * 512:(hf + 1) * 512], in_=ps[:]),
                             reads=[pn], writes=[yn_], multi=True)
                    else:
                        P.op("dve", lambda e, ps=ps, y_t=y_t, hf=hf: e.tensor_copy(out=y_t[:, hf * 512:(hf + 1) * 512], in_=ps[:]),
                             reads=[pn], writes=[yn_], multi=True)
                P.op("sp", lambda e, y_t=y_t, b=b, r_=r_: e.dma_start(out=S["yrows"][b * 256 + r_ * 128:b * 256 + (r_ + 1) * 128, :], in_=y_t[:]),
                     reads=[yn_], key="st_" + yn_)


def pass_combine(K, l, last):
    nc, P, I, S = K.nc, K.P, K.I, K.S
    import contextlib
    with contextlib.ExitStack() as st:
        sb = lambda n, s, d=F32: st.enter_context(nc.sbuf_tensor("sb_" + n, list(s), d))
        tg = "H%d_" % l
        gate_bc = [sb(tg + "gate%d" % s_, [128, 1024]) for s_ in range(2)]
        gbc = sb(tg + "gbc", [128, 1024])
        bbc = sb(tg + "bbc", [128, 1024])
        for s_ in range(2):
            bcast_rows(K, l, gate_bc[s_], tg + "gate%d" % s_, 5 * 1024, s_)
        P.op("sp", lambda e: e.dma_start(out=gbc[:], in_=I["ln2_g"][l].partition_broadcast(128)), writes=[tg + "gbc"], key="pc4")
        P.op("sp", lambda e: e.dma_start(out=bbc[:], in_=I["ln2_b"][l].partition_broadcast(128)), writes=[tg + "bbc"], key="pc5")
        r1 = [sb(tg + "r1_%d" % i, [128, 1024]) for i in range(3)]
        r2 = [sb(tg + "r2_%d" % i, [128, 1024]) for i in range(3)]
        xt = [sb(tg + "x%d" % i, [128, 1024]) for i in range(3)]
        x2t = [sb(tg + "x2_%d" % i, [128, 1024]) for i in range(2)]
        st6 = sb(tg + "st6", [128, 12])
        mv = sb(tg + "mv", [128, 2])
        it = 0
        for c in range(NCH):
            if last and c < 2:
                continue
            s_ = chunk_stream(c)
            sl = it % 3
            sl2 = it % 2
            it += 1
            a1, a2, x_t, x2_t = r1[sl], r2[sl], xt[sl], x2t[sl2]
            n1, n2, xn, x2n = tg + "r1_%d" % sl, tg + "r2_%d" % sl, tg + "x%d" % sl, tg + "x2_%d" % sl2
            P.op("pool", lambda e, a1=a1, c=c: e.indirect_dma_start(
                out=a1[:, :], out_offset=None, in_=S["yrows"][:, :],
                in_offset=bass.IndirectOffsetOnAxis(ap=K.DEST[:, c, 0:1], axis=0)),
                reads=["DEST"], writes=[n1], key=n1)
            P.op("pool", lambda e, a2=a2, c=c: e.indirect_dma_start(
                out=a2[:, :], out_offset=None, in_=S["yrows"][:, :],
                in_offset=bass.IndirectOffsetOnAxis(ap=K.DEST[:, c, 1:2], axis=0)),
                reads=["DEST"], writes=[n2], key=n2)
            P.op("sp", lambda e, x_t=x_t, c=c: e.dma_start(out=x_t[:], in_=S["x1"][c * 128:(c + 1) * 128, :]), writes=[xn], key=xn)
            P.op("dve", lambda e, a1=a1, c=c: e.tensor_scalar_mul(out=a1[:], in0=a1[:], scalar1=K.W12[:, c, 0:1]), reads=[n1], writes=[n1])
            P.op("dve", lambda e, a1=a1, a2=a2, c=c: e.scalar_tensor_tensor(out=a1[:], in0=a2[:], scalar=K.W12[:, c, 1:2], in1=a1[:],
                                                                          op0=ALU.mult, op1=ALU.add), reads=[n1, n2], writes=[n1])
            P.op("pool", lambda e, a1=a1, s_=s_: e.tensor_tensor(out=a1[:], in0=a1[:], in1=gate_bc[s_][:], op=ALU.mult),
                 reads=[n1, tg + "gate%d" % s_], writes=[n1])
            P.op("dve", lambda e, a1=a1, x_t=x_t: e.scalar_tensor_tensor(out=a1[:], in0=x_t[:], scalar=ALPHA, in1=a1[:], op0=ALU.mult, op1=ALU.add),
                 reads=[xn, n1], writes=[n1])
            layer_norm_tile(K, tg, a1, x2_t, st6, mv, gbc, bbc, (n1, x2n))
            if last:
                P.op("sp", lambda e, x2_t=x2_t, c=c: e.dma_start(out=K.out[(c - 2) * 128:(c - 1) * 128, :], in_=x2_t[:]),
                     reads=[x2n], key="st_" + x2n)
            else:
                P.op("sp", lambda e, x2_t=x2_t, c=c: e.dma_start(out=S["xres"][c * 128:(c + 1) * 128, :], in_=x2_t[:]),
                     reads=[x2n], key="st_" + x2n)


def layer(K, l, stop):
    nc, P, I, S = K.nc, K.P, K.I, K.S
    import contextlib
    with contextlib.ExitStack() as st:
        K.modfm = st.enter_context(nc.sbuf_tensor("sb_modfm%d" % l, [128, 48, 2], F32))
        K.RK = st.enter_context(nc.sbuf_tensor("sb_RK%d" % l, [128, NCH, 32], F32))
        K.E12 = st.enter_context(nc.sbuf_tensor("sb_E12_%d" % l, [128, NCH, 2], F32))
        K.W12 = st.enter_context(nc.sbuf_tensor("sb_W12_%d" % l, [128, NCH, 2], F32))
        K.pref = st.enter_context(nc.sbuf_tensor("sb_pref%d" % l, [128, 32], F32))
        K.IDXW = st.enter_context(nc.sbuf_tensor("sb_IDXW%d" % l, [128, NBLK], I32))
        K.DEST = st.enter_context(nc.sbuf_tensor("sb_DEST%d" % l, [128, NCH, 2], I32))
        with contextlib.ExitStack() as st2:
            phase_mod(K, l, st2)
            P.barrier()
        if stop == "mod":
            return
        src = I["xcat"] if l == 0 else S["xres"]
        P.barrier()
        pass_inproj(K, l, src, 0)
        P.barrier()
        pass_inproj(K, l, src, 1)
        P.barrier()
        if stop == "A":
            return
        with contextlib.ExitStack() as st3:
            L = layer_consts(K, l, st3)
            P.barrier()
            pass_conv_bwd(K, l, L)
            P.barrier()
            if stop == "B":
                return
            pass_ssd_fwd(K, l, L)
            P.barrier()
            if stop == "C":
                return
        pass_pool(K, l)
        P.barrier()
        if stop == "D1":
            return
        pass_merge(K, l, src)
        P.barrier()
        if stop == "D2":
            return
        pass_dispatch(K, l)
        P.barrier()
        if "dest" in K.S:
            P.op("sp", lambda e: e.dma_start(out=K.S["dest"], in_=K.DEST[:].rearrange("p c k -> p (c k)")), key="dbg1")
            P.op("sp", lambda e: e.dma_start(out=K.S["idxw"], in_=K.IDXW[:]), key="dbg2")
            P.op("sp", lambda e: e.dma_start(out=K.S["e12"], in_=K.E12[:].rearrange("p c k -> p (c k)")), key="dbg1")
            P.op("sp", lambda e: e.dma_start(out=K.S["w12"], in_=K.W12[:].rearrange("p c k -> p (c k)")), key="dbg2")
            P.barrier()
        if stop == "F":
            return
        pass_experts(K, l)
        P.barrier()
        if stop == "G":
            return
        pass_combine(K, l, l == 1)
        P.barrier()


def host_inputs(inputs, b):
    f = np.float32
    m = {}
    m["xcat"] = np.ascontiguousarray(np.concatenate([inputs["ctx"][b], inputs["x"][b]], axis=0), dtype=f)
    cc = np.stack([inputs["c_ctx"].reshape(8, 128).T, inputs["c"][b].reshape(8, 128).T], axis=-1)
    m["cc"] = np.ascontiguousarray(cc, dtype=f)
    m["w_ada"] = np.ascontiguousarray(inputs["w_ada"], dtype=f)
    m["b_ada"] = np.ascontiguousarray(inputs["b_ada"], dtype=f)
    m["w_in"] = np.ascontiguousarray(inputs["w_in"], dtype=f)
    m["b_gate"] = np.ascontiguousarray(inputs["b_gate"].reshape(2, 16, 128).transpose(0, 2, 1), dtype=f)
    m["ident"] = np.eye(128, dtype=f)
    ii = np.arange(128)
    cm = np.zeros((128, 7, 128), f)
    cm[:, 6, :] = (ii[:, None] < ii[None, :])
    cm[:, 0, :] = np.eye(128)
    cm[:, 1, :] = (ii[:, None] <= ii[None, :])
    cm[:, 2, :] = (ii[:, None] >= ii[None, :])
    cm[:, 3, :] = 1.0
    cm[:, 4, :] = np.where(ii[None, :] >= ii[:, None], 0.0, -60000.0)
    cm[:, 5, :] = np.where(ii[None, :] <= ii[:, None], 0.0, -60000.0)
    m["cmat"] = cm
    cw = inputs["conv_w"].reshape(2, 5, 24, 128).transpose(0, 3, 1, 2)
    m["cw"] = np.ascontiguousarray(cw, dtype=f)
    m["cbfm"] = np.ascontiguousarray(inputs["conv_b"].reshape(2, 24, 128).transpose(0, 2, 1), dtype=f)
    m["conv_b"] = np.ascontiguousarray(inputs["conv_b"], dtype=f)
    m["dt_bias"] = np.ascontiguousarray(inputs["dt_bias"].reshape(2, 64), dtype=f)
    m["a_log"] = np.ascontiguousarray(inputs["a_log"].reshape(2, 64), dtype=f)
    m["d_skip"] = np.ascontiguousarray(inputs["d_skip"], dtype=f)
    pp, pc = pool_consts()
    m["poolP"] = pp
    m["poolC"] = pc
    m["pool_w"] = np.ascontiguousarray(inputs["pool_w"], dtype=f)
    m["pscfm"] = np.ascontiguousarray(inputs["pool_scale"].reshape(2, 8, 128).transpose(0, 2, 1), dtype=f)
    m["gnfm"] = np.ascontiguousarray(inputs["ssd_norm_g"].reshape(2, 16, 128).transpose(0, 2, 1), dtype=f)
    for k in ("w_branch_a", "w_branch_b", "w_out", "ln1_g", "ln1_b", "ln2_g", "ln2_b"):
        m[k] = np.ascontiguousarray(inputs[k], dtype=f)
    wre = inputs["w_router_expert"].transpose(0, 2, 1, 3).reshape(2, 1024, 32)
    wrr = np.concatenate([inputs["w_router_group"], wre], axis=-1)
    m["wr"] = np.ascontiguousarray(wrr.reshape(2, 8, 128, 36).transpose(0, 2, 1, 3), dtype=f)
    m["br"] = np.ascontiguousarray(np.concatenate([inputs["b_router_group"], inputs["b_router_expert"].reshape(2, 32)], axis=-1), dtype=f)
    m["wgu"], m["wdr"] = expert_layout(inputs)
    cv = np.zeros((128, 197), f)
    cv[:, 0:66] = 256.0 * np.arange(66)[None, :]
    cv[:, 66:98] = np.arange(32)[None, :]
    cv[:, 98:196] = np.arange(98)[None, :]
    cv[:, 196] = np.arange(128)
    m["cvec"] = cv
    sel = np.zeros((2, 2, 128), f)
    sel[0, 0, :] = 1
    sel[1, 1, :] = 1
    m["sel"] = sel
    return m


_PC = {}
_PC2 = {}


def expert_layout(inputs):
    key = ("wl", id(inputs["w_expert_gate"]))
    global _PC2
    if key not in _PC2:
        gu = np.concatenate([inputs["w_expert_gate"], inputs["w_expert_up"]], axis=-1)
        gu = gu.reshape(2, NE, 8, 128, 1024).transpose(0, 1, 3, 2, 4).reshape(2, NE * 128, 8 * 1024)
        wd = inputs["w_expert_down"].reshape(2, NE, 4, 128, 1024).transpose(0, 1, 3, 2, 4).reshape(2, NE * 128, 4 * 1024)
        _PC2 = {key: (np.ascontiguousarray(gu, dtype=np.float32), np.ascontiguousarray(wd, dtype=np.float32))}
    return _PC2[key]


def pool_consts():
    if "p" in _PC:
        return _PC["p"]
    bf = ml_dtypes.bfloat16
    pp = np.zeros((3, 128, 31, 512), np.float32)
    tp = np.arange(128)
    t = np.arange(512)
    for ti, r0 in enumerate((0, 64, 120)):
        for k in POOLK:
            r = r0 + t // 64
            c = t % 64
            cnt_r = np.minimum(r + k // 2, 128) - np.maximum(r - k // 2, 0)
            cnt_c = np.minimum(c + k // 2, 64) - np.maximum(c - k // 2, 0)
            inv = 1.0 / (cnt_r * cnt_c)
            for d in range(POOL_DMIN[k], POOL_DMAX[k] + 1):
                rp = r0 + 2 * d + tp // 64
                cp = tp % 64
                inwin = ((rp[:, None] >= r[None, :] - k // 2) & (rp[:, None] < r[None, :] + k // 2) &
                         (cp[:, None] >= c[None, :] - k // 2) & (cp[:, None] < c[None, :] + k // 2))
                valid = ((rp >= 0) & (rp < 128))[:, None]
                mat = np.where(inwin & valid, inv[None, :], 0.0)
                mat = mat - ((rp[:, None] == r[None, :]) & (cp[:, None] == c[None, :]))
                pp[ti, :, pool_idx(k, d), :] = mat
    pc = np.zeros((128, 8, 256), np.float32)
    t = np.arange(256)
    for kg, k in enumerate(POOLK):
        cnt = np.minimum(t + k // 2, 256) - np.maximum(t - k // 2, 0)
        inv = 1.0 / cnt
        for sl_ in range(2):
            tpp = sl_ * 128 + tp
            inwin = (tpp[:, None] >= t[None, :] - k // 2) & (tpp[:, None] < t[None, :] + k // 2)
            pc[:, kg * 2 + sl_, :] = np.where(inwin, inv[None, :], 0.0) - (tpp[:, None] == t[None, :])
    _PC["p"] = (pp.astype(bf), pc.astype(bf))
    return _PC["p"]


def kernel(**inputs):
    inputs = {k: np.asarray(v) for k, v in inputs.items()}
    nc = build()
    in_maps = [host_inputs(inputs, b) for b in range(8)]
    res = run_bass_kernel_spmd(nc, in_maps, core_ids=list(range(8)))
    return np.stack([r["out"] for r in res.results], axis=0).astype(np.float32)
```

```python
import numpy as np
import ml_dtypes
import concourse.bass as bass
import concourse.mybir as mybir
from concourse.bass_utils import run_bass_kernel_spmd

F32 = mybir.dt.float32
BF16 = mybir.dt.bfloat16
I32 = mybir.dt.int32
AF = mybir.ActivationFunctionType
ALU = mybir.AluOpType
AX = mybir.AxisListType

D = 1024
LCTX = 256
LLAT = 8192
T = LCTX + LLAT
NCH = T // 128
DI = 2048
NH = 32
HP = 64
NG = 4
NS = 128
DXBC = 3072
INC = 8256
C_Z, C_XBC, C_DT, C_U, C_G = 0, 2048, 5120, 5184, 6208
NE = 32
DE = 512
NBLK = 98
NROWS = NBLK * 256
ALPHA = (2.0 * 2) ** 0.25
EPS = 1e-5
GEN = 30000
SAME_SYNC = True
USE_ALIAS = True


import re
_CANON_PAT = r"^(st_|bc_)?(?:[A-H]\d_\d_|[A-H]\d_)"


_ONESHOT = ("ident", "sel", "cmat", "cvec", "cc", "bada", "bg", "modrows_d", "c1", "c2", "c3", "c4", "c5", "c6",
            "pc1", "pc2", "pc3", "pc4", "pc5")


_ALIAS = {"bc_gate0": "g0", "bc_scb0": "g0", "bc_shb0": "g2", "bc_gate1": "g1", "bc_scb1": "g1", "bc_shb1": "g3",
          "pP2": "pP0", "pP3": "pP1", "zf2": "zf0", "zf3": "zf1",
          "sc0_0": "r1_0", "sc1_0": "r1_1", "sc2_0": "r1_2", "sc0_1": "r2_0", "sc1_1": "r2_1", "sc2_1": "r2_2",
          "st_yb0": "st_x1_0", "st_yb1": "st_x1_1", "st_x2_0": "st_x1_0", "st_x2_1": "st_x1_1",
          "wd0": "xs0", "wd1": "xs1", "wgu0": "bt0", "wgu1": "bt1", "xr0": "bct0", "xr1": "bct1",
          "rb0_0": "ut0", "rb0_1": "ut1", "rb1_0": "ut2", "rb1_1": "ut3", "rb2_0": "pP0", "rb2_1": "pP1",
          "rb3_0": "zf0", "rb3_1": "zf1", "wada0": "wld0", "wada1": "wld1", "st_cumT0": "st_dt0", "st_cumT1": "st_dt1"}


def _canon(key):
    key = re.sub(_CANON_PAT, lambda m: (m.group(1) or ""), key)
    if key in _ONESHOT:
        return "once%d" % (_ONESHOT.index(key) % 3)
    return _ALIAS.get(key, key) if USE_ALIAS else key


class _Op:
    __slots__ = ("fn", "deps", "key", "val", "sig", "cnt", "eng", "idx", "pseudo")


class Prog:
    ENGS = ("pe", "act", "dve", "pool", "sp")

    def __init__(self, nc):
        self.nc = nc
        self.ops = {e: [] for e in self.ENGS}
        self.lw = {}
        self.rd = {}
        self.keys = {}
        self.lastkey = {}

    def op(self, eng, fn, reads=(), writes=(), key=None, multi=False, extra=()):
        deps = set(extra)
        for r in reads:
            deps.update(w for w, _ in self.lw.get(r, ()))
        for wr in writes:
            if multi:
                deps.update(w for w, m in self.lw.get(wr, ()) if not m)
            else:
                deps.update(w for w, _ in self.lw.get(wr, ()))
            deps.update(self.rd.get(wr, ()))
        if key is not None:
            key = _canon(key)
        if key is not None and key in self.lastkey:
            deps.add(self.lastkey[key])
        o = _Op()
        o.fn = fn
        o.eng = eng
        o.idx = len(self.ops[eng])
        o.deps = deps
        o.key = key
        o.sig = False
        o.pseudo = False
        o.cnt = 0
        o.val = 0
        if key is not None:
            v = self.keys.get(key, 0) + 16
            self.keys[key] = v
            o.val = v
        self.ops[eng].append(o)
        for wr in writes:
            if multi:
                if self.rd.get(wr):
                    self.lw[wr] = [(o, True)]
                    self.rd[wr] = []
                else:
                    self.lw.setdefault(wr, []).append((o, True))
            else:
                self.lw[wr] = [(o, False)]
                self.rd[wr] = []
        for r in reads:
            self.rd.setdefault(r, []).append(o)
        if key is not None:
            self.lastkey[key] = o
        return o

    def barrier(self, final=False):
        tails = []
        for e in self.ENGS:
            for o in reversed(self.ops[e]):
                if not o.pseudo and o.key is None:
                    tails.append(o)
                    break
        tails += [o for k, o in self.lastkey.items() if final or not k.startswith("bg")]
        for e in self.ENGS:
            self.op(e, lambda eng: None, extra=tails).pseudo = True
        self.lw = {k: v for k, v in self.lw.items() if isinstance(k, str) and k.startswith("bg:")}
        self.rd = {k: v for k, v in self.rd.items() if isinstance(k, str) and k.startswith("bg:")}

    def emit(self):
        nc = self.nc
        for e in self.ENGS:
            for o in self.ops[e]:
                for d in o.deps:
                    if d.key is None and (d.eng != e or (SAME_SYNC and e != "pe")):
                        d.sig = True
        ngen = {}
        for e in self.ENGS:
            c = 0
            for o in self.ops[e]:
                if o.key is None and o.sig:
                    c += 1
                o.cnt = c
            ngen[e] = max(1, (c + GEN - 1) // GEN)
        self.counts = {e: (len(self.ops[e]), self.ops[e][-1].cnt if self.ops[e] else 0) for e in self.ENGS}
        import contextlib
        with contextlib.ExitStack() as st:
            esem = {e: [st.enter_context(nc.semaphore("s_%s_%d" % (e, g))) for g in range(ngen[e])]
                    for e in self.ENGS}
            dsem = {k: st.enter_context(nc.semaphore("d_%d" % i)) for i, k in enumerate(self.keys)}
            block = st.enter_context(nc.Block())

            def run(e, eng):
                seen = {}
                for o in self.ops[e]:
                    waits = {}
                    for d in o.deps:
                        if d.key is not None:
                            sem, v = dsem[d.key], d.val
                        elif d.eng != e or (SAME_SYNC and e != "pe"):
                            g = (d.cnt - 1) // GEN
                            sem, v = esem[d.eng][g], d.cnt - g * GEN
                        else:
                            continue
                        if v > waits.get(sem, (0, None))[0]:
                            waits[sem] = (v, sem)
                    for v, sem in waits.values():
                        if seen.get(sem, 0) >= v:
                            continue
                        seen[sem] = v
                        eng.wait_ge(sem, v)
                    ins = o.fn(eng)
                    if ins is None:
                        continue
                    if o.key is not None:
                        ins.then_inc(dsem[o.key], 16)
                    elif o.sig:
                        g = (o.cnt - 1) // GEN
                        ins.then_inc(esem[e][g], 1)

            @block.tensor
            def _(eng):
                run("pe", eng)

            @block.scalar
            def _(eng):
                run("act", eng)

            @block.vector
            def _(eng):
                run("dve", eng)

            @block.gpsimd
            def _(eng):
                run("pool", eng)

            @block.sync
            def _(eng):
                run("sp", eng)


class Ctx:
    pass


def chunk_stream(c):
    return 0 if c < 2 else 1


def build(nlayers=2, stop=None, dbg=()):
    nc = bass.Bass("TRN2", target_bir_lowering=False)
    P = Prog(nc)
    K = Ctx()
    K.nc, K.P = nc, P
    dt = nc.dram_tensor

    def ext(name, shape, dtype=F32):
        return dt(name, list(shape), dtype, kind="ExternalInput").ap()

    I = {}
    I["xcat"] = ext("xcat", [T, D])
    I["cc"] = ext("cc", [128, 8, 2])
    I["w_ada"] = ext("w_ada", [2, D, 6 * D])
    I["b_ada"] = ext("b_ada", [2, 6 * D])
    I["w_in"] = ext("w_in", [2, D, INC])
    I["b_gate"] = ext("b_gate", [2, 128, 16])
    I["ident"] = ext("ident", [128, 128])
    I["cmat"] = ext("cmat", [128, 7, 128])
    I["cw"] = ext("cw", [2, 128, 5, 24])
    I["cbfm"] = ext("cbfm", [2, 128, 24])
    I["conv_b"] = ext("conv_b", [2, DXBC])
    I["dt_bias"] = ext("dt_bias", [2, 64])
    I["a_log"] = ext("a_log", [2, 64])
    I["d_skip"] = ext("d_skip", [2, 32])
    I["poolP"] = ext("poolP", [3, 128, 31, 512], BF16)
    I["poolC"] = ext("poolC", [128, 8, 256], BF16)
    I["pool_w"] = ext("pool_w", [2, 4, 256, 256])
    I["pscfm"] = ext("pscfm", [2, 128, 8])
    I["gnfm"] = ext("gnfm", [2, 128, 16])
    I["w_branch_a"] = ext("w_branch_a", [2, DI, D])
    I["w_branch_b"] = ext("w_branch_b", [2, D, D])
    I["w_out"] = ext("w_out", [2, D, D])
    I["ln1_g"] = ext("ln1_g", [2, D])
    I["ln1_b"] = ext("ln1_b", [2, D])
    I["ln2_g"] = ext("ln2_g", [2, D])
    I["ln2_b"] = ext("ln2_b", [2, D])
    I["wr"] = ext("wr", [2, 128, 8, 36])
    I["br"] = ext("br", [2, 36])
    I["wgu"] = ext("wgu", [2, NE * 128, 8 * 1024])
    I["wdr"] = ext("wdr", [2, NE * 128, 4 * 1024])
    I["cvec"] = ext("cvec", [128, 197])
    I["sel"] = ext("sel", [2, 2, 128])
    K.I = I
    out = dt("out", [LLAT, D], F32, kind="ExternalOutput").ap()
    K.out = out
    S = {}

    def scr(name, shape, dtype):
        S[name] = dt(name, list(shape), dtype, kind=("ExternalOutput" if name in dbg else "Internal")).ap()
    scr("xres", [T, D], F32)
    scr("modrows", [2, 2, 6 * D], F32)
    scr("sz", [T, DI], BF16)
    scr("xbcT", [DXBC, T], BF16)
    scr("dtraw", [T, 64], F32)
    scr("u", [T, D], BF16)
    scr("gT", [DI, T], BF16)
    scr("xs", [T, DI], BF16)
    scr("Bt", [T, 512], BF16)
    scr("bct", [NCH, 128, 1024], BF16)
    scr("sbin", [NCH, 128, 2048], BF16)
    scr("yaT", [DI, T], BF16)
    scr("ypT", [D, T], BF16)
    scr("x1", [T, D], F32)
    scr("h2T", [D, T], BF16)
    scr("wgub", [2 * NE * 128, 8 * 1024], BF16)
    scr("wdb", [2 * NE * 128, 4 * 1024], BF16)
    scr("cumT", [NCH, 64, 128], F32)
    scr("xin", [NROWS, D], BF16)
    scr("yrows", [NROWS, D], F32)
    if "dest" in dbg:
        S["dest"] = dt("dest", [128, NCH * 2], I32, kind="ExternalOutput").ap()
        S["idxw"] = dt("idxw", [128, NBLK], I32, kind="ExternalOutput").ap()
        S["e12"] = dt("e12", [128, NCH * 2], F32, kind="ExternalOutput").ap()
        S["w12"] = dt("w12", [128, NCH * 2], F32, kind="ExternalOutput").ap()
    K.S = S
    K.dbg = {}

    import contextlib
    with contextlib.ExitStack() as st:
        def sb(name, shape, dtype=F32):
            return st.enter_context(nc.sbuf_tensor("sb_" + name, list(shape), dtype))
        K.sb = sb
        K.ps = [st.enter_context(nc.psum_tensor("ps%d" % i, [128, 512], F32)) for i in range(8)]
        K.ident = sb("ident", [128, 128])
        K.identb = sb("identb", [128, 128], BF16)
        K.sel = sb("sel", [2, 2, 128])
        P.op("sp", lambda e: e.dma_start(out=K.ident[:], in_=I["ident"]), writes=["ident"], key="ident")
        P.op("sp", lambda e: e.dma_start(out=K.sel[:], in_=I["sel"]), writes=["sel"], key="sel")
        P.op("dve", lambda e: e.tensor_copy(out=K.identb[:], in_=K.ident[:]), reads=["ident"], writes=["identb"])
        K.cm = sb("cmat", [128, 7, 128])
        K.cb = sb("cmatb", [128, 7, 128], BF16)
        K.cvec = sb("cvec", [128, 197])
        P.op("sp", lambda e: e.dma_start(out=K.cvec[:], in_=I["cvec"]), writes=["cvec"], key="cvec")
        P.op("sp", lambda e: e.dma_start(out=K.cm[:], in_=I["cmat"]), writes=["cmat"], key="cmat")
        P.op("dve", lambda e: e.tensor_copy(out=K.cb[:], in_=K.cm[:]), reads=["cmat"], writes=["cmatb"])
        for l in range(nlayers):
            layer(K, l, stop)
        P.barrier(final=True)
        P.emit()
    return nc


def phase_mod(K, l, st):
    nc, P, I = K.nc, K.P, K.I
    sb = lambda n, s, d=F32: st.enter_context(nc.sbuf_tensor("sb_" + n, list(s), d))
    cc = sb("cc%d" % l, [128, 8, 2])
    cs = sb("cs%d" % l, [128, 8, 2], BF16)
    bada = sb("bada%d" % l, [2, 6 * D])
    P.op("sp", lambda e: e.dma_start(out=cc[:], in_=I["cc"]), writes=["cc"], key="cc")
    P.op("sp", lambda e: e.dma_start(out=bada[:], in_=I["b_ada"][l].partition_broadcast(2)), writes=["bada"], key="bada")
    P.op("act", lambda e: e.activation(out=cs[:], in_=cc[:], func=AF.Silu), reads=["cc"], writes=["cs"])
    wbuf = [sb("wada%d_%d" % (l, i), [128, 8, 1024], BF16) for i in range(2)]
    rows = sb("modrows%d" % l, [2, 6 * D])
    for blk in range(6):
        wb = wbuf[blk % 2]
        wn = "wada%d" % (blk % 2)
        P.op("pool", lambda e, wb=wb, blk=blk: e.dma_start(
            out=wb[:], in_=I["w_ada"][l, :, blk * 1024:(blk + 1) * 1024].rearrange("(k p) n -> p k n", p=128)),
            writes=[wn], key=wn)
        for hf in range(2):
            ps = K.ps[hf]
            pn = "ps%d" % hf

            def mm(e, wb=wb, hf=hf, ps=ps):
                for k in range(8):
                    ins = e.matmul(ps[0:2, :], cs[:, k, :], wb[:, k, hf * 512:(hf + 1) * 512],
                                   start=(k == 0), stop=(k == 7))
                return ins
            P.op("pe", mm, reads=["cs", wn], writes=[pn])
            c0 = blk * 1024 + hf * 512
            P.op("dve", lambda e, ps=ps, c0=c0: e.tensor_tensor(
                out=rows[0:2, c0:c0 + 512], in0=ps[0:2, :], in1=bada[0:2, c0:c0 + 512], op=ALU.add),
                reads=[pn, "bada"], writes=["modrows"])
    P.op("sp", lambda e: e.dma_start(out=K.S["modrows"][l], in_=rows[:]), reads=["modrows"], writes=["modrows_d"], key="modrows_d")
    modfm = K.modfm
    ps = K.ps[2]

    def tr(e):
        for j in range(48):
            ins = e.matmul(ps[:, 2 * j:2 * j + 2], rows[0:2, j * 128:(j + 1) * 128], K.ident[0:2, 0:2],
                           start=True, stop=True)
        return ins
    P.op("pe", tr, reads=["modrows", "ident"], writes=["ps2"])
    P.op("dve", lambda e: e.tensor_copy(out=modfm[:].rearrange("p j s -> p (j s)"), in_=ps[:, 0:96]),
         reads=["ps2"], writes=["modfm"])
    for a in (8, 32):
        P.op("dve", lambda e, a=a: e.tensor_scalar_add(out=modfm[:, a:a + 8, :], in0=modfm[:, a:a + 8, :], scalar1=1.0),
             reads=["modfm"], writes=["modfm"])


def bcast_rows(K, l, dst, dname, col0, s):
    K.P.op("sp", lambda e: e.dma_start(out=dst[:], in_=K.S["modrows"][l, s, col0:col0 + 1024].partition_broadcast(128)),
           writes=[dname], key="bc_" + dname)


def load_xT(K, st_name, src, c, xt, hT, q, shcol, sccol, want_f32=None):
    P = K.P
    s = chunk_stream(c)
    xn = st_name
    P.op("sp", lambda e: e.dma_start(out=xt[:], in_=src[c * 128:(c + 1) * 128, :]),
         reads=[("x", c)], writes=[xn], key=xn)
    for hf in range(2):
        ps = K.ps[hf]
        pn = "ps%d" % hf

        def tr(e, ps=ps, hf=hf):
            for jj in range(4):
                j = hf * 4 + jj
                ins = e.matmul(ps[:, jj * 128:(jj + 1) * 128], xt[:, j * 128:(j + 1) * 128], K.ident[:],
                               start=True, stop=True)
            return ins
        P.op("pe", tr, reads=[xn, "ident"], writes=[pn])
        for jj in range(4):
            j = hf * 4 + jj
            o = hT[0][:, j, q * 128:(q + 1) * 128]
            sc = K.modfm[:, sccol + j, s:s + 1]
            sh = K.modfm[:, shcol + j, s:s + 1]
            if hf == 0:
                P.op("act", lambda e, ps=ps, jj=jj, o=o, sc=sc, sh=sh: e.activation(
                    out=o, in_=ps[:, jj * 128:(jj + 1) * 128], func=AF.Identity, bias=sh, scale=sc),
                    reads=[pn, "modfm"], writes=[hT[1]])
            else:
                P.op("dve", lambda e, ps=ps, jj=jj, o=o, sc=sc, sh=sh: e.tensor_scalar(
                    out=o, in0=ps[:, jj * 128:(jj + 1) * 128], scalar1=sc, scalar2=sh, op0=ALU.mult, op1=ALU.add),
                    reads=[pn, "modfm"], writes=[hT[1]])
            if want_f32 is not None:
                o2 = want_f32[0][:, j, q * 128:(q + 1) * 128]
                P.op("dve" if jj % 2 == 0 else "act",
                     (lambda e, ps=ps, jj=jj, o2=o2, sc=sc, sh=sh: e.tensor_scalar(
                         out=o2, in0=ps[:, jj * 128:(jj + 1) * 128], scalar1=sc, scalar2=sh, op0=ALU.mult, op1=ALU.add))
                     if jj % 2 == 0 else
                     (lambda e, ps=ps, jj=jj, o2=o2, sc=sc, sh=sh: e.activation(
                         out=o2, in_=ps[:, jj * 128:(jj + 1) * 128], func=AF.Identity, bias=sh, scale=sc)),
                     reads=[pn, "modfm"], writes=[want_f32[1]])


def groups():
    g = [(0, 2)]
    for k in range(16):
        g.append((2 + 4 * k, 4))
    return g


def pass_inproj(K, l, src, sub):
    nc, P, I, S = K.nc, K.P, K.I, K.S
    import contextlib
    with contextlib.ExitStack() as st:
        sb = lambda n, s, d=F32: st.enter_context(nc.sbuf_tensor("sb_" + n, list(s), d))
        if sub == 0:
            col0, ncol = 0, 5120
        else:
            col0, ncol = 5120, 3136
        tg = "A%d_%d_" % (l, sub)
        w = sb(tg + "w", [128, 8, ncol], BF16)
        piece = 640 if sub == 0 else 784
        for pi in range(ncol // piece):
            P.op("pool", lambda e, pi=pi: e.dma_start(
                out=w[:, :, pi * piece:(pi + 1) * piece],
                in_=I["w_in"][l, :, col0 + pi * piece:col0 + (pi + 1) * piece].rearrange("(k p) n -> p k n", p=128)),
                writes=[tg + "w"], key="wld%d" % (pi % 4), multi=True)
        xts = [sb(tg + "x%d" % i, [128, D]) for i in range(2)]
        hTs = [sb(tg + "h%d" % i, [128, 8, 512], BF16) for i in range(2)]
        if sub == 1:
            bg = sb(tg + "bg", [128, 16])
            P.op("sp", lambda e: e.dma_start(out=bg[:], in_=I["b_gate"][l]), writes=[tg + "bg"], key=tg + "bg")
        stg_tm = [sb(tg + "tm%d" % i, [128, 2048], BF16) for i in range(2)]
        stg_fm = [sb(tg + "fm%d" % i, [128, 8, 512], BF16) for i in range(2)]
        stg_dt = [sb(tg + "dt%d" % i, [128, 64]) for i in range(2)]
        psn = [2, 3, 4, 5, 6, 7]
        pscnt = [0]

        def nextps():
            i = psn[pscnt[0] % len(psn)]
            pscnt[0] += 1
            return K.ps[i], "ps%d" % i
        xi = 0
        tmi = 0
        fmi = 0
        for gi, (c0, ncg) in enumerate(groups()):
            hT = hTs[gi % 2]
            hn = tg + "h%d" % (gi % 2)
            ntok = ncg * 128
            t0 = c0 * 128
            for q in range(ncg):
                load_xT(K, tg + "x%d" % (xi % 2), src, c0 + q, xts[xi % 2], (hT, hn), q, 0, 8)
                xi += 1
            for q in range(ncg):
                c = c0 + q
                if sub == 0:
                    blocks = [(C_Z + b * 512, 512) for b in range(4)]
                else:
                    blocks = [(C_DT, 64), (C_U, 512), (C_U + 512, 512)]
                stm = stg_tm[tmi % 2]
                stn = tg + "tm%d" % (tmi % 2)
                sdt = stg_dt[tmi % 2]
                sdn = tg + "dt%d" % (tmi % 2)
                tmi += 1
                for bi, (cb, nb) in enumerate(blocks):
                    ps, pn = nextps()

                    def mm(e, ps=ps, q=q, cb=cb, nb=nb, hT=hT):
                        for k in range(8):
                            ins = e.matmul(ps[:, 0:nb], hT[:, k, q * 128:(q + 1) * 128], w[:, k, cb - col0:cb - col0 + nb],
                                           start=(k == 0), stop=(k == 7))
                        return ins
                    P.op("pe", mm, reads=[hn, tg + "w"], writes=[pn])
                    if sub == 0:
                        P.op("act", lambda e, ps=ps, bi=bi, stm=stm: e.activation(
                            out=stm[:, bi * 512:(bi + 1) * 512], in_=ps[:], func=AF.Silu),
                            reads=[pn], writes=[stn])
                    elif nb == 64:
                        P.op("dve", lambda e, ps=ps, sdt=sdt: e.tensor_copy(out=sdt[:], in_=ps[:, 0:64]),
                             reads=[pn], writes=[sdn])
                    else:
                        P.op("dve", lambda e, ps=ps, bi=bi, stm=stm: e.tensor_copy(
                            out=stm[:, (bi - 1) * 512:bi * 512], in_=ps[:]),
                            reads=[pn], writes=[stn])
                if sub == 0:
                    P.op("sp", lambda e, stm=stm, c=c: e.dma_start(out=S["sz"][c * 128:(c + 1) * 128, :], in_=stm[:]),
                         reads=[stn], writes=[("sz", c)], key="st_" + stn)
                else:
                    P.op("sp", lambda e, stm=stm, c=c: e.dma_start(out=S["u"][c * 128:(c + 1) * 128, :], in_=stm[:, 0:1024]),
                         reads=[stn], writes=[("u", c)], key="st_" + stn)
                    P.op("sp", lambda e, sdt=sdt, c=c: e.dma_start(out=S["dtraw"][c * 128:(c + 1) * 128, :], in_=sdt[:]),
                         reads=[sdn], writes=[("dtraw", c)], key="st_" + sdn)
            if sub == 0:
                fcol, nfc, dst, dname = C_XBC, 24, S["xbcT"], "xbcT"
            else:
                fcol, nfc, dst, dname = C_G, 16, S["gT"], "gT"
            for m0 in range(0, nfc, 8):
                sfm = stg_fm[fmi % 2]
                sfn = tg + "fm%d" % (fmi % 2)
                fmi += 1
                for mm_ in range(8):
                    m = m0 + mm_
                    ps, pn = nextps()
                    cb = fcol + m * 128 - col0

                    def mm(e, ps=ps, cb=cb, hT=hT, ntok=ntok):
                        for k in range(8):
                            ins = e.matmul(ps[:, 0:ntok], w[:, k, cb:cb + 128], hT[:, k, 0:ntok],
                                           start=(k == 0), stop=(k == 7))
                        return ins
                    P.op("pe", mm, reads=[hn, tg + "w"], writes=[pn])
                    if sub == 0:
                        eng = "dve" if mm_ % 2 == 0 else "act"
                        if eng == "dve":
                            P.op("dve", lambda e, ps=ps, mm_=mm_, sfm=sfm, ntok=ntok: e.tensor_copy(
                                out=sfm[:, mm_, 0:ntok], in_=ps[:, 0:ntok]), reads=[pn], writes=[sfn])
                        else:
                            P.op("act", lambda e, ps=ps, mm_=mm_, sfm=sfm, ntok=ntok: e.copy(
                                out=sfm[:, mm_, 0:ntok], in_=ps[:, 0:ntok]), reads=[pn], writes=[sfn])
                    else:
                        P.op("act", lambda e, ps=ps, mm_=mm_, sfm=sfm, ntok=ntok, m=m: e.activation(
                            out=sfm[:, mm_, 0:ntok], in_=ps[:, 0:ntok], func=AF.Sigmoid, bias=bg[:, m:m + 1], scale=1.0),
                            reads=[pn, tg + "bg"], writes=[sfn])
                P.op("sp", lambda e, sfm=sfm, m0=m0, t0=t0, ntok=ntok, dst=dst: e.dma_start(
                    out=dst[m0 * 128:(m0 + 8) * 128, t0:t0 + ntok].rearrange("(m p) t -> p m t", p=128),
                    in_=sfm[:, :, 0:ntok]),
                    reads=[sfn], writes=[(dname, gi, m0)], key="st_" + sfn)


def layer_consts(K, l, st):
    nc, P, I = K.nc, K.P, K.I
    sb = lambda n, s, d=F32: st.enter_context(nc.sbuf_tensor("sb_" + n, list(s), d))
    L = Ctx()
    L.cw = sb("cw%d" % l, [128, 5, 24])
    L.cbfm = sb("cbfm%d" % l, [128, 24])
    L.cbrow32 = sb("cbrow32_%d" % l, [1, DXBC])
    L.cbrow = sb("cbrow%d" % l, [1, DXBC], BF16)
    L.onesrow = sb("onesrow%d" % l, [1, 128], BF16)
    L.dtb = sb("dtb%d" % l, [128, 64])
    L.negA = sb("negA%d" % l, [128, 64])
    L.dskd = sb("dskd%d" % l, [128, 32, 128], BF16)
    L.dsk = sb("dsk%d" % l, [128, 32])
    P.op("sp", lambda e: e.dma_start(out=L.cw[:], in_=I["cw"][l]), writes=["cw"], key="c1")
    P.op("sp", lambda e: e.dma_start(out=L.cbfm[:], in_=I["cbfm"][l]), writes=["cbfm"], key="c2")
    P.op("sp", lambda e: e.dma_start(out=L.cbrow32[:], in_=I["conv_b"][l:l + 1, :]), writes=["cbrow32"], key="c3")
    P.op("sp", lambda e: e.dma_start(out=L.dtb[:], in_=I["dt_bias"][l].partition_broadcast(128)), writes=["dtb"], key="c4")
    P.op("sp", lambda e: e.dma_start(out=L.negA[:], in_=I["a_log"][l].partition_broadcast(128)), writes=["negA"], key="c5")
    P.op("sp", lambda e: e.dma_start(out=L.dsk[:], in_=I["d_skip"][l].partition_broadcast(128)), writes=["dsk"], key="c6")
    P.op("dve", lambda e: e.tensor_copy(out=L.cbrow[:], in_=L.cbrow32[:]), reads=["cbrow32"], writes=["cbrow"])
    P.op("dve", lambda e: e.memset(L.onesrow[:], 1.0), writes=["onesrow"])
    P.op("act", lambda e: e.activation(out=L.negA[:], in_=L.negA[:], func=AF.Exp), reads=["negA"], writes=["negA"])
    P.op("dve", lambda e: e.tensor_scalar_mul(out=L.negA[:], in0=L.negA[:], scalar1=-1.0), reads=["negA"], writes=["negA"])
    P.op("dve", lambda e: e.tensor_tensor(out=L.dskd[:], in0=K.cm[:, 0, :].unsqueeze(1).to_broadcast([128, 32, 128]),
                                          in1=L.dsk[:].unsqueeze(2).to_broadcast([128, 32, 128]), op=ALU.mult),
         reads=["cmat", "dsk"], writes=["dskd"])
    return L


def dt_prep(K, L, c, R, tag, cumT=None):
    P, S = K.P, K.S
    n = lambda x: tag + x
    P.op("sp", lambda e: e.dma_start(out=R["raw"][:], in_=S["dtraw"][c * 128:(c + 1) * 128, :]),
         writes=[n("raw")], key=n("raw"))
    P.op("dve", lambda e: e.tensor_tensor(out=R["v"][:], in0=R["raw"][:], in1=L.dtb[:], op=ALU.add),
         reads=[n("raw"), "dtb"], writes=[n("v")])
    P.op("dve", lambda e: e.scalar_tensor_tensor(out=R["t"][:], in0=R["v"][:], scalar=-1.0, in1=R["v"][:],
                                                 op0=ALU.mult, op1=ALU.max),
         reads=[n("v")], writes=[n("t")])
    P.op("act", lambda e: e.activation(out=R["t"][:], in_=R["t"][:], func=AF.Exp, scale=-1.0),
         reads=[n("t")], writes=[n("t")])
    P.op("act", lambda e: e.activation(out=R["t"][:], in_=R["t"][:], func=AF.Ln, bias=1.0, scale=1.0),
         reads=[n("t")], writes=[n("t")])
    P.op("dve", lambda e: e.scalar_tensor_tensor(out=R["dt"][:], in0=R["v"][:], scalar=0.0, in1=R["t"][:],
                                                 op0=ALU.max, op1=ALU.add),
         reads=[n("v"), n("t")], writes=[n("dt")])
    P.op("dve", lambda e: e.tensor_tensor(out=R["a"][:], in0=R["dt"][:], in1=L.negA[:], op=ALU.mult),
         reads=[n("dt"), "negA"], writes=[n("a")])
    P.op("dve", lambda e: e.tensor_copy(out=R["ahl"][:, 0, :], in_=R["a"][:]), reads=[n("a")], writes=[n("ahi")])
    P.op("dve", lambda e: e.tensor_tensor(out=R["ahl"][:, 1, :], in0=R["a"][:], in1=R["ahl"][:, 0, :], op=ALU.subtract),
         reads=[n("a"), n("ahi")], writes=[n("alo")])
    ps = K.ps[7]

    def mm(e):
        e.matmul(ps[:, 0:32], K.cm[:, 1, :], R["a"][:, 0:32], start=True, stop=True)
        e.matmul(ps[:, 32:64], K.cm[:, 2, :], R["a"][:, 32:64], start=True, stop=True)
        return e.matmul(ps[:, 64:128], K.cm[:, 3, :], R["a"][:], start=True, stop=True)
    P.op("pe", mm, reads=[n("a"), "cmat"], writes=["ps7"])
    P.op("act", lambda e: e.copy(out=R["cum"][:], in_=ps[:, 0:64]), reads=["ps7"], writes=[n("cum")])
    P.op("act", lambda e: e.copy(out=R["tot"][:], in_=ps[:, 64:128]), reads=["ps7"], writes=[n("tot")])
    P.op("act", lambda e: e.activation(out=R["dec"][:], in_=R["tot"][:], func=AF.Exp), reads=[n("tot")], writes=[n("dec")])
    P.op("dve", lambda e: e.tensor_tensor(out=R["tail"][:], in0=R["tot"][:], in1=R["cum"][:], op=ALU.subtract),
         reads=[n("tot"), n("cum")], writes=[n("tail")])
    P.op("act", lambda e: e.activation(out=R["tail"][:], in_=R["tail"][:], func=AF.Exp), reads=[n("tail")], writes=[n("tail")])
    P.op("dve", lambda e: e.tensor_tensor(out=R["tail"][:], in0=R["tail"][:], in1=R["dt"][:], op=ALU.mult),
         reads=[n("tail"), n("dt")], writes=[n("tail")])
    P.op("act", lambda e: e.activation(out=R["e"][:], in_=R["cum"][:], func=AF.Exp), reads=[n("cum")], writes=[n("e")])
    if cumT is not None:
        ct, ctn = cumT
        P.op("pe", lambda e: e.matmul(ps[0:64, 128:256], R["cum"][:], K.cm[:, 0, :], start=True, stop=True),
             reads=[n("cum"), "cmat"], writes=["ps7"])
        P.op("act", lambda e: e.copy(out=ct[:], in_=ps[0:64, 128:256]), reads=["ps7"], writes=[ctn])
        P.op("act", lambda e: e.dma_start(out=K.S["cumT"][c], in_=ct[:]), reads=[ctn], writes=[("cumT", c)], key="st_" + ctn)
    P.op("act", lambda e: e.activation(out=R["lnb"][:], in_=R["dt"][:], func=AF.Ln), reads=[n("dt")], writes=[n("lnb")])
    P.op("dve", lambda e: e.tensor_tensor(out=R["lnb"][:], in0=R["lnb"][:], in1=R["cum"][:], op=ALU.subtract),
         reads=[n("lnb"), n("cum")], writes=[n("lnb")])


def alloc_prep(K, st, tag):
    nc = K.nc
    R = {}
    for nm in ("raw", "v", "t", "dt", "a", "cum", "dec", "tail", "e", "lnb", "tot"):
        R[nm] = st.enter_context(nc.sbuf_tensor("sb_" + tag + nm, [128, 64], F32))
    R["ahl"] = st.enter_context(nc.sbuf_tensor("sb_" + tag + "ahl", [128, 2, 64], BF16))
    return R


def pass_conv_bwd(K, l, L):
    nc, P, I, S = K.nc, K.P, K.I, K.S
    import contextlib
    with contextlib.ExitStack() as st:
        sb = lambda n, s, d=F32: st.enter_context(nc.sbuf_tensor("sb_" + n, list(s), d))
        tg = "B%d_" % l
        L.diagw = sb(tg + "diagw", [128, 24, 5, 128], BF16)
        for hf_, eng_ in ((0, "dve"), (1, "dve")):
            P.op(eng_, lambda e, hf_=hf_: e.tensor_tensor(
                out=L.diagw[:, hf_ * 12:(hf_ + 1) * 12],
                in0=K.cm[:, 0, :].unsqueeze(1).unsqueeze(1).to_broadcast([128, 12, 5, 128]),
                in1=L.cw[:].rearrange("p k m -> p m k")[:, hf_ * 12:(hf_ + 1) * 12, :].unsqueeze(3).to_broadcast([128, 12, 5, 128]),
                op=ALU.mult), reads=["cmat", "cw"], writes=["diagw"], multi=True)
        raws = [sb(tg + "raw%d" % i, [128, 24, 516], BF16) for i in range(2)]
        xs = [sb(tg + "xs%d" % i, [128, 2048], BF16) for i in range(2)]
        bt = [sb(tg + "bt%d" % i, [128, 512], BF16) for i in range(2)]
        bct = [sb(tg + "bct%d" % i, [128, 1024], BF16) for i in range(2)]
        xdt = [sb(tg + "xdt%d" % i, [128, 2048], BF16) for i in range(2)]
        state = sb(tg + "state", [128, 2048])
        tmp = sb(tg + "tmp", [128, 2048])
        sbo = [sb(tg + "sbo%d" % i, [128, 2048], BF16) for i in range(2)]
        Rs = [alloc_prep(K, st, tg + "p%d" % i) for i in range(2)]
        P.op("pool", lambda e: e.memset(state[:], 0.0), writes=[tg + "state"])
        gl = groups()
        order = [0] + list(range(16, 0, -1))
        it = 0
        for gi_i, gi in enumerate(order):
            c0, ncg = gl[gi]
            ntok = ncg * 128
            t0 = c0 * 128
            raw = raws[gi_i % 2]
            rn = tg + "raw%d" % (gi_i % 2)
            left_ok = gi not in (0, 1)
            right_ok = gi not in (0, 16)
            lo = t0 - 2 if left_ok else t0
            hi = t0 + ntok + 2 if right_ok else t0 + ntok
            if not left_ok:
                P.op("pool", lambda e, raw=raw: e.memset(raw[:, :, 0:2], 0.0), writes=[rn])
            if not right_ok:
                P.op("pool", lambda e, raw=raw, ntok=ntok: e.memset(raw[:, :, ntok + 2:ntok + 4], 0.0), writes=[rn],
                     multi=left_ok is False)
            for m0 in range(0, 24, 8):
                P.op("sp", lambda e, raw=raw, lo=lo, hi=hi, t0=t0, m0=m0: e.dma_start(
                    out=raw[:, m0:m0 + 8, lo - (t0 - 2):hi - (t0 - 2)],
                    in_=S["xbcT"][m0 * 128:(m0 + 8) * 128, lo:hi].rearrange("(m p) t -> p m t", p=128)),
                    writes=[rn], key=tg + "raw%d" % (m0 // 8), multi=True)
            for q in range(ncg - 1, -1, -1):
                c = c0 + q
                o = q * 128
                sl = it % 2
                if it < NE:
                    i_ = l * NE + it
                    P.op("pool", lambda e, i_=i_: e.dma_start(out=S["wgub"][i_ * 128:(i_ + 1) * 128, :],
                                                             in_=I["wgu"].rearrange("l r n -> (l r) n")[i_ * 128:(i_ + 1) * 128, :]),
                         writes=["bg:w%d" % l], key="bg%d" % (it % 4), multi=True)
                    P.op("pool", lambda e, i_=i_: e.dma_start(out=S["wdb"][i_ * 128:(i_ + 1) * 128, :],
                                                             in_=I["wdr"].rearrange("l r n -> (l r) n")[i_ * 128:(i_ + 1) * 128, :]),
                         writes=["bg:w%d" % l], key="bg%d" % (it % 4), multi=True)
                it += 1
                xs_t, bt_t, bct_t, xdt_t, R = xs[sl], bt[sl], bct[sl], xdt[sl], Rs[sl]
                xn, bn, cn, dn = tg + "xs%d" % sl, tg + "bt%d" % sl, tg + "bct%d" % sl, tg + "xdt%d" % sl
                pt = tg + "p%d" % sl
                dt_prep(K, L, c, R, pt)
                for b4 in range(5):
                    ps = K.ps[b4 % 4]
                    pn = "ps%d" % (b4 % 4)

                    def mm(e, ps=ps, b4=b4, raw=raw, o=o):
                        for bb in range(4):
                            m = b4 * 4 + bb
                            for k in range(5):
                                e.matmul(ps[:, bb * 128:(bb + 1) * 128], raw[:, m, o + k:o + k + 128], L.diagw[:, m, k, :],
                                         start=(k == 0), stop=False)
                            ins = e.matmul(ps[:, bb * 128:(bb + 1) * 128], L.onesrow[0:1, :], L.cbrow[0:1, m * 128:(m + 1) * 128],
                                           start=False, stop=True)
                        return ins
                    P.op("pe", mm, reads=[rn, "diagw", "cbrow", "onesrow"], writes=[pn])
                    if b4 < 4:
                        P.op("act", lambda e, ps=ps, b4=b4, xs_t=xs_t: e.activation(
                            out=xs_t[:, b4 * 512:(b4 + 1) * 512], in_=ps[:], func=AF.Silu), reads=[pn], writes=[xn])
                    else:
                        P.op("act", lambda e, ps=ps, bt_t=bt_t: e.activation(out=bt_t[:], in_=ps[:], func=AF.Silu),
                             reads=[pn], writes=[bn])
                for b4 in range(2):
                    ps = K.ps[4 + b4]
                    pn = "ps%d" % (4 + b4)

                    def mm2(e, ps=ps, b4=b4, raw=raw, o=o):
                        for bb in range(4):
                            m = 16 + b4 * 4 + bb
                            for k in range(5):
                                ins = e.matmul(ps[:, bb * 128:(bb + 1) * 128], L.diagw[:, m, k, :], raw[:, m, o + k:o + k + 128],
                                               start=(k == 0), stop=(k == 4))
                        return ins
                    P.op("pe", mm2, reads=[rn, "diagw"], writes=[pn])
                    for bb in range(4):
                        m = 16 + b4 * 4 + bb
                        P.op("act", lambda e, ps=ps, bb=bb, m=m, b4=b4, bct_t=bct_t: e.activation(
                            out=bct_t[:, (b4 * 4 + bb) * 128:(b4 * 4 + bb + 1) * 128], in_=ps[:, bb * 128:(bb + 1) * 128],
                            func=AF.Silu, bias=L.cbfm[:, m:m + 1], scale=1.0), reads=[pn, "cbfm"], writes=[cn])
                P.op("sp", lambda e, xs_t=xs_t, c=c: e.dma_start(out=S["xs"][c * 128:(c + 1) * 128, :], in_=xs_t[:]),
                     reads=[xn], key="st_" + xn)
                P.op("sp", lambda e, bt_t=bt_t, c=c: e.dma_start(out=S["Bt"][c * 128:(c + 1) * 128, :], in_=bt_t[:]),
                     reads=[bn], key="st_" + bn)
                P.op("sp", lambda e, bct_t=bct_t, c=c: e.dma_start(out=S["bct"][c], in_=bct_t[:]),
                     reads=[cn], key="st_" + cn)
                so = sbo[sl]
                son = tg + "sbo%d" % sl
                P.op("act", lambda e, so=so: e.copy(out=so[:], in_=state[:]), reads=[tg + "state"], writes=[son])
                P.op("sp", lambda e, so=so, c=c: e.dma_start(out=S["sbin"][c], in_=so[:]), reads=[son], key="st_" + son)
                P.op("dve", lambda e, xs_t=xs_t, xdt_t=xdt_t, R=R: e.tensor_tensor(
                    out=xdt_t[:].rearrange("p (h d) -> p h d", h=32), in0=xs_t[:].rearrange("p (h d) -> p h d", h=32),
                    in1=R["tail"][:, 32:64].unsqueeze(2).to_broadcast([128, 32, 64]), op=ALU.mult),
                    reads=[xn, pt + "tail"], writes=[dn])
                for g in range(4):
                    ps = K.ps[6]

                    def mm3(e, ps=ps, g=g, bt_t=bt_t, xdt_t=xdt_t):
                        return e.matmul(ps[:], bt_t[:, g * 128:(g + 1) * 128], xdt_t[:, g * 512:(g + 1) * 512],
                                        start=True, stop=True)
                    P.op("pe", mm3, reads=[bn, dn], writes=["ps6"])
                    P.op("pool", lambda e, g=g, R=R: e.tensor_tensor(
                        out=tmp[:, g * 512:(g + 1) * 512].rearrange("p (h d) -> p h d", h=8),
                        in0=state[:, g * 512:(g + 1) * 512].rearrange("p (h d) -> p h d", h=8),
                        in1=R["dec"][:, 32 + g * 8:32 + (g + 1) * 8].unsqueeze(2).to_broadcast([128, 8, 64]), op=ALU.mult),
                        reads=[tg + "state", pt + "dec"], writes=[tg + "tmp"])
                    P.op("dve", lambda e, g=g, ps=ps: e.tensor_tensor(
                        out=state[:, g * 512:(g + 1) * 512], in0=ps[:], in1=tmp[:, g * 512:(g + 1) * 512], op=ALU.add),
                        reads=["ps6", tg + "tmp"], writes=[tg + "state"])


def pass_ssd_fwd(K, l, L):
    nc, P, I, S = K.nc, K.P, K.I, K.S
    import contextlib
    with contextlib.ExitStack() as st:
        sb = lambda n, s, d=F32: st.enter_context(nc.sbuf_tensor("sb_" + n, list(s), d))
        tg = "C%d_" % l
        xs = [sb(tg + "xs%d" % i, [128, 2048], BF16) for i in range(2)]
        bt = [sb(tg + "bt%d" % i, [128, 512], BF16) for i in range(2)]
        bct = [sb(tg + "bct%d" % i, [128, 1024], BF16) for i in range(2)]
        sbi = [sb(tg + "sbi%d" % i, [128, 2048], BF16) for i in range(2)]
        szt = [sb(tg + "sz%d" % i, [128, 2048], BF16) for i in range(2)]
        Rs = [alloc_prep(K, st, tg + "p%d" % i) for i in range(2)]
        Wd = [[sb(tg + "W%d_%d" % (d, i), [128, 8, 128], BF16) for i in range(2)] for d in range(2)]
        Wsum = [sb(tg + "Ws%d" % i, [128, 8, 128], BF16) for i in range(2)]
        Mt = [sb(tg + "M%d" % i, [128, 8, 128], BF16) for i in range(2)]
        t1 = [sb(tg + "t1_%d" % i, [128, 512]) for i in range(2)]
        t2 = [sb(tg + "t2_%d" % i, [128, 512]) for i in range(2)]
        yz = [sb(tg + "yz%d" % i, [128, 2048]) for i in range(2)]
        junk = sb(tg + "junk", [128, 512], BF16)
        yn = sb(tg + "yn", [128, 2048], BF16)
        ss = [sb(tg + "ss%d" % i, [128, 4]) for i in range(2)]
        xdtf = [sb(tg + "xdtf%d" % i, [128, 2048], BF16) for i in range(2)]
        state = sb(tg + "state", [128, 2048])
        sfb = sb(tg + "sfb", [128, 2048], BF16)
        tmp = [sb(tg + "tmp%d" % i, [128, 512]) for i in range(2)]
        yaT = [sb(tg + "yaT%d" % i, [128, 16, 128], BF16) for i in range(2)]
        cumTs = [sb(tg + "cumT%d" % i, [64, 128]) for i in range(2)]
        rowbc = [sb(tg + "rb%d" % i, [128, 2, 8, 128]) for i in range(4)]
        P.op("pool", lambda e: e.memset(state[:], 0.0), writes=[tg + "state"])
        P.op("pool", lambda e: e.memset(sfb[:], 0.0), writes=[tg + "sfb"])
        ps_cb = [K.ps[0], K.ps[0]]
        ps_y = [K.ps[3], K.ps[4]]
        ps_of, ps_ob, ps_s = K.ps[5], K.ps[6], K.ps[7]

        def names(c):
            sl = c % 2
            return dict(sl=sl, xs=xs[sl], bt=bt[sl], bct=bct[sl], sbi=sbi[sl], sz=szt[sl], R=Rs[sl],
                        xn=tg + "xs%d" % sl, bn=tg + "bt%d" % sl, cn=tg + "bct%d" % sl, sn=tg + "sbi%d" % sl,
                        zn=tg + "sz%d" % sl, pt=tg + "p%d" % sl)

        def load(c):
            N = names(c)
            P.op("sp", lambda e: e.dma_start(out=N["xs"][:], in_=S["xs"][c * 128:(c + 1) * 128, :]), writes=[N["xn"]], key=N["xn"])
            P.op("sp", lambda e: e.dma_start(out=N["bt"][:], in_=S["Bt"][c * 128:(c + 1) * 128, :]), writes=[N["bn"]], key=N["bn"])
            P.op("sp", lambda e: e.dma_start(out=N["bct"][:], in_=S["bct"][c]), writes=[N["cn"]], key=N["cn"])
            P.op("sp", lambda e: e.dma_start(out=N["sbi"][:], in_=S["sbin"][c]), writes=[N["sn"]], key=N["sn"])
            P.op("sp", lambda e: e.dma_start(out=N["sz"][:], in_=S["sz"][c * 128:(c + 1) * 128, :]), writes=[N["zn"]], key=N["zn"])
            dt_prep(K, L, c, N["R"], N["pt"], cumT=(cumTs[c % 2], tg + "cumT%d" % (c % 2)))

        def rb_load(c, g):
            ri = (c * 4 + g) % 4
            rb, rbn = rowbc[ri], tg + "rb%d" % ri
            flat = S["cumT"][c].rearrange("h t -> (h t)")
            for d in range(2):
                o_ = (d * 32 + g * 8) * 128
                P.op("sp", lambda e, d=d, o_=o_: e.dma_start(out=rb[:, d].rearrange("p h t -> p (h t)"),
                                                            in_=flat[o_:o_ + 1024].partition_broadcast(128)),
                     reads=[("cumT", c)], writes=[rbn], key=rbn + "_%d" % d, multi=True)

        def stage_a(c, g):
            N = names(c)
            R, pt = N["R"], N["pt"]
            wi = (c * 4 + g) % 2
            ri = (c * 4 + g) % 4
            rb, rbn = rowbc[ri], tg + "rb%d" % ri
            for d in range(2):
                P.op("dve", lambda e, d=d: e.tensor_tensor(out=rb[:, d], in0=rb[:, d],
                                                          in1=K.cm[:, 4 + d, :].unsqueeze(1).to_broadcast([128, 8, 128]), op=ALU.add),
                     reads=[rbn, "cmat"], writes=[rbn])

        def stage_a2(c, g):
            N = names(c)
            R, pt = N["R"], N["pt"]
            wi = (c * 4 + g) % 2
            ri = (c * 4 + g) % 4
            rb, rbn = rowbc[ri], tg + "rb%d" % ri
            for d in range(2):
                W = Wd[d][wi]
                wn = tg + "W%d_%d" % (d, wi)
                for hh in range(8):
                    col = d * 32 + g * 8 + hh
                    P.op("act", lambda e, d=d, hh=hh, col=col, W=W: e.activation(
                        out=W[:, hh, :], in_=rb[:, d, hh, :], func=AF.Exp, bias=R["lnb"][:, col:col + 1], scale=1.0),
                        reads=[rbn, pt + "lnb"], writes=[wn], multi=True)

        def chunk_head(c):
            N = names(c)
            bct_t = N["bct"]

            pcb = K.ps[c % 2]

            def mmcb(e):
                for g in range(4):
                    ins = e.matmul(pcb[:, g * 128:(g + 1) * 128], bct_t[:, g * 128:(g + 1) * 128],
                                   bct_t[:, (4 + g) * 128:(5 + g) * 128], start=True, stop=True)
                return ins
            P.op("pe", mmcb, reads=[N["cn"]], writes=["ps%d" % (c % 2)])
            xd = xdtf[c % 2]
            P.op("pool", lambda e: e.tensor_tensor(
                out=xd[:].rearrange("p (h d) -> p h d", h=32), in0=N["xs"][:].rearrange("p (h d) -> p h d", h=32),
                in1=N["R"]["tail"][:, 0:32].unsqueeze(2).to_broadcast([128, 32, 64]), op=ALU.mult),
                reads=[N["xn"], N["pt"] + "tail"], writes=[tg + "xdtf%d" % (c % 2)])

        def stage_b(c, g, part):
            N = names(c)
            R, pt, xs_t, bt_t, bct_t, sbi_t, sz_t = N["R"], N["pt"], N["xs"], N["bt"], N["bct"], N["sbi"], N["sz"]
            xn, bn, cn, sn, zn = N["xn"], N["bn"], N["cn"], N["sn"], N["zn"]
            wi = (c * 4 + g) % 2
            M, mn = Mt[wi], tg + "M%d" % wi
            Ws, wsn = Wsum[wi], tg + "Ws%d" % wi
            py, pyn = ps_y[wi], "ps%d" % (3 + wi)
            a1, n1 = t1[wi], tg + "t1_%d" % wi
            a2, n2 = t2[wi], tg + "t2_%d" % wi
            yz_t, yzn = yz[c % 2], tg + "yz%d" % (c % 2)
            ss_t, ssn = ss[c % 2], tg + "ss%d" % (c % 2)
            xd, xdn = xdtf[c % 2], tg + "xdtf%d" % (c % 2)
            tm, tmn = tmp[wi], tg + "tmp%d" % wi
            if part == 2:
              P.op("pool", lambda e: e.tensor_tensor(out=Ws[:], in0=Wd[0][wi][:], in1=Wd[1][wi][:], op=ALU.add),
                 reads=[tg + "W0_%d" % wi, tg + "W1_%d" % wi], writes=[wsn])
              P.op("dve", lambda e: e.tensor_tensor(
                out=M[:], in0=Ws[:], in1=K.ps[c % 2][:, g * 128:(g + 1) * 128].unsqueeze(1).to_broadcast([128, 8, 128]),
                op=ALU.mult), reads=[wsn, "ps%d" % (c % 2)], writes=[mn])

            def mmy(e):
                for hh in range(8):
                    h = g * 8 + hh
                    e.matmul(py[:, hh * 64:(hh + 1) * 64], M[:, hh, :], xs_t[:, h * 64:(h + 1) * 64], start=True, stop=False)
                    ins = e.matmul(py[:, hh * 64:(hh + 1) * 64], L.dskd[:, h, :], xs_t[:, h * 64:(h + 1) * 64], start=False, stop=True)
                return ins
            if part == 3:
                P.op("pe", mmy, reads=[mn, xn, "dskd"], writes=[pyn])
            if part != 1:
                pass
            if part == 1:
              if True:
                  P.op("pe", lambda e: e.matmul(ps_of[:], bct_t[:, (4 + g) * 128:(5 + g) * 128], sfb[:, g * 512:(g + 1) * 512], start=True, stop=True),
                       reads=[cn, tg + "sfb"], writes=["ps5"])
                  P.op("pe", lambda e: e.matmul(ps_ob[:], bct_t[:, (4 + g) * 128:(5 + g) * 128], sbi_t[:, g * 512:(g + 1) * 512], start=True, stop=True),
                       reads=[cn, sn], writes=["ps6"])
                  P.op("pe", lambda e: e.matmul(ps_s[:], bt_t[:, g * 128:(g + 1) * 128], xd[:, g * 512:(g + 1) * 512], start=True, stop=True),
                       reads=[bn, xdn], writes=["ps7"])
            if part == 4:
                P.op("dve", lambda e: e.tensor_tensor(
                    out=a1[:].rearrange("p (h d) -> p h d", h=8), in0=ps_of[:].rearrange("p (h d) -> p h d", h=8),
                    in1=R["e"][:, g * 8:(g + 1) * 8].unsqueeze(2).to_broadcast([128, 8, 64]), op=ALU.mult),
                    reads=["ps5", pt + "e"], writes=[n1])
                P.op("dve", lambda e: e.tensor_tensor(
                    out=a2[:].rearrange("p (h d) -> p h d", h=8), in0=ps_ob[:].rearrange("p (h d) -> p h d", h=8),
                    in1=R["e"][:, 32 + g * 8:32 + (g + 1) * 8].unsqueeze(2).to_broadcast([128, 8, 64]), op=ALU.mult),
                    reads=["ps6", pt + "e"], writes=[n2])
                P.op("pool", lambda e: e.tensor_tensor(out=a1[:], in0=a1[:], in1=a2[:], op=ALU.add), reads=[n1, n2], writes=[n1])
                P.op("pool", lambda e: e.tensor_tensor(
                    out=tm[:].rearrange("p (h d) -> p h d", h=8), in0=state[:, g * 512:(g + 1) * 512].rearrange("p (h d) -> p h d", h=8),
                    in1=R["dec"][:, g * 8:(g + 1) * 8].unsqueeze(2).to_broadcast([128, 8, 64]), op=ALU.mult),
                    reads=[tg + "state", pt + "dec"], writes=[tmn])
                P.op("dve", lambda e: e.tensor_tensor(out=state[:, g * 512:(g + 1) * 512], in0=ps_s[:], in1=tm[:], op=ALU.add),
                     reads=["ps7", tmn], writes=[tg + "state"])
                P.op("act", lambda e: e.copy(out=sfb[:, g * 512:(g + 1) * 512], in_=state[:, g * 512:(g + 1) * 512]),
                     reads=[tg + "state"], writes=[tg + "sfb"])
            if part == 5:
                P.op("dve", lambda e: e.tensor_tensor(out=a1[:], in0=py[:], in1=a1[:], op=ALU.add), reads=[pyn, n1], writes=[n1])
                P.op("pool", lambda e: e.tensor_tensor(out=yz_t[:, g * 512:(g + 1) * 512], in0=a1[:], in1=sz_t[:, g * 512:(g + 1) * 512], op=ALU.mult),
                     reads=[n1, zn], writes=[yzn], multi=True)
                P.op("act", lambda e: e.activation(out=junk[:], in_=yz_t[:, g * 512:(g + 1) * 512],
                                                   func=AF.Square, accum_out=ss_t[:, g:g + 1]), reads=[yzn], writes=[ssn], multi=True)

        def chunk_tail(c):
            sl = c % 2
            yz_t, yzn = yz[sl], tg + "yz%d" % sl
            ss_t, ssn = ss[sl], tg + "ss%d" % sl
            s1 = tg + "ss1_%d" % sl
            P.op("dve", lambda e: e.tensor_reduce(out=ss_t[:, 0:1], in_=ss_t[:, 0:4], axis=AX.X, op=ALU.add), reads=[ssn], writes=[s1])
            P.op("dve", lambda e: e.tensor_scalar(out=ss_t[:, 0:1], in0=ss_t[:, 0:1], scalar1=1.0 / DI, scalar2=EPS, op0=ALU.mult, op1=ALU.add),
                 reads=[s1], writes=[s1])
            P.op("act", lambda e: e.activation(out=ss_t[:, 0:1], in_=ss_t[:, 0:1], func=AF.Ln), reads=[s1], writes=[s1])
            P.op("act", lambda e: e.activation(out=ss_t[:, 0:1], in_=ss_t[:, 0:1], func=AF.Exp, scale=-0.5), reads=[s1], writes=[s1])
            P.op("act", lambda e: e.activation(out=yn[:], in_=yz_t[:], func=AF.Copy, scale=ss_t[:, 0:1]),
                 reads=[yzn, s1], writes=[tg + "yn", ssn, yzn])
            ya = yaT[sl]
            yan = tg + "yaT%d" % sl
            for f4 in range(4):
                pt_, ptn = ps_y[f4 % 2], "ps%d" % (3 + f4 % 2)

                def mmt(e, f4=f4, pt_=pt_):
                    for ff in range(4):
                        f = f4 * 4 + ff
                        ins = e.matmul(pt_[:, ff * 128:(ff + 1) * 128], yn[:, f * 128:(f + 1) * 128], K.cb[:, 0, :], start=True, stop=True)
                    return ins
                P.op("pe", mmt, reads=[tg + "yn", "cmatb"], writes=[ptn])
                if f4 % 2 == 0:
                    P.op("dve", lambda e, f4=f4, pt_=pt_: e.tensor_copy(out=ya[:, f4 * 4:(f4 + 1) * 4, :], in_=pt_[:].rearrange("p (f t) -> p f t", f=4)),
                         reads=[ptn], writes=[yan], multi=True)
                else:
                    P.op("act", lambda e, f4=f4, pt_=pt_: e.copy(out=ya[:, f4 * 4:(f4 + 1) * 4, :], in_=pt_[:].rearrange("p (f t) -> p f t", f=4)),
                         reads=[ptn], writes=[yan], multi=True)
            P.op("sp", lambda e: e.dma_start(out=S["yaT"][:, c * 128:(c + 1) * 128].rearrange("(f p) t -> p f t", p=128), in_=ya[:]),
                 reads=[yan], key="st_" + yan)

        seq = [(c, g) for c in range(NCH) for g in range(4)]
        load(0)
        chunk_head(0)
        for n in range(3):
            rb_load(*seq[n])
        for n in range(2):
            stage_a(*seq[n])
            stage_a2(*seq[n])
        for n, (c, g) in enumerate(seq):
            if g == 0 and c + 1 < NCH:
                load(c + 1)
            if n + 3 < len(seq):
                rb_load(*seq[n + 3])
            stage_b(c, g, 1)
            stage_b(c, g, 2)
            if n + 2 < len(seq):
                c2, g2 = seq[n + 2]
                if g2 == 0:
                    chunk_head(c2)
                stage_a(c2, g2)
                stage_a2(c2, g2)
            stage_b(c, g, 3)
            stage_b(c, g, 4)
            stage_b(c, g, 5)
            if g == 3:
                chunk_tail(c)


POOLK = (2, 4, 8, 16)
POOL_DMIN = {2: -1, 4: -1, 8: -2, 16: -4}
POOL_DMAX = {2: 3, 4: 4, 8: 5, 16: 7}


def pool_idx(k, d):
    base = 0
    for kk in POOLK:
        if kk == k:
            return base + d - POOL_DMIN[k]
        base += POOL_DMAX[kk] - POOL_DMIN[kk] + 1
    raise ValueError


def pass_pool(K, l):
    nc, P, I, S = K.nc, K.P, K.I, K.S
    import contextlib
    with contextlib.ExitStack() as st:
        sb = lambda n, s, d=F32: st.enter_context(nc.sbuf_tensor("sb_" + n, list(s), d))
        tg = "D1_%d_" % l
        pP = sb(tg + "pP", [128, 31, 512], BF16)
        pC = sb(tg + "pC", [128, 8, 256], BF16)
        pw = sb(tg + "pw", [128, 8, 256], BF16)
        psc = sb(tg + "psc", [128, 8])
        ut = [sb(tg + "ut%d" % i, [128, 12, 1024], BF16) for i in range(2)]
        pmT = sb(tg + "pmT", [128, 8, 512], BF16)
        ypT = [sb(tg + "ypT%d" % i, [128, 8, 512], BF16) for i in range(2)]
        P.op("sp", lambda e: e.dma_start(out=pC[:], in_=I["poolC"]), writes=[tg + "pC"], key="pc1")
        P.op("pool", lambda e: e.dma_start(out=pw[:], in_=I["pool_w"][l].rearrange("g (kc p) d -> p (g kc) d", p=128)),
             writes=[tg + "pw"], key="pc2")
        P.op("sp", lambda e: e.dma_start(out=psc[:], in_=I["pscfm"][l]), writes=[tg + "psc"], key="pc3")
        cur_type = [None]
        for gi, (c0, ncg) in enumerate(groups()):
            ntok = ncg * 128
            t0 = c0 * 128
            u_t = ut[gi % 2]
            un = tg + "ut%d" % (gi % 2)
            if gi == 0:
                tiles = {0: 0, 1: 1}
                for sl_, c in tiles.items():
                    P.op("sp", lambda e, u_t=u_t, sl_=sl_, c=c: e.dma_start(out=u_t[:, sl_, :], in_=S["u"][c * 128:(c + 1) * 128, :]),
                         writes=[un], key="ut%d" % (sl_ % 4), multi=True)
            else:
                ptype = 0 if gi == 1 else (2 if gi == 16 else 1)
                if cur_type[0] != ptype:
                    cur_type[0] = ptype
                    for pi in range(4):
                        a, b = pi * 8, min(31, pi * 8 + 8)
                        P.op("sp", lambda e, a=a, b=b, ptype=ptype: e.dma_start(out=pP[:, a:b, :], in_=I["poolP"][ptype, :, a:b, :]),
                             writes=[tg + "pP"], key="pP%d" % pi, multi=True)
                lt0 = (gi - 1) * 4
                for d in range(-4, 8):
                    lt = lt0 + d
                    if 0 <= lt < 64:
                        c = 2 + lt
                        P.op("sp", lambda e, u_t=u_t, d=d, c=c: e.dma_start(out=u_t[:, d + 4, :], in_=S["u"][c * 128:(c + 1) * 128, :]),
                             writes=[un], key="ut%d" % ((d + 4) % 4), multi=True)
            for kg, k in enumerate(POOLK):
                for cc in range(2):
                    ps = K.ps[(kg * 2 + cc) % 4]
                    pn = "ps%d" % ((kg * 2 + cc) % 4)
                    ch0 = kg * 256 + cc * 128
                    if gi == 0:
                        mats = [(sl_, pC[:, kg * 2 + sl_, :]) for sl_ in range(2)]
                    else:
                        mats = []
                        for d in range(POOL_DMIN[k], POOL_DMAX[k] + 1):
                            if 0 <= lt0 + d < 64:
                                mats.append((d + 4, pP[:, pool_idx(k, d), :]))

                    def mm(e, ps=ps, mats=mats, u_t=u_t, ch0=ch0, ntok=ntok):
                        for i, (sl_, pm) in enumerate(mats):
                            ins = e.matmul(ps[:, 0:ntok], u_t[:, sl_, ch0:ch0 + 128], pm[:, 0:ntok],
                                           start=(i == 0), stop=(i == len(mats) - 1))
                        return ins
                    P.op("pe", mm, reads=[un, tg + "pP", tg + "pC"], writes=[pn])
                    if cc == 0:
                        P.op("dve", lambda e, ps=ps, kg=kg, cc=cc, ntok=ntok: e.tensor_copy(
                            out=pmT[:, kg * 2 + cc, 0:ntok], in_=ps[:, 0:ntok]), reads=[pn], writes=[tg + "pmT"], multi=True)
                    else:
                        P.op("act", lambda e, ps=ps, kg=kg, cc=cc, ntok=ntok: e.copy(
                            out=pmT[:, kg * 2 + cc, 0:ntok], in_=ps[:, 0:ntok]), reads=[pn], writes=[tg + "pmT"], multi=True)
            yp = ypT[gi % 2]
            ypn = tg + "ypT%d" % (gi % 2)
            for kg in range(4):
                for dc in range(2):
                    ps = K.ps[4 + (kg * 2 + dc) % 4]
                    pn = "ps%d" % (4 + (kg * 2 + dc) % 4)

                    def mm2(e, ps=ps, kg=kg, dc=dc, ntok=ntok):
                        for kc in range(2):
                            ins = e.matmul(ps[:, 0:ntok], pw[:, kg * 2 + kc, dc * 128:(dc + 1) * 128], pmT[:, kg * 2 + kc, 0:ntok],
                                           start=(kc == 0), stop=(kc == 1))
                        return ins
                    P.op("pe", mm2, reads=[tg + "pw", tg + "pmT"], writes=[pn])
                    j = kg * 2 + dc
                    if dc == 0:
                        P.op("act", lambda e, ps=ps, j=j, yp=yp, ntok=ntok: e.activation(
                            out=yp[:, j, 0:ntok], in_=ps[:, 0:ntok], func=AF.Copy, scale=psc[:, j:j + 1]),
                            reads=[pn, tg + "psc"], writes=[ypn], multi=True)
                    else:
                        P.op("dve", lambda e, ps=ps, j=j, yp=yp, ntok=ntok: e.tensor_scalar_mul(
                            out=yp[:, j, 0:ntok], in0=ps[:, 0:ntok], scalar1=psc[:, j:j + 1]),
                            reads=[pn, tg + "psc"], writes=[ypn], multi=True)
            P.op("sp", lambda e, yp=yp, t0=t0, ntok=ntok: e.dma_start(
                out=S["ypT"][:, t0:t0 + ntok].rearrange("(k p) t -> p k t", p=128), in_=yp[:, :, 0:ntok]),
                reads=[ypn, tg + "pmT"], key="st_" + ypn)


def layer_norm_tile(K, tg, r, x1, st6, mv, gbc, bbc, deps_r):
    P = K.P
    rn, xn = deps_r
    for hf in range(2):
        P.op("dve", lambda e, hf=hf: e.bn_stats(out=st6[:, hf * 6:(hf + 1) * 6], in_=r[:, hf * 512:(hf + 1) * 512]),
             reads=[rn], writes=[tg + "st6"], multi=True)
    P.op("dve", lambda e: e.bn_aggr(out=mv[:, 0:2], in_=st6[:]), reads=[tg + "st6"], writes=[tg + "mv"])
    P.op("dve", lambda e: e.tensor_scalar_add(out=mv[:, 1:2], in0=mv[:, 1:2], scalar1=EPS), reads=[tg + "mv"], writes=[tg + "mv"])
    P.op("act", lambda e: e.activation(out=mv[:, 1:2], in_=mv[:, 1:2], func=AF.Ln), reads=[tg + "mv"], writes=[tg + "mv"])
    P.op("act", lambda e: e.activation(out=mv[:, 1:2], in_=mv[:, 1:2], func=AF.Exp, scale=-0.5), reads=[tg + "mv"], writes=[tg + "mv"])
    P.op("dve", lambda e: e.tensor_scalar(out=r[:], in0=r[:], scalar1=mv[:, 0:1], scalar2=mv[:, 1:2],
                                          op0=ALU.subtract, op1=ALU.mult), reads=[rn, tg + "mv"], writes=[rn, tg + "st6"])
    P.op("pool", lambda e: e.tensor_tensor(out=r[:], in0=r[:], in1=gbc[:], op=ALU.mult), reads=[rn, tg + "gbc"], writes=[rn])
    P.op("pool", lambda e: e.tensor_tensor(out=x1[:], in0=r[:], in1=bbc[:], op=ALU.add), reads=[rn, tg + "bbc"], writes=[xn])


def pass_merge(K, l, src):
    nc, P, I, S = K.nc, K.P, K.I, K.S
    import contextlib
    with contextlib.ExitStack() as st:
        sb = lambda n, s, d=F32: st.enter_context(nc.sbuf_tensor("sb_" + n, list(s), d))
        tg = "D2_%d_" % l
        Wa = sb(tg + "Wa", [128, 16, 1024], BF16)
        Wb = sb(tg + "Wb", [128, 8, 1024], BF16)
        Wo = sb(tg + "Wo", [128, 8, 1024], BF16)
        gfm = sb(tg + "gfm", [128, 16])
        wr = sb(tg + "wr", [128, 8, 36])
        brow = sb(tg + "brow", [128, 36])
        for i in range(4):
            P.op("pool", lambda e, i=i: e.dma_start(out=Wa[:, i * 4:(i + 1) * 4, :],
                                                    in_=I["w_branch_a"][l, i * 512:(i + 1) * 512, :].rearrange("(k p) n -> p k n", p=128)),
                 writes=[tg + "Wa"], key="wld%d" % i, multi=True)
        for i in range(2):
            P.op("pool", lambda e, i=i: e.dma_start(out=Wb[:, i * 4:(i + 1) * 4, :],
                                                    in_=I["w_branch_b"][l, i * 512:(i + 1) * 512, :].rearrange("(k p) n -> p k n", p=128)),
                 writes=[tg + "Wb"], key="wld%d" % i, multi=True)
            P.op("pool", lambda e, i=i: e.dma_start(out=Wo[:, i * 4:(i + 1) * 4, :],
                                                    in_=I["w_out"][l, i * 512:(i + 1) * 512, :].rearrange("(k p) n -> p k n", p=128)),
                 writes=[tg + "Wo"], key="wld%d" % (2 + i), multi=True)
        P.op("sp", lambda e: e.dma_start(out=gfm[:], in_=I["gnfm"][l]), writes=[tg + "gfm"], key="pc1")
        P.op("sp", lambda e: e.dma_start(out=wr[:], in_=I["wr"][l]), writes=[tg + "wr"], key="pc2")
        P.op("sp", lambda e: e.dma_start(out=brow[:], in_=I["br"][l].partition_broadcast(128)), writes=[tg + "brow"], key="pc3")
        for k in range(16):
            P.op("dve" if k % 2 == 0 else "pool", lambda e, k=k: e.tensor_scalar_mul(out=Wa[:, k, :], in0=Wa[:, k, :], scalar1=gfm[:, k:k + 1]),
                 reads=[tg + "Wa", tg + "gfm"], writes=[tg + "Wa2"], multi=True)
        gate_bc = [sb(tg + "gate%d" % s_, [128, 1024]) for s_ in range(2)]
        gbc = sb(tg + "gbc", [128, 1024])
        bbc = sb(tg + "bbc", [128, 1024])
        for s_ in range(2):
            bcast_rows(K, l, gate_bc[s_], tg + "gate%d" % s_, 2 * 1024, s_)
        P.op("sp", lambda e: e.dma_start(out=gbc[:], in_=I["ln1_g"][l].partition_broadcast(128)), writes=[tg + "gbc"], key="pc4")
        P.op("sp", lambda e: e.dma_start(out=bbc[:], in_=I["ln1_b"][l].partition_broadcast(128)), writes=[tg + "bbc"], key="pc5")
        yaT = sb(tg + "yaT", [128, 16, 512], BF16)
        gT = sb(tg + "gT", [128, 16, 512], BF16)
        ypT = sb(tg + "ypT", [128, 8, 512], BF16)
        mT = sb(tg + "mT", [128, 8, 512], BF16)
        t1 = [sb(tg + "t1_%d" % i, [128, 512]) for i in range(2)]
        t2 = [sb(tg + "t2_%d" % i, [128, 512]) for i in range(2)]
        xt = [sb(tg + "x%d" % i, [128, 1024]) for i in range(2)]
        rt = [sb(tg + "r%d" % i, [128, 1024]) for i in range(2)]
        x1t = [sb(tg + "x1_%d" % i, [128, 1024]) for i in range(2)]
        st6 = sb(tg + "st6", [128, 12])
        mv = sb(tg + "mv", [128, 2])
        h2b = [sb(tg + "h2b%d" % i, [128, 8, 128], BF16) for i in range(2)]
        h2f = [sb(tg + "h2f%d" % i, [128, 8, 128]) for i in range(2)]
        rsm = {nm: sb(tg + "rs_" + nm, [128, w_]) for nm, w_ in
               (("lg", 36), ("m4", 1), ("oh", 4), ("ex", 4), ("se", 1), ("tmp32", 32), ("le8", 8), ("m1", 1), ("mk1", 8),
                ("le2", 8), ("m2", 1), ("mk2", 8), ("w1", 1), ("w2", 1), ("g8", 8), ("s32", 32), ("ssel", 32), ("jk", 32), ("mk12", 8))}
        rsm["s32b"] = sb(tg + "rs_s32b", [128, 32], BF16)
        P.op("pool", lambda e: e.memset(K.pref[:], 0.0), writes=["pref"])
        it = 0
        for gi, (c0, ncg) in enumerate(groups()):
            ntok = ncg * 128
            t0 = c0 * 128
            P.op("sp", lambda e, t0=t0, ntok=ntok: e.dma_start(
                out=yaT[:, :, 0:ntok], in_=S["yaT"][:, t0:t0 + ntok].rearrange("(k p) t -> p k t", p=128)),
                writes=[tg + "yaT"], key=tg + "yaT")
            P.op("sp", lambda e, t0=t0, ntok=ntok: e.dma_start(
                out=gT[:, :, 0:ntok], in_=S["gT"][:, t0:t0 + ntok].rearrange("(k p) t -> p k t", p=128)),
                writes=[tg + "gT"], key=tg + "gT")
            P.op("sp", lambda e, t0=t0, ntok=ntok: e.dma_start(
                out=ypT[:, :, 0:ntok], in_=S["ypT"][:, t0:t0 + ntok].rearrange("(k p) t -> p k t", p=128)),
                writes=[tg + "ypT"], key=tg + "ypT")
            for oc in range(8):
                psA, pnA = K.ps[(oc % 2) * 2], "ps%d" % ((oc % 2) * 2)
                psB, pnB = K.ps[(oc % 2) * 2 + 1], "ps%d" % ((oc % 2) * 2 + 1)

                def mma(e, psA=psA, oc=oc, ntok=ntok):
                    for k in range(16):
                        ins = e.matmul(psA[:, 0:ntok], Wa[:, k, oc * 128:(oc + 1) * 128], yaT[:, k, 0:ntok], start=(k == 0), stop=(k == 15))
                    return ins
                P.op("pe", mma, reads=[tg + "Wa2", tg + "yaT"], writes=[pnA])

                def mmb(e, psB=psB, oc=oc, ntok=ntok):
                    for k in range(8):
                        ins = e.matmul(psB[:, 0:ntok], Wb[:, k, oc * 128:(oc + 1) * 128], ypT[:, k, 0:ntok], start=(k == 0), stop=(k == 7))
                    return ins
                P.op("pe", mmb, reads=[tg + "Wb", tg + "ypT"], writes=[pnB])
                a1, a2 = t1[oc % 2], t2[oc % 2]
                n1, n2 = tg + "t1_%d" % (oc % 2), tg + "t2_%d" % (oc % 2)
                P.op("dve", lambda e, psA=psA, oc=oc, a1=a1, ntok=ntok: e.tensor_tensor(
                    out=a1[:, 0:ntok], in0=psA[:, 0:ntok], in1=gT[:, oc, 0:ntok], op=ALU.mult), reads=[pnA, tg + "gT"], writes=[n1])
                P.op("dve", lambda e, psB=psB, oc=oc, a2=a2, ntok=ntok: e.tensor_tensor(
                    out=a2[:, 0:ntok], in0=psB[:, 0:ntok], in1=gT[:, 8 + oc, 0:ntok], op=ALU.mult), reads=[pnB, tg + "gT"], writes=[n2])
                P.op("pool", lambda e, oc=oc, a1=a1, a2=a2, ntok=ntok: e.tensor_tensor(
                    out=mT[:, oc, 0:ntok], in0=a1[:, 0:ntok], in1=a2[:, 0:ntok], op=ALU.add), reads=[n1, n2], writes=[tg + "mT"], multi=True)
            for q in range(ncg):
                c = c0 + q
                s_ = chunk_stream(c)
                sl = it % 2
                it += 1
                x_t, r_t, x1_t = xt[sl], rt[sl], x1t[sl]
                xn, rn, x1n = tg + "x%d" % sl, tg + "r%d" % sl, tg + "x1_%d" % sl
                P.op("sp", lambda e, x_t=x_t, c=c: e.dma_start(out=x_t[:], in_=src[c * 128:(c + 1) * 128, :]), writes=[xn], key=xn)
                for hf in range(2):
                    ps, pn = K.ps[4 + hf], "ps%d" % (4 + hf)

                    def mmo(e, ps=ps, hf=hf, q=q):
                        for k in range(8):
                            ins = e.matmul(ps[:], mT[:, k, q * 128:(q + 1) * 128], Wo[:, k, hf * 512:(hf + 1) * 512], start=(k == 0), stop=(k == 7))
                        return ins
                    P.op("pe", mmo, reads=[tg + "mT", tg + "Wo"], writes=[pn])
                    P.op("dve", lambda e, ps=ps, hf=hf, r_t=r_t, s_=s_: e.tensor_tensor(
                        out=r_t[:, hf * 512:(hf + 1) * 512], in0=ps[:], in1=gate_bc[s_][:, hf * 512:(hf + 1) * 512], op=ALU.mult),
                        reads=[pn, tg + "gate%d" % s_], writes=[rn], multi=True)
                P.op("dve", lambda e, x_t=x_t, r_t=r_t: e.scalar_tensor_tensor(
                    out=r_t[:], in0=x_t[:], scalar=ALPHA, in1=r_t[:], op0=ALU.mult, op1=ALU.add), reads=[xn, rn], writes=[rn])
                layer_norm_tile(K, tg, r_t, x1_t, st6, mv, gbc, bbc, (rn, x1n))
                P.op("sp", lambda e, x1_t=x1_t, c=c: e.dma_start(out=S["x1"][c * 128:(c + 1) * 128, :], in_=x1_t[:]),
                     reads=[x1n], key="st_" + x1n)
                hb, hf32 = h2b[sl], h2f[sl]
                hbn, hfn = tg + "h2b%d" % sl, tg + "h2f%d" % sl
                for hf in range(2):
                    ps, pn = K.ps[6 + hf], "ps%d" % (6 + hf)

                    def tr(e, ps=ps, hf=hf, x1_t=x1_t):
                        for jj in range(4):
                            j = hf * 4 + jj
                            ins = e.matmul(ps[:, jj * 128:(jj + 1) * 128], x1_t[:, j * 128:(j + 1) * 128], K.ident[:], start=True, stop=True)
                        return ins
                    P.op("pe", tr, reads=[x1n, "ident"], writes=[pn])
                    for jj in range(4):
                        j = hf * 4 + jj
                        sc = K.modfm[:, 32 + j, s_:s_ + 1]
                        sh = K.modfm[:, 24 + j, s_:s_ + 1]
                        P.op("dve", lambda e, ps=ps, jj=jj, j=j, hf32=hf32, sc=sc, sh=sh: e.tensor_scalar(
                            out=hf32[:, j, :], in0=ps[:, jj * 128:(jj + 1) * 128], scalar1=sc, scalar2=sh, op0=ALU.mult, op1=ALU.add),
                            reads=[pn, "modfm"], writes=[hfn], multi=True)
                    P.op("act", lambda e, hf=hf, hb=hb, hf32=hf32: e.copy(out=hb[:, hf * 4:(hf + 1) * 4, :], in_=hf32[:, hf * 4:(hf + 1) * 4, :]),
                         reads=[hfn], writes=[hbn], multi=True)
                P.op("sp", lambda e, hb=hb, c=c: e.dma_start(
                    out=S["h2T"][:, c * 128:(c + 1) * 128].rearrange("(k p) t -> p k t", p=128), in_=hb[:]),
                    reads=[hbn], key="st_" + hbn)
                ps, pn = K.ps[4], "ps4"

                def mmr(e, ps=ps, hf32=hf32):
                    for k in range(8):
                        ins = e.matmul(ps[:, 0:36], hf32[:, k, :], wr[:, k, :], start=(k == 0), stop=(k == 7))
                    return ins
                P.op("pe", mmr, reads=[hfn, tg + "wr"], writes=[pn])
                route(K, tg, rsm, ps, pn, brow, c)


def route(K, tg, r, ps, pn, brow, c):
    P = K.P
    V = lambda fn, rd, wr_: P.op("dve", fn, reads=[tg + x if not x.startswith("ps") else x for x in rd], writes=[tg + x for x in wr_])
    A = lambda fn, rd, wr_: P.op("act", fn, reads=[tg + x for x in rd], writes=[tg + x for x in wr_])
    lg, m4, oh, ex, se, tmp32, le8, m1, mk1, le2, m2, mk2, w1, w2, g8 = [r[k] for k in
        ("lg", "m4", "oh", "ex", "se", "tmp32", "le8", "m1", "mk1", "le2", "m2", "mk2", "w1", "w2", "g8")]
    V(lambda e: e.tensor_tensor(out=lg[:], in0=ps[:, 0:36], in1=brow[:], op=ALU.add), [pn, "brow"], ["rs_lg"])
    V(lambda e: e.tensor_reduce(out=m4[:], in_=lg[:, 0:4], axis=AX.X, op=ALU.max), ["rs_lg"], ["rs_m4"])
    V(lambda e: e.tensor_scalar(out=oh[:], in0=lg[:, 0:4], scalar1=m4[:, 0:1], scalar2=None, op0=ALU.is_ge), ["rs_lg", "rs_m4"], ["rs_oh"])
    V(lambda e: e.tensor_scalar(out=ex[:], in0=lg[:, 0:4], scalar1=m4[:, 0:1], scalar2=None, op0=ALU.subtract), ["rs_lg", "rs_m4"], ["rs_ex"])
    A(lambda e: e.activation(out=ex[:], in_=ex[:], func=AF.Exp, accum_out=se[:, 0:1]), ["rs_ex"], ["rs_ex", "rs_se"])
    V(lambda e: e.reciprocal(out=se[:], in_=se[:]), ["rs_se"], ["rs_se"])
    V(lambda e: e.tensor_tensor(out=tmp32[:].rearrange("p (g e) -> p g e", g=4), in0=lg[:, 4:36].rearrange("p (g e) -> p g e", g=4),
                                in1=oh[:].unsqueeze(2).to_broadcast([128, 4, 8]), op=ALU.mult), ["rs_lg", "rs_oh"], ["rs_tmp32"])
    V(lambda e: e.tensor_reduce(out=le8[:], in_=tmp32[:].rearrange("p (g e) -> p e g", g=4), axis=AX.X, op=ALU.add), ["rs_tmp32"], ["rs_le8"])
    V(lambda e: e.tensor_reduce(out=m1[:], in_=le8[:], axis=AX.X, op=ALU.max), ["rs_le8"], ["rs_m1"])
    V(lambda e: e.tensor_scalar(out=mk1[:], in0=le8[:], scalar1=m1[:, 0:1], scalar2=None, op0=ALU.is_ge), ["rs_le8", "rs_m1"], ["rs_mk1"])
    V(lambda e: e.scalar_tensor_tensor(out=le2[:], in0=mk1[:], scalar=-1e30, in1=le8[:], op0=ALU.mult, op1=ALU.add), ["rs_mk1", "rs_le8"], ["rs_le2"])
    V(lambda e: e.tensor_reduce(out=m2[:], in_=le2[:], axis=AX.X, op=ALU.max), ["rs_le2"], ["rs_m2"])
    V(lambda e: e.tensor_scalar(out=mk2[:], in0=le2[:], scalar1=m2[:, 0:1], scalar2=None, op0=ALU.is_ge), ["rs_le2", "rs_m2"], ["rs_mk2"])
    V(lambda e: e.tensor_tensor(out=w1[:], in0=m2[:], in1=m1[:], op=ALU.subtract), ["rs_m1", "rs_m2"], ["rs_w1"])
    A(lambda e: e.activation(out=w1[:], in_=w1[:], func=AF.Exp), ["rs_w1"], ["rs_w1"])
    V(lambda e: e.tensor_scalar_add(out=w1[:], in0=w1[:], scalar1=1.0), ["rs_w1"], ["rs_w1"])
    V(lambda e: e.reciprocal(out=w1[:], in_=w1[:]), ["rs_w1"], ["rs_w1"])
    V(lambda e: e.tensor_scalar(out=w2[:], in0=w1[:], scalar1=-1.0, scalar2=1.0, op0=ALU.mult, op1=ALU.add), ["rs_w1"], ["rs_w2"])
    V(lambda e: e.tensor_tensor(out=w1[:], in0=w1[:], in1=se[:], op=ALU.mult), ["rs_w1", "rs_se"], ["rs_w1"])
    V(lambda e: e.tensor_tensor(out=w2[:], in0=w2[:], in1=se[:], op=ALU.mult), ["rs_w2", "rs_se"], ["rs_w2"])
    V(lambda e: e.tensor_scalar(out=g8[:], in0=mk1[:], scalar1=w1[:, 0:1], scalar2=None, op0=ALU.mult), ["rs_mk1", "rs_w1"], ["rs_g8"])
    V(lambda e: e.scalar_tensor_tensor(out=g8[:], in0=mk2[:], scalar=w2[:, 0:1], in1=g8[:], op0=ALU.mult, op1=ALU.add), ["rs_mk2", "rs_w2", "rs_g8"], ["rs_g8"])
    ssel, mk12, s32, s32b, jk = r["ssel"], r["mk12"], r["s32"], r["s32b"], r["jk"]
    V(lambda e: e.tensor_copy(out=K.W12[:, c, 0:1], in_=w1[:]), ["rs_w1"], ["rs_wc"])
    V(lambda e: e.tensor_copy(out=K.W12[:, c, 1:2], in_=w2[:]), ["rs_w2"], ["rs_wc2"])
    for ki, mk in enumerate((mk1, mk2)):
        mkn = "rs_mk1" if ki == 0 else "rs_mk2"
        V(lambda e, mk=mk: e.tensor_tensor(out=s32[:].rearrange("p (g e) -> p g e", g=4),
                                           in0=oh[:].unsqueeze(2).to_broadcast([128, 4, 8]),
                                           in1=mk[:].unsqueeze(1).to_broadcast([128, 4, 8]), op=ALU.mult),
          ["rs_oh", mkn], ["rs_s32"])
        V(lambda e: e.tensor_tensor(out=jk[:], in0=s32[:], in1=K.cvec[:, 66:98], op=ALU.mult), ["rs_s32"], ["rs_jk"])
        V(lambda e, ki=ki: e.tensor_reduce(out=K.E12[:, c, ki:ki + 1], in_=jk[:], axis=AX.X, op=ALU.add), ["rs_jk"], ["rs_e%d" % ki])
        if ki == 0:
            V(lambda e: e.tensor_copy(out=ssel[:], in_=s32[:]), ["rs_s32"], ["rs_ssel"])
        else:
            V(lambda e: e.tensor_tensor(out=s32b[:], in0=s32[:], in1=ssel[:], op=ALU.add), ["rs_s32", "rs_ssel"], ["rs_s32b"])
    psr, psrn = K.ps[5], "ps5"

    def mmc(e):
        e.matmul(psr[:, 0:32], K.cb[:, 6, :], s32b[:], start=True, stop=True)
        return e.matmul(psr[:, 32:64], K.cb[:, 3, :], s32b[:], start=True, stop=True)
    P.op("pe", mmc, reads=[tg + "rs_s32b", "cmatb"], writes=[psrn])
    P.op("dve", lambda e: e.tensor_tensor(out=K.RK[:, c, :], in0=psr[:, 0:32], in1=K.pref[:], op=ALU.add),
         reads=[psrn, "pref"], writes=[("RK", c)])
    P.op("dve", lambda e: e.tensor_tensor(out=K.pref[:], in0=psr[:, 32:64], in1=K.pref[:], op=ALU.add),
         reads=[psrn, "pref"], writes=["pref"])


def pass_moe(K, l, last):
    nc, P, I, S = K.nc, K.P, K.I, K.S
    import contextlib
    with contextlib.ExitStack() as st:
        sb = lambda n, s, d=F32: st.enter_context(nc.sbuf_tensor("sb_" + n, list(s), d))
        tg = "E%d_" % l
        NSG = 11
        h2T = sb(tg + "h2T", [128, 8, NSG * 128], BF16)
        yacc = sb(tg + "yacc", [128, NSG, 1024])
        wg = [sb(tg + "wg%d" % i, [128, 8, 512], BF16) for i in range(2)]
        wu = [sb(tg + "wu%d" % i, [128, 8, 512], BF16) for i in range(2)]
        wd = [sb(tg + "wd%d" % i, [128, 4, 1024], BF16) for i in range(2)]
        sg_ = [sb(tg + "sg%d" % i, [128, 512]) for i in range(2)]
        h1T = [sb(tg + "h1T%d" % i, [128, 4, 512], BF16) for i in range(2)]
        gate_bc = [sb(tg + "gate%d" % s_, [128, 1024]) for s_ in range(2)]
        gbc = sb(tg + "gbc", [128, 1024])
        bbc = sb(tg + "bbc", [128, 1024])
        xt = [sb(tg + "x%d" % i, [128, 1024]) for i in range(2)]
        x2t = [sb(tg + "x2_%d" % i, [128, 1024]) for i in range(2)]
        st6 = sb(tg + "st6", [128, 12])
        mv = sb(tg + "mv", [128, 2])
        for s_ in range(2):
            bcast_rows(K, l, gate_bc[s_], tg + "gate%d" % s_, 5 * 1024, s_)
        P.op("sp", lambda e: e.dma_start(out=gbc[:], in_=I["ln2_g"][l].partition_broadcast(128)), writes=[tg + "gbc"], key="pc4")
        P.op("sp", lambda e: e.dma_start(out=bbc[:], in_=I["ln2_b"][l].partition_broadcast(128)), writes=[tg + "bbc"], key="pc5")
        wi = 0
        hi_ = 0
        xi = 0
        for sg0 in range(0, NCH, NSG):
            cs = list(range(sg0, min(NCH, sg0 + NSG)))
            if last and cs[-1] < 2:
                continue
            ntok = len(cs) * 128
            t0 = sg0 * 128
            P.op("sp", lambda e, t0=t0, ntok=ntok: e.dma_start(
                out=h2T[:, :, 0:ntok], in_=S["h2T"][:, t0:t0 + ntok].rearrange("(k p) t -> p k t", p=128)),
                writes=[tg + "h2T"], key=tg + "h2T")
            P.op("pool", lambda e: e.memset(yacc[:], 0.0), writes=[tg + "yacc"] + [(tg + "yaccn", ci) for ci in range(NSG)])
            subs = [(o, min(512, ntok - o)) for o in range(0, ntok, 512)]
            for ex in range(NE):
                sl = wi % 2
                wi += 1
                wg_t, wu_t, wd_t = wg[sl], wu[sl], wd[sl]
                wgn, wun, wdn = tg + "wg%d" % sl, tg + "wu%d" % sl, tg + "wd%d" % sl
                P.op("pool", lambda e, wg_t=wg_t, ex=ex: e.dma_start(
                    out=wg_t[:], in_=I["w_eg"][l, ex].rearrange("(k p) n -> p k n", p=128)), writes=[wgn], key=wgn)
                P.op("pool", lambda e, wu_t=wu_t, ex=ex: e.dma_start(
                    out=wu_t[:], in_=I["w_eu"][l, ex].rearrange("(k p) n -> p k n", p=128)), writes=[wun], key=wun)
                P.op("pool", lambda e, wd_t=wd_t, ex=ex: e.dma_start(
                    out=wd_t[:], in_=I["w_ed"][l, ex].rearrange("(k p) n -> p k n", p=128)), writes=[wdn], key=wdn)
                for (o, n) in subs:
                    hs = hi_ % 2
                    hi_ += 1
                    h1 = h1T[hs]
                    h1n = tg + "h1T%d" % hs
                    for cc in range(4):
                        psG, pnG = K.ps[(cc % 2) * 2], "ps%d" % ((cc % 2) * 2)
                        psU, pnU = K.ps[(cc % 2) * 2 + 1], "ps%d" % ((cc % 2) * 2 + 1)

                        def mmg(e, psG=psG, cc=cc, o=o, n=n, wg_t=wg_t):
                            for k in range(8):
                                ins = e.matmul(psG[:, 0:n], wg_t[:, k, cc * 128:(cc + 1) * 128], h2T[:, k, o:o + n], start=(k == 0), stop=(k == 7))
                            return ins
                        P.op("pe", mmg, reads=[wgn, tg + "h2T"], writes=[pnG])

                        def mmu(e, psU=psU, cc=cc, o=o, n=n, wu_t=wu_t):
                            for k in range(8):
                                ins = e.matmul(psU[:, 0:n], wu_t[:, k, cc * 128:(cc + 1) * 128], h2T[:, k, o:o + n], start=(k == 0), stop=(k == 7))
                            return ins
                        P.op("pe", mmu, reads=[wun, tg + "h2T"], writes=[pnU])
                        sgt = sg_[cc % 2]
                        sgn = tg + "sg%d" % (cc % 2)
                        P.op("act", lambda e, psG=psG, sgt=sgt, n=n: e.activation(out=sgt[:, 0:n], in_=psG[:, 0:n], func=AF.Silu),
                             reads=[pnG], writes=[sgn])
                        P.op("dve", lambda e, psU=psU, sgt=sgt, n=n, cc=cc, h1=h1: e.tensor_tensor(
                            out=h1[:, cc, 0:n], in0=psU[:, 0:n], in1=sgt[:, 0:n], op=ALU.mult),
                            reads=[pnU, sgn], writes=[h1n], multi=True)
                    for q in range(n // 128):
                        ci = (o // 128) + q
                        c = cs[ci]
                        for hf in range(2):
                            ps, pn = K.ps[4 + (q * 2 + hf) % 4], "ps%d" % (4 + (q * 2 + hf) % 4)

                            def mmd(e, ps=ps, q=q, hf=hf, h1=h1, wd_t=wd_t):
                                for k in range(4):
                                    ins = e.matmul(ps[:], h1[:, k, q * 128:(q + 1) * 128], wd_t[:, k, hf * 512:(hf + 1) * 512],
                                                   start=(k == 0), stop=(k == 3))
                                return ins
                            P.op("pe", mmd, reads=[h1n, wdn], writes=[pn])
                            P.op("dve", lambda e, ps=ps, ci=ci, hf=hf, c=c, ex=ex: e.scalar_tensor_tensor(
                                out=yacc[:, ci, hf * 512:(hf + 1) * 512], in0=ps[:], scalar=K.Gall[:, c, ex:ex + 1],
                                in1=yacc[:, ci, hf * 512:(hf + 1) * 512], op0=ALU.mult, op1=ALU.add),
                                reads=[pn, ("G", c), tg + "yacc"], writes=[(tg + "yacc", ci, hf)])
            for ci, c in enumerate(cs):
                if last and c < 2:
                    continue
                s_ = chunk_stream(c)
                sl = xi % 2
                xi += 1
                x_t, x2_t = xt[sl], x2t[sl]
                xn, x2n = tg + "x%d" % sl, tg + "x2_%d" % sl
                P.op("sp", lambda e, x_t=x_t, c=c: e.dma_start(out=x_t[:], in_=S["x1"][c * 128:(c + 1) * 128, :]), writes=[xn], key=xn)
                yv = yacc[:, ci, :]
                yn_ = (tg + "yaccn", ci)
                P.op("dve", lambda e, yv=yv, s_=s_: e.tensor_tensor(out=yv, in0=yv, in1=gate_bc[s_][:], op=ALU.mult),
                     reads=[(tg + "yacc", ci, 0), (tg + "yacc", ci, 1), tg + "gate%d" % s_], writes=[yn_])
                P.op("dve", lambda e, x_t=x_t, yv=yv: e.scalar_tensor_tensor(out=yv, in0=x_t[:], scalar=ALPHA, in1=yv, op0=ALU.mult, op1=ALU.add),
                     reads=[xn, yn_], writes=[yn_])
                layer_norm_tile(K, tg, yv, x2_t, st6, mv, gbc, bbc, (yn_, x2n))
                if last:
                    P.op("sp", lambda e, x2_t=x2_t, c=c: e.dma_start(out=K.out[(c - 2) * 128:(c - 1) * 128, :], in_=x2_t[:]),
                         reads=[x2n], key="st_" + x2n)
                else:
                    P.op("sp", lambda e, x2_t=x2_t, c=c: e.dma_start(out=S["xres"][c * 128:(c + 1) * 128, :], in_=x2_t[:]),
                         reads=[x2n], key="st_" + x2n)


def pass_dispatch(K, l):
    nc, P, I, S = K.nc, K.P, K.I, K.S
    import contextlib
    with contextlib.ExitStack() as st:
        sb = lambda n, s, d=F32: st.enter_context(nc.sbuf_tensor("sb_" + n, list(s), d))
        tg = "F%d_" % l
        big = sb(tg + "big", [128, NBLK * 32])
        nb = sb(tg + "nb", [128, 32])
        nbT = sb(tg + "nbT", [32, 128])
        bs = sb(tg + "bs", [128, 32])
        be = sb(tg + "be", [128, 32])
        pst = sb(tg + "pst", [128, 32])
        blke = sb(tg + "blke", [128, NBLK])
        rkp = sb(tg + "rkp", [128, NCH, 32])
        oh = sb(tg + "oh", [128, NCH, 32])
        dst = sb(tg + "dst", [128, NCH, 2])
        cnt = K.pref
        P.op("dve", lambda e: e.tensor_tensor(out=big[:, 0:32 * 66].rearrange("p (e j) -> p e j", e=32),
                                              in0=cnt[:].unsqueeze(2).to_broadcast([128, 32, 66]),
                                              in1=K.cvec[:, 0:66].unsqueeze(1).to_broadcast([128, 32, 66]), op=ALU.is_gt),
             reads=["pref", "cvec"], writes=[tg + "big"])
        P.op("dve", lambda e: e.tensor_reduce(out=nb[:], in_=big[:, 0:32 * 66].rearrange("p (e j) -> p e j", e=32), axis=AX.X, op=ALU.add),
             reads=[tg + "big"], writes=[tg + "nb"])
        ps = K.ps[0]
        P.op("pe", lambda e: e.matmul(ps[0:32, 0:128], nb[:], K.cm[:, 0, :], start=True, stop=True), reads=[tg + "nb", "cmat"], writes=["ps0"])
        P.op("dve", lambda e: e.tensor_copy(out=nbT[:], in_=ps[0:32, 0:128]), reads=["ps0"], writes=[tg + "nbT"])
        ps1 = K.ps[1]
        P.op("pe", lambda e: e.matmul(ps1[:, 0:32], nbT[:], K.cm[0:32, 6, 0:32], start=True, stop=True), reads=[tg + "nbT", "cmat"], writes=["ps1"])
        P.op("dve", lambda e: e.tensor_copy(out=bs[:], in_=ps1[:, 0:32]), reads=["ps1"], writes=[tg + "bs"])
        P.op("dve", lambda e: e.tensor_tensor(out=be[:], in0=bs[:], in1=nb[:], op=ALU.add), reads=[tg + "bs", tg + "nb"], writes=[tg + "be"])
        P.op("dve", lambda e: e.tensor_scalar_mul(out=pst[:], in0=bs[:], scalar1=256.0), reads=[tg + "bs"], writes=[tg + "pst"])
        P.op("dve", lambda e: e.tensor_tensor(out=big[:].rearrange("p (b e) -> p b e", b=NBLK),
                                              in0=be[:].unsqueeze(1).to_broadcast([128, NBLK, 32]),
                                              in1=K.cvec[:, 98:98 + NBLK].unsqueeze(2).to_broadcast([128, NBLK, 32]), op=ALU.is_le),
             reads=[tg + "be", "cvec", tg + "nb"], writes=[tg + "big"])
        P.op("dve", lambda e: e.tensor_reduce(out=blke[:], in_=big[:].rearrange("p (b e) -> p b e", b=NBLK), axis=AX.X, op=ALU.add),
             reads=[tg + "big"], writes=[tg + "blke"])
        P.op("dve", lambda e: e.tensor_scalar_min(out=blke[:], in0=blke[:], scalar1=31.0), reads=[tg + "blke"], writes=[tg + "blke"])
        P.op("dve", lambda e: e.tensor_scalar(out=blke[:], in0=blke[:], scalar1=128.0, scalar2=K.cvec[:, 196:197], op0=ALU.mult, op1=ALU.add),
             reads=[tg + "blke", "cvec"], writes=[tg + "blke"])
        if l > 0:
            P.op("dve", lambda e: e.tensor_scalar_add(out=blke[:], in0=blke[:], scalar1=float(l * NE * 128)), reads=[tg + "blke"], writes=[tg + "blke"])
        P.op("dve", lambda e: e.tensor_copy(out=K.IDXW[:], in_=blke[:]), reads=[tg + "blke"], writes=["IDXW"])
        P.op("dve", lambda e: e.tensor_tensor(out=rkp[:], in0=K.RK[:], in1=pst[:].unsqueeze(1).to_broadcast([128, NCH, 32]), op=ALU.add),
             reads=[("RK", c) for c in range(NCH)] + [tg + "pst"], writes=[tg + "rkp"])
        for ki in range(2):
            P.op("dve", lambda e, ki=ki: e.tensor_tensor(out=oh[:], in0=K.cvec[:, 66:98].unsqueeze(1).to_broadcast([128, NCH, 32]),
                                                        in1=K.E12[:, :, ki:ki + 1].to_broadcast([128, NCH, 32]), op=ALU.is_equal),
                 reads=["cvec"] + [tg + "rs_e%d" % ki], writes=[tg + "oh"])
            P.op("dve", lambda e: e.tensor_tensor(out=oh[:], in0=oh[:], in1=rkp[:], op=ALU.mult), reads=[tg + "oh", tg + "rkp"], writes=[tg + "oh"])
            P.op("dve", lambda e, ki=ki: e.tensor_reduce(out=dst[:, :, ki:ki + 1], in_=oh[:], axis=AX.X, op=ALU.add),
                 reads=[tg + "oh"], writes=[tg + "dst%d" % ki])
        P.op("dve", lambda e: e.tensor_copy(out=K.DEST[:], in_=dst[:]), reads=[tg + "dst0", tg + "dst1"], writes=["DEST"])
        scb = [sb(tg + "scb%d" % s_, [128, 1024]) for s_ in range(2)]
        shb = [sb(tg + "shb%d" % s_, [128, 1024]) for s_ in range(2)]
        for s_ in range(2):
            bcast_rows(K, l, scb[s_], tg + "scb%d" % s_, 4 * 1024, s_)
            bcast_rows(K, l, shb[s_], tg + "shb%d" % s_, 3 * 1024, s_)
            P.op("pool", lambda e, s_=s_: e.tensor_scalar_add(out=scb[s_][:], in0=scb[s_][:], scalar1=1.0),
                 reads=[tg + "scb%d" % s_], writes=[tg + "scb%d" % s_])
        xt = [sb(tg + "x%d" % i, [128, 1024]) for i in range(3)]
        ht = [sb(tg + "h%d" % i, [128, 1024], BF16) for i in range(3)]
        zt = sb(tg + "zt", [128, 2048], BF16)
        P.op("pool", lambda e: e.memset(zt[:], 0.0), writes=[tg + "zt"])
        for b in range(NBLK):
            P.op("sp", lambda e, b=b: e.dma_start(out=S["xin"][b * 256:(b + 1) * 256, :].rearrange("(p two) d -> p (two d)", two=2), in_=zt[:]),
                 reads=[tg + "zt"], writes=["xinz"], key="zf%d" % (b % 4), multi=True)
        for c in range(NCH):
            s_ = chunk_stream(c)
            sl = c % 3
            x_t, h_t = xt[sl], ht[sl]
            xn, hn = tg + "x%d" % sl, tg + "h%d" % sl
            P.op("sp", lambda e, x_t=x_t, c=c: e.dma_start(out=x_t[:], in_=S["x1"][c * 128:(c + 1) * 128, :]), writes=[xn], key=xn)
            P.op("dve", lambda e, x_t=x_t, s_=s_: e.tensor_tensor(out=x_t[:], in0=x_t[:], in1=scb[s_][:], op=ALU.mult),
                 reads=[xn, tg + "scb%d" % s_], writes=[xn])
            P.op("pool", lambda e, x_t=x_t, h_t=h_t, s_=s_: e.tensor_tensor(out=h_t[:], in0=x_t[:], in1=shb[s_][:], op=ALU.add),
                 reads=[xn, tg + "shb%d" % s_], writes=[hn])
            for ki in range(2):
                P.op("pool", lambda e, h_t=h_t, c=c, ki=ki: e.indirect_dma_start(
                    out=S["xin"][:, :], out_offset=bass.IndirectOffsetOnAxis(ap=K.DEST[:, c, ki:ki + 1], axis=0),
                    in_=h_t[:, :], in_offset=None), reads=[hn, "DEST", "xinz"], writes=[("xin", c, ki)], key=tg + "sc%d_%d" % (sl, ki))


def pass_experts(K, l):
    nc, P, I, S = K.nc, K.P, K.I, K.S
    import contextlib
    with contextlib.ExitStack() as st:
        sb = lambda n, s, d=F32: st.enter_context(nc.sbuf_tensor("sb_" + n, list(s), d))
        tg = "G%d_" % l
        wgu = [sb(tg + "wgu%d" % i, [128, 8 * 1024], BF16) for i in range(3)]
        wd = [sb(tg + "wd%d" % i, [128, 4 * 1024], BF16) for i in range(3)]
        xr = [sb(tg + "xr%d" % i, [128, 2, 1024], BF16) for i in range(2)]
        xT = [sb(tg + "xT%d" % i, [128, 8, 256], BF16) for i in range(2)]
        sg_ = [sb(tg + "sg%d" % i, [128, 256]) for i in range(2)]
        h1T = [sb(tg + "h1T%d" % i, [128, 4, 256], BF16) for i in range(2)]
        yb = [sb(tg + "yb%d" % i, [128, 1024]) for i in range(2)]
        yi = 0
        for b in range(NBLK):
            sl = b % 2
            sw = b % 3
            wgu_t, wd_t, xr_t, xT_t, h1 = wgu[sw], wd[sw], xr[sl], xT[sl], h1T[sl]
            wgn, wdn = tg + "wgu%d" % sw, tg + "wd%d" % sw
            xrn, xTn, h1n = [tg + x + "%d" % sl for x in ("xr", "xT", "h1T")]
            P.op("pool", lambda e, wgu_t=wgu_t, b=b: e.indirect_dma_start(
                out=wgu_t[:, :], out_offset=None, in_=S["wgub"][:, :],
                in_offset=bass.IndirectOffsetOnAxis(ap=K.IDXW[:, b:b + 1], axis=0)),
                reads=["IDXW", "bg:w%d" % l], writes=[wgn], key=wgn)
            P.op("pool", lambda e, wd_t=wd_t, b=b: e.indirect_dma_start(
                out=wd_t[:, :], out_offset=None, in_=S["wdb"][:, :],
                in_offset=bass.IndirectOffsetOnAxis(ap=K.IDXW[:, b:b + 1], axis=0)),
                reads=["IDXW", "bg:w%d" % l], writes=[wdn], key=wdn)
            P.op("sp", lambda e, xr_t=xr_t, b=b: e.dma_start(
                out=xr_t[:], in_=S["xin"][b * 256:(b + 1) * 256, :].rearrange("(r p) d -> p r d", p=128)),
                writes=[xrn], key=xrn)
            for r_ in range(2):
                for f4 in range(2):
                    ps, pn = K.ps[(r_ * 2 + f4) % 2], "ps%d" % ((r_ * 2 + f4) % 2)

                    def tr(e, ps=ps, r_=r_, f4=f4, xr_t=xr_t):
                        for ff in range(4):
                            f = f4 * 4 + ff
                            ins = e.matmul(ps[:, ff * 128:(ff + 1) * 128], xr_t[:, r_, f * 128:(f + 1) * 128], K.cb[:, 0, :], start=True, stop=True)
                        return ins
                    P.op("pe", tr, reads=[xrn, "cmatb"], writes=[pn])
                    if f4 == 0:
                        P.op("act", lambda e, ps=ps, r_=r_, f4=f4, xT_t=xT_t: e.copy(
                            out=xT_t[:, f4 * 4:(f4 + 1) * 4, r_ * 128:(r_ + 1) * 128], in_=ps[:].rearrange("p (f t) -> p f t", f=4)),
                            reads=[pn], writes=[xTn], multi=True)
                    else:
                        P.op("dve", lambda e, ps=ps, r_=r_, f4=f4, xT_t=xT_t: e.tensor_copy(
                            out=xT_t[:, f4 * 4:(f4 + 1) * 4, r_ * 128:(r_ + 1) * 128], in_=ps[:].rearrange("p (f t) -> p f t", f=4)),
                            reads=[pn], writes=[xTn], multi=True)
            for cc in range(4):
                ps, pn = K.ps[2 + cc % 2], "ps%d" % (2 + cc % 2)

                def mmg(e, ps=ps, cc=cc, wgu_t=wgu_t, xT_t=xT_t):
                    for k in range(8):
                        e.matmul(ps[:, 0:256], wgu_t[:, k * 1024 + cc * 128:k * 1024 + (cc + 1) * 128], xT_t[:, k, :], start=(k == 0), stop=(k == 7))
                    for k in range(8):
                        ins = e.matmul(ps[:, 256:512], wgu_t[:, k * 1024 + 512 + cc * 128:k * 1024 + 512 + (cc + 1) * 128], xT_t[:, k, :],
                                       start=(k == 0), stop=(k == 7))
                    return ins
                P.op("pe", mmg, reads=[wgn, xTn], writes=[pn])
                sgt, sgn = sg_[cc % 2], tg + "sg%d" % (cc % 2)
                P.op("act", lambda e, ps=ps, sgt=sgt: e.activation(out=sgt[:], in_=ps[:, 0:256], func=AF.Silu), reads=[pn], writes=[sgn])
                P.op("dve", lambda e, ps=ps, sgt=sgt, cc=cc, h1=h1: e.tensor_tensor(out=h1[:, cc, :], in0=ps[:, 256:512], in1=sgt[:], op=ALU.mult),
                     reads=[pn, sgn], writes=[h1n], multi=True)
            for r_ in range(2):
                y_t, yn_ = yb[yi % 2], tg + "yb%d" % (yi % 2)
                yi += 1
                for hf in range(2):
                    ps, pn = K.ps[4 + (r_ * 2 + hf)], "ps%d" % (4 + r_ * 2 + hf)

                    def mmd(e, ps=ps, r_=r_, hf=hf, h1=h1, wd_t=wd_t):
                        for k in range(4):
                            ins = e.matmul(ps[:], h1[:, k, r_ * 128:(r_ + 1) * 128], wd_t[:, k * 1024 + hf * 512:k * 1024 + (hf + 1) * 512],
                                           start=(k == 0), stop=(k == 3))
                        return ins
                    P.op("pe", mmd, reads=[h1n, wdn], writes=[pn])
                    if hf == 0:
                        P.op("act", lambda e, ps=ps, y_t=y_t, hf=hf: e.copy(out=y_t[:, hf * 512:(hf + 1) * 512], in_=ps[:]),
                             reads=[pn], writes=[yn_], multi=True)
                    else:
                        P.op("dve", lambda e, ps=ps, y_t=y_t, hf=hf: e.tensor_copy(out=y_t[:, hf * 512:(hf + 1) * 512], in_=ps[:]),
                             reads=[pn], writes=[yn_], multi=True)
                P.op("sp", lambda e, y_t=y_t, b=b, r_=r_: e.dma_start(out=S["yrows"][b * 256 + r_ * 128:b * 256 + (r_ + 1) * 128, :], in_=y_t[:]),
                     reads=[yn_], key="st_" + yn_)


def pass_combine(K, l, last):
    nc, P, I, S = K.nc, K.P, K.I, K.S
    import contextlib
    with contextlib.ExitStack() as st:
        sb = lambda n, s, d=F32: st.enter_context(nc.sbuf_tensor("sb_" + n, list(s), d))
        tg = "H%d_" % l
        gate_bc = [sb(tg + "gate%d" % s_, [128, 1024]) for s_ in range(2)]
        gbc = sb(tg + "gbc", [128, 1024])
        bbc = sb(tg + "bbc", [128, 1024])
        for s_ in range(2):
            bcast_rows(K, l, gate_bc[s_], tg + "gate%d" % s_, 5 * 1024, s_)
        P.op("sp", lambda e: e.dma_start(out=gbc[:], in_=I["ln2_g"][l].partition_broadcast(128)), writes=[tg + "gbc"], key="pc4")
        P.op("sp", lambda e: e.dma_start(out=bbc[:], in_=I["ln2_b"][l].partition_broadcast(128)), writes=[tg + "bbc"], key="pc5")
        r1 = [sb(tg + "r1_%d" % i, [128, 1024]) for i in range(3)]
        r2 = [sb(tg + "r2_%d" % i, [128, 1024]) for i in range(3)]
        xt = [sb(tg + "x%d" % i, [128, 1024]) for i in range(3)]
        x2t = [sb(tg + "x2_%d" % i, [128, 1024]) for i in range(2)]
        st6 = sb(tg + "st6", [128, 12])
        mv = sb(tg + "mv", [128, 2])
        it = 0
        for c in range(NCH):
            if last and c < 2:
                continue
            s_ = chunk_stream(c)
            sl = it % 3
            sl2 = it % 2
            it += 1
            a1, a2, x_t, x2_t = r1[sl], r2[sl], xt[sl], x2t[sl2]
            n1, n2, xn, x2n = tg + "r1_%d" % sl, tg + "r2_%d" % sl, tg + "x%d" % sl, tg + "x2_%d" % sl2
            P.op("pool", lambda e, a1=a1, c=c: e.indirect_dma_start(
                out=a1[:, :], out_offset=None, in_=S["yrows"][:, :],
                in_offset=bass.IndirectOffsetOnAxis(ap=K.DEST[:, c, 0:1], axis=0)),
                reads=["DEST"], writes=[n1], key=n1)
            P.op("pool", lambda e, a2=a2, c=c: e.indirect_dma_start(
                out=a2[:, :], out_offset=None, in_=S["yrows"][:, :],
                in_offset=bass.IndirectOffsetOnAxis(ap=K.DEST[:, c, 1:2], axis=0)),
                reads=["DEST"], writes=[n2], key=n2)
            P.op("sp", lambda e, x_t=x_t, c=c: e.dma_start(out=x_t[:], in_=S["x1"][c * 128:(c + 1) * 128, :]), writes=[xn], key=xn)
            P.op("dve", lambda e, a1=a1, c=c: e.tensor_scalar_mul(out=a1[:], in0=a1[:], scalar1=K.W12[:, c, 0:1]), reads=[n1], writes=[n1])
            P.op("dve", lambda e, a1=a1, a2=a2, c=c: e.scalar_tensor_tensor(out=a1[:], in0=a2[:], scalar=K.W12[:, c, 1:2], in1=a1[:],
                                                                          op0=ALU.mult, op1=ALU.add), reads=[n1, n2], writes=[n1])
            P.op("pool", lambda e, a1=a1, s_=s_: e.tensor_tensor(out=a1[:], in0=a1[:], in1=gate_bc[s_][:], op=ALU.mult),
                 reads=[n1, tg + "gate%d" % s_], writes=[n1])
            P.op("dve", lambda e, a1=a1, x_t=x_t: e.scalar_tensor_tensor(out=a1[:], in0=x_t[:], scalar=ALPHA, in1=a1[:], op0=ALU.mult, op1=ALU.add),
                 reads=[xn, n1], writes=[n1])
            layer_norm_tile(K, tg, a1, x2_t, st6, mv, gbc, bbc, (n1, x2n))
            if last:
                P.op("sp", lambda e, x2_t=x2_t, c=c: e.dma_start(out=K.out[(c - 2) * 128:(c - 1) * 128, :], in_=x2_t[:]),
                     reads=[x2n], key="st_" + x2n)
            else:
                P.op("sp", lambda e, x2_t=x2_t, c=c: e.dma_start(out=S["xres"][c * 128:(c + 1) * 128, :], in_=x2_t[:]),
                     reads=[x2n], key="st_" + x2n)


def layer(K, l, stop):
    nc, P, I, S = K.nc, K.P, K.I, K.S
    import contextlib
    with contextlib.ExitStack() as st:
        K.modfm = st.enter_context(nc.sbuf_tensor("sb_modfm%d" % l, [128, 48, 2], F32))
        K.RK = st.enter_context(nc.sbuf_tensor("sb_RK%d" % l, [128, NCH, 32], F32))
        K.E12 = st.enter_context(nc.sbuf_tensor("sb_E12_%d" % l, [128, NCH, 2], F32))
        K.W12 = st.enter_context(nc.sbuf_tensor("sb_W12_%d" % l, [128, NCH, 2], F32))
        K.pref = st.enter_context(nc.sbuf_tensor("sb_pref%d" % l, [128, 32], F32))
        K.IDXW = st.enter_context(nc.sbuf_tensor("sb_IDXW%d" % l, [128, NBLK], I32))
        K.DEST = st.enter_context(nc.sbuf_tensor("sb_DEST%d" % l, [128, NCH, 2], I32))
        with contextlib.ExitStack() as st2:
            phase_mod(K, l, st2)
            P.barrier()
        if stop == "mod":
            return
        src = I["xcat"] if l == 0 else S["xres"]
        P.barrier()
        pass_inproj(K, l, src, 0)
        P.barrier()
        pass_inproj(K, l, src, 1)
        P.barrier()
        if stop == "A":
            return
        with contextlib.ExitStack() as st3:
            L = layer_consts(K, l, st3)
            P.barrier()
            pass_conv_bwd(K, l, L)
            P.barrier()
            if stop == "B":
                return
            pass_ssd_fwd(K, l, L)
            P.barrier()
            if stop == "C":
                return
        pass_pool(K, l)
        P.barrier()
        if stop == "D1":
            return
        pass_merge(K, l, src)
        P.barrier()
        if stop == "D2":
            return
        pass_dispatch(K, l)
        P.barrier()
        if "dest" in K.S:
            P.op("sp", lambda e: e.dma_start(out=K.S["dest"], in_=K.DEST[:].rearrange("p c k -> p (c k)")), key="dbg1")
            P.op("sp", lambda e: e.dma_start(out=K.S["idxw"], in_=K.IDXW[:]), key="dbg2")
            P.op("sp", lambda e: e.dma_start(out=K.S["e12"], in_=K.E12[:].rearrange("p c k -> p (c k)")), key="dbg1")
            P.op("sp", lambda e: e.dma_start(out=K.S["w12"], in_=K.W12[:].rearrange("p c k -> p (c k)")), key="dbg2")
            P.barrier()
        if stop == "F":
            return
        pass_experts(K, l)
        P.barrier()
        if stop == "G":
            return
        pass_combine(K, l, l == 1)
        P.barrier()


def host_inputs(inputs, b):
    f = np.float32
    m = {}
    m["xcat"] = np.ascontiguousarray(np.concatenate([inputs["ctx"][b], inputs["x"][b]], axis=0), dtype=f)
    cc = np.stack([inputs["c_ctx"].reshape(8, 128).T, inputs["c"][b].reshape(8, 128).T], axis=-1)
    m["cc"] = np.ascontiguousarray(cc, dtype=f)
    m["w_ada"] = np.ascontiguousarray(inputs["w_ada"], dtype=f)
    m["b_ada"] = np.ascontiguousarray(inputs["b_ada"], dtype=f)
    m["w_in"] = np.ascontiguousarray(inputs["w_in"], dtype=f)
    m["b_gate"] = np.ascontiguousarray(inputs["b_gate"].reshape(2, 16, 128).transpose(0, 2, 1), dtype=f)
    m["ident"] = np.eye(128, dtype=f)
    ii = np.arange(128)
    cm = np.zeros((128, 7, 128), f)
    cm[:, 6, :] = (ii[:, None] < ii[None, :])
    cm[:, 0, :] = np.eye(128)
    cm[:, 1, :] = (ii[:, None] <= ii[None, :])
    cm[:, 2, :] = (ii[:, None] >= ii[None, :])
    cm[:, 3, :] = 1.0
    cm[:, 4, :] = np.where(ii[None, :] >= ii[:, None], 0.0, -60000.0)
    cm[:, 5, :] = np.where(ii[None, :] <= ii[:, None], 0.0, -60000.0)
    m["cmat"] = cm
    cw = inputs["conv_w"].reshape(2, 5, 24, 128).transpose(0, 3, 1, 2)
    m["cw"] = np.ascontiguousarray(cw, dtype=f)
    m["cbfm"] = np.ascontiguousarray(inputs["conv_b"].reshape(2, 24, 128).transpose(0, 2, 1), dtype=f)
    m["conv_b"] = np.ascontiguousarray(inputs["conv_b"], dtype=f)
    m["dt_bias"] = np.ascontiguousarray(inputs["dt_bias"].reshape(2, 64), dtype=f)
    m["a_log"] = np.ascontiguousarray(inputs["a_log"].reshape(2, 64), dtype=f)
    m["d_skip"] = np.ascontiguousarray(inputs["d_skip"], dtype=f)
    pp, pc = pool_consts()
    m["poolP"] = pp
    m["poolC"] = pc
    m["pool_w"] = np.ascontiguousarray(inputs["pool_w"], dtype=f)
    m["pscfm"] = np.ascontiguousarray(inputs["pool_scale"].reshape(2, 8, 128).transpose(0, 2, 1), dtype=f)
    m["gnfm"] = np.ascontiguousarray(inputs["ssd_norm_g"].reshape(2, 16, 128).transpose(0, 2, 1), dtype=f)
    for k in ("w_branch_a", "w_branch_b", "w_out", "ln1_g", "ln1_b", "ln2_g", "ln2_b"):
        m[k] = np.ascontiguousarray(inputs[k], dtype=f)
    wre = inputs["w_router_expert"].transpose(0, 2, 1, 3).reshape(2, 1024, 32)
    wrr = np.concatenate([inputs["w_router_group"], wre], axis=-1)
    m["wr"] = np.ascontiguousarray(wrr.reshape(2, 8, 128, 36).transpose(0, 2, 1, 3), dtype=f)
    m["br"] = np.ascontiguousarray(np.concatenate([inputs["b_router_group"], inputs["b_router_expert"].reshape(2, 32)], axis=-1), dtype=f)
    m["wgu"], m["wdr"] = expert_layout(inputs)
    cv = np.zeros((128, 197), f)
    cv[:, 0:66] = 256.0 * np.arange(66)[None, :]
    cv[:, 66:98] = np.arange(32)[None, :]
    cv[:, 98:196] = np.arange(98)[None, :]
    cv[:, 196] = np.arange(128)
    m["cvec"] = cv
    sel = np.zeros((2, 2, 128), f)
    sel[0, 0, :] = 1
    sel[1, 1, :] = 1
    m["sel"] = sel
    return m


_PC = {}
_PC2 = {}


def expert_layout(inputs):
    key = ("wl", id(inputs["w_expert_gate"]))
    global _PC2
    if key not in _PC2:
        gu = np.concatenate([inputs["w_expert_gate"], inputs["w_expert_up"]], axis=-1)
        gu = gu.reshape(2, NE, 8, 128, 1024).transpose(0, 1, 3, 2, 4).reshape(2, NE * 128, 8 * 1024)
        wd = inputs["w_expert_down"].reshape(2, NE, 4, 128, 1024).transpose(0, 1, 3, 2, 4).reshape(2, NE * 128, 4 * 1024)
        _PC2 = {key: (np.ascontiguousarray(gu, dtype=np.float32), np.ascontiguousarray(wd, dtype=np.float32))}
    return _PC2[key]


def pool_consts():
    if "p" in _PC:
        return _PC["p"]
    bf = ml_dtypes.bfloat16
    pp = np.zeros((3, 128, 31, 512), np.float32)
    tp = np.arange(128)
    t = np.arange(512)
    for ti, r0 in enumerate((0, 64, 120)):
        for k in POOLK:
            r = r0 + t // 64
            c = t % 64
            cnt_r = np.minimum(r + k // 2, 128) - np.maximum(r - k // 2, 0)
            cnt_c = np.minimum(c + k // 2, 64) - np.maximum(c - k // 2, 0)
            inv = 1.0 / (cnt_r * cnt_c)
            for d in range(POOL_DMIN[k], POOL_DMAX[k] + 1):
                rp = r0 + 2 * d + tp // 64
                cp = tp % 64
                inwin = ((rp[:, None] >= r[None, :] - k // 2) & (rp[:, None] < r[None, :] + k // 2) &
                         (cp[:, None] >= c[None, :] - k // 2) & (cp[:, None] < c[None, :] + k // 2))
                valid = ((rp >= 0) & (rp < 128))[:, None]
                mat = np.where(inwin & valid, inv[None, :], 0.0)
                mat = mat - ((rp[:, None] == r[None, :]) & (cp[:, None] == c[None, :]))
                pp[ti, :, pool_idx(k, d), :] = mat
    pc = np.zeros((128, 8, 256), np.float32)
    t = np.arange(256)
    for kg, k in enumerate(POOLK):
        cnt = np.minimum(t + k // 2, 256) - np.maximum(t - k // 2, 0)
        inv = 1.0 / cnt
        for sl_ in range(2):
            tpp = sl_ * 128 + tp
            inwin = (tpp[:, None] >= t[None, :] - k // 2) & (tpp[:, None] < t[None, :] + k // 2)
            pc[:, kg * 2 + sl_, :] = np.where(inwin, inv[None, :], 0.0) - (tpp[:, None] == t[None, :])
    _PC["p"] = (pp.astype(bf), pc.astype(bf))
    return _PC["p"]


def kernel(**inputs):
    inputs = {k: np.asarray(v) for k, v in inputs.items()}
    nc = build()
    in_maps = [host_inputs(inputs, b) for b in range(8)]
    res = run_bass_kernel_spmd(nc, in_maps, core_ids=list(range(8)))
    return np.stack([r["out"] for r in res.results], axis=0).astype(np.float32)
```
